# Optimizing a Trainium2 kernel written in Bass

```python
import functools
import jax, jax.numpy as jnp
from jax import lax
import numpy as np

D_MODEL = 1024
BATCH = 8
SEQ = 2048
DEPTH = 1
DEC_BATCH = 32
DEC_SEQ = 8
PAST_LEN = 16384
PAGE_SIZE = 128

MIX_DIM = D_MODEL
HEAD_DIM = 64
CONV_DIM = MIX_DIM // 4
CONV_W = 3
NSA_DIM = MIX_DIM - CONV_DIM
N_Q_HEADS = NSA_DIM // HEAD_DIM
N_KV = 2
Q_PER_KV = N_Q_HEADS // N_KV
CMP_BLOCK = 32
CMP_STRIDE = 16
CMP_HID = 2 * HEAD_DIM
SEL_BLOCK = 64
CMP_PER_SEL = SEL_BLOCK // CMP_STRIDE
N_SELECT = 16
WINDOW = 512
Q_BLOCK = 128
D_FF = -(-8 * D_MODEL // (3 * 256)) * 256
ALPHA = (2 * DEPTH) ** 0.25
BETA = (8 * DEPTH) ** -0.25
EPS = 1e-5
SCALE = HEAD_DIM ** -0.5
KV_COLS = 2 * N_KV * HEAD_DIM
_C3 = 3 * CONV_DIM
SPLITS = (CONV_DIM, 2 * CONV_DIM, _C3, _C3 + NSA_DIM, _C3 + NSA_DIM + KV_COLS,
          _C3 + NSA_DIM + 2 * KV_COLS, _C3 + NSA_DIM + 3 * KV_COLS)
PROJ_DIM = _C3 + NSA_DIM + 3 * KV_COLS + 3 * N_Q_HEADS

kernel_name = 'hybrid_conv_nsa_deepnorm_adaln_step'


def _layernorm(x, g, b):
    xf = x.astype(jnp.float32)
    mu = jnp.mean(xf, axis=-1, keepdims=True)
    var = jnp.mean(jnp.square(xf - mu), axis=-1, keepdims=True)
    return ((xf - mu) * lax.rsqrt(var + EPS) * g.astype(jnp.float32) + b.astype(jnp.float32)).astype(x.dtype)


def _rmsnorm(x, g):
    xf = x.astype(jnp.float32)
    return (xf * lax.rsqrt(jnp.mean(jnp.square(xf), axis=-1, keepdims=True) + EPS) * g.astype(jnp.float32)).astype(x.dtype)


def _modulation(c, w_ada, b_ada):
    mod = jax.nn.silu(c) @ w_ada + b_ada
    return [m[:, None, :] for m in jnp.split(mod, 6, axis=-1)]


def _pad_rows(a, n):
    return jnp.pad(a, [(0, 0), (0, n)] + [(0, 0)] * (a.ndim - 2))


def _in_proj(h, w_in):
    bn, t = h.shape[:2]
    hc, gb, gc, q, kvc, kvs, kvw, gl = jnp.split(h @ w_in, SPLITS, axis=-1)
    kv_shape = (bn, t, 2, N_KV, HEAD_DIM)
    gates = jax.nn.sigmoid(gl.astype(jnp.float32)).astype(h.dtype).reshape(bn, t, 3, N_KV, Q_PER_KV)
    return (gc * hc, gb, q.reshape(bn, t, N_KV, Q_PER_KV, HEAD_DIM),
            kvc.reshape(kv_shape), kvs.reshape(kv_shape), kvw.reshape(kv_shape), gates)


def _short_conv(u_ext, w):
    t = u_ext.shape[1] - (CONV_W - 1)
    y = w[0] * u_ext[:, 0:t]
    for j in range(1, CONV_W):
        y = y + w[j] * u_ext[:, j:j + t]
    return y


def _chunk_partials(rows, w1):
    bn, n = rows.shape[:2]
    ch = rows.reshape(bn, n // CMP_STRIDE, CMP_STRIDE, 2, N_KV, HEAD_DIM)
    top = jnp.einsum('bcjegd,ejdh->bcegh', ch, w1[:, :CMP_STRIDE])
    bot = jnp.einsum('bcjegd,ejdh->bcegh', ch, w1[:, CMP_STRIDE:])
    return top, bot


def _compress(top, bot, pe, w1, b1, w2):
    bias = jnp.einsum('ejd,ejdh->eh', pe, w1) + b1
    hid = jax.nn.gelu(top[:, :-1] + bot[:, 1:] + bias[:, None, :])
    return jnp.einsum('bcegh,ehd->bcegd', hid, w2)


def _masked_softmax(s, mask):
    s = jnp.where(mask, s, -jnp.inf)
    m = jnp.max(s, axis=-1, keepdims=True)
    m = jnp.where(jnp.isfinite(m), m, 0.0)
    e = jnp.exp(s - m)
    return e / jnp.maximum(jnp.sum(e, axis=-1, keepdims=True), 1e-30)


def _attend_shared(q, k, v, mask):
    s = jnp.einsum('btgrd,bsgd->bgrts', q, k).astype(jnp.float32) * SCALE
    p = _masked_softmax(s, mask)
    return jnp.einsum('bgrts,bsgd->btgrd', p.astype(v.dtype), v), p


def _attend_gathered(q, k, v, mask):
    s = jnp.einsum('btgrd,btgsd->btgrs', q, k).astype(jnp.float32) * SCALE
    p = _masked_softmax(s, mask[:, :, :, None, :])
    return jnp.einsum('btgrs,btgsd->btgrd', p.astype(v.dtype), v)


def _block_importance(imp, n_blocks):
    nc = imp.shape[-1]
    lead = [(0, 0)] * (imp.ndim - 1)
    pp = jnp.pad(imp, lead + [(0, CMP_PER_SEL * n_blocks - nc)])
    pp = pp.reshape(imp.shape[:-1] + (n_blocks, CMP_PER_SEL))
    tail = pp[..., -1]
    prev = jnp.pad(tail[..., :-1], lead + [(1, 0)])
    return pp.sum(axis=-1) + prev


def _flatten_sel(kv, idx):
    bn, t, g, k = idx.shape
    pos = idx[..., None] * SEL_BLOCK + jnp.arange(SEL_BLOCK)
    return (kv.reshape(bn, t, g, k * SEL_BLOCK, 2, HEAD_DIM), pos.reshape(bn, t, g, k * SEL_BLOCK))


def _gather_local(blocks, idx):
    b = jnp.arange(idx.shape[0])[:, None, None, None]
    g = jnp.arange(N_KV)[None, None, :, None]
    return _flatten_sel(blocks[b, idx, :, :, g, :], idx)


def _gather_paged(pool_blocks, new_blocks, page_table, blocks_per_page, idx):
    n_past = page_table.shape[1] * blocks_per_page
    n_new = new_blocks.shape[1]
    b = jnp.arange(idx.shape[0])[:, None, None, None]
    g = jnp.arange(N_KV)[None, None, :, None]
    jp = jnp.minimum(idx, n_past - 1)
    phys = page_table[b, jp // blocks_per_page] * blocks_per_page + jp % blocks_per_page
    kv_past = pool_blocks[phys, :, :, g, :]
    kv_new = new_blocks[b, jnp.clip(idx - n_past, 0, n_new - 1), :, :, g, :]
    kv = jnp.where((idx < n_past)[..., None, None, None], kv_past, kv_new)
    return _flatten_sel(kv, idx)


def _nsa_core(q, q_pos, gates, kv_cmpr, cmp_end, n_blocks, gather_sel, kvw, w_pos):
    cmask = cmp_end[None, :] <= q_pos[:, None]
    o_cmp, p_cmp = _attend_shared(q, kv_cmpr[:, :, 0], kv_cmpr[:, :, 1], cmask)
    imp = jnp.transpose(p_cmp.sum(axis=2), (0, 2, 1, 3))
    score = _block_importance(imp, n_blocks)
    j = jnp.arange(n_blocks)[None, :]
    cur = (q_pos // SEL_BLOCK)[:, None]
    valid = (j * SEL_BLOCK <= q_pos[:, None])[None, :, None, :]
    forced = ((j == 0) | (j == cur) | (j == cur - 1))[None, :, None, :]
    score = jnp.where(forced, jnp.inf, jnp.where(valid, score, -jnp.inf))
    _, idx = lax.top_k(score, min(N_SELECT, n_blocks))
    kvs, spos = gather_sel(idx)
    smask = spos <= q_pos[None, :, None, None]
    o_slc = _attend_gathered(q, kvs[..., 0, :], kvs[..., 1, :], smask)
    wmask = (w_pos[None, :] <= q_pos[:, None]) & (w_pos[None, :] > q_pos[:, None] - WINDOW) & (w_pos[None, :] >= 0)
    o_win, _ = _attend_shared(q, kvw[:, :, 0], kvw[:, :, 1], wmask)
    g = gates[..., None]
    return g[:, :, 0] * o_cmp + g[:, :, 1] * o_slc + g[:, :, 2] * o_win


def _prompt_mixer(h, w_in, conv_w, cmp_params):
    bn, s = h.shape[:2]
    u, gb, q, kvc, kvs, kvw, gates = _in_proj(h, w_in)
    y_conv = gb * _short_conv(jnp.pad(u, ((0, 0), (CONV_W - 1, 0), (0, 0))), conv_w)
    top, bot = _chunk_partials(_pad_rows(kvc, (-s) % CMP_STRIDE), cmp_params[1])
    kv_cmpr = _compress(top, bot, *cmp_params)
    cmp_end = CMP_STRIDE * jnp.arange(kv_cmpr.shape[1]) + (CMP_BLOCK - 1)
    ns = -(-s // SEL_BLOCK)
    slc_blocks = _pad_rows(kvs, ns * SEL_BLOCK - s).reshape(bn, ns, SEL_BLOCK, 2, N_KV, HEAD_DIM)
    kvw_pad = jnp.pad(kvw, ((0, 0), (WINDOW, 0), (0, 0), (0, 0), (0, 0)))
    qb = min(Q_BLOCK, s)
    nqb = s // qb

    def one_block(item):
        b = item // nqb
        start = (item % nqb) * qb
        pick = lambda a: lax.dynamic_slice_in_dim(a, b, 1, axis=0)
        q_i = lax.dynamic_slice_in_dim(pick(q), start, qb, axis=1)
        g_i = lax.dynamic_slice_in_dim(pick(gates), start, qb, axis=1)
        kvw_i = lax.dynamic_slice_in_dim(pick(kvw_pad), start, qb + WINDOW, axis=1)
        q_pos = start + jnp.arange(qb)
        w_pos = start - WINDOW + jnp.arange(qb + WINDOW)
        gather = functools.partial(_gather_local, pick(slc_blocks))
        return _nsa_core(q_i, q_pos, g_i, pick(kv_cmpr), cmp_end, ns, gather, kvw_i, w_pos)

    o = lax.map(one_block, jnp.arange(bn * nqb)).reshape(bn, s, NSA_DIM)
    w_keep = min(WINDOW, s)
    return y_conv, o, (kvc, kvs, kvw[:, s - w_keep:], u[:, s - (CONV_W - 1):])


def _sample_mixer(h, cmp_pool, slc_pool, win_buf, conv_buf, page_table, w_in, conv_w, cmp_params):
    bn, t = h.shape[:2]
    n_pages = page_table.shape[1]
    page = cmp_pool.shape[1]
    past = n_pages * page
    u, gb, q, kvc, kvs, kvw, gates = _in_proj(h, w_in)
    u_ext = jnp.concatenate([conv_buf.astype(u.dtype), u], axis=1)
    y_conv = gb * _short_conv(u_ext, conv_w)
    past_cmp = cmp_pool[page_table].reshape(bn, past, 2, N_KV, HEAD_DIM)
    top_p, bot_p = _chunk_partials(past_cmp, cmp_params[1])
    top_n, bot_n = _chunk_partials(_pad_rows(kvc, (-t) % CMP_STRIDE), cmp_params[1])
    kv_cmpr = _compress(jnp.concatenate([top_p, top_n], axis=1), jnp.concatenate([bot_p, bot_n], axis=1), *cmp_params)
    cmp_end = CMP_STRIDE * jnp.arange(kv_cmpr.shape[1]) + (CMP_BLOCK - 1)
    bpp = page // SEL_BLOCK
    nnb = -(-t // SEL_BLOCK)
    ns = n_pages * bpp + nnb
    new_blocks = _pad_rows(kvs, nnb * SEL_BLOCK - t).reshape(bn, nnb, SEL_BLOCK, 2, N_KV, HEAD_DIM)
    pool_blocks = slc_pool.reshape(-1, SEL_BLOCK, 2, N_KV, HEAD_DIM)
    gather = functools.partial(_gather_paged, pool_blocks, new_blocks, page_table, bpp)
    kvw_all = jnp.concatenate([win_buf, kvw], axis=1)
    w_buf = win_buf.shape[1]
    q_pos = past + jnp.arange(t)
    w_pos = past - w_buf + jnp.arange(w_buf + t)
    o = _nsa_core(q, q_pos, gates, kv_cmpr, cmp_end, ns, gather, kvw_all, w_pos)
    return y_conv, o.reshape(bn, t, NSA_DIM), (kvc, kvs, kvw_all[:, t:], u_ext[:, t:])


def _merge_groups(y_conv, o_nsa, g_conv, g_nsa, w_o):
    return jnp.concatenate([_rmsnorm(y_conv, g_conv), _rmsnorm(o_nsa, g_nsa)], axis=-1) @ w_o


def _post_block(x, mix, mods, ln1_g, ln1_b, ln2_g, ln2_b, w_gate, w_up, w_down):
    gate1, shift2, scale2, gate2 = mods[2], mods[3], mods[4], mods[5]
    x = _layernorm(ALPHA * x + gate1 * mix, ln1_g, ln1_b)
    h = x * (1 + scale2) + shift2
    f = (jax.nn.silu(h @ w_gate) * (h @ w_up)) @ w_down
    return _layernorm(ALPHA * x + gate2 * f, ln2_g, ln2_b)


def setup_inputs(seed: int = 0) -> dict:
    key = jax.random.key(seed)
    ks = iter(jax.random.split(key, 32))

    def nrm(shape, scale=1.0):
        return jax.random.normal(next(ks), shape, jnp.float32) * scale

    n_pages = PAST_LEN // PAGE_SIZE
    n_used = DEC_BATCH * n_pages
    n_phys = n_used + max(1, n_used // 4)
    w_buf = min(WINDOW, PAST_LEN)
    L = DEPTH
    page_table = jax.random.permutation(next(ks), n_phys)[:n_used].reshape(DEC_BATCH, n_pages).astype(jnp.int32)
    return {
        'x_prompt': nrm((BATCH, SEQ, D_MODEL)),
        'x_sample': nrm((DEC_BATCH, DEC_SEQ, D_MODEL)),
        'cache_cmp_kv': nrm((L, n_phys, PAGE_SIZE, 2, N_KV, HEAD_DIM)),
        'cache_slc_kv': nrm((L, n_phys, PAGE_SIZE, 2, N_KV, HEAD_DIM)),
        'cache_win_kv': nrm((L, DEC_BATCH, w_buf, 2, N_KV, HEAD_DIM)),
        'state_conv': nrm((L, DEC_BATCH, CONV_W - 1, CONV_DIM)),
        'page_table': page_table,
        'c_prompt': nrm((BATCH, D_MODEL)),
        'c_sample': nrm((DEC_BATCH, D_MODEL)),
        'w_ada': nrm((L, D_MODEL, 6 * D_MODEL), D_MODEL ** -0.5),
        'b_ada': nrm((L, 6 * D_MODEL), 0.02),
        'w_in': nrm((L, D_MODEL, PROJ_DIM), D_MODEL ** -0.5),
        'conv_w': nrm((L, CONV_W, CONV_DIM), CONV_W ** -0.5),
        'cmp_pe': nrm((L, 2, CMP_BLOCK, HEAD_DIM), 0.1),
        'cmp_w1': nrm((L, 2, CMP_BLOCK, HEAD_DIM, CMP_HID), (CMP_BLOCK * HEAD_DIM) ** -0.5),
        'cmp_b1': nrm((L, 2, CMP_HID), 0.02),
        'cmp_w2': nrm((L, 2, CMP_HID, HEAD_DIM), CMP_HID ** -0.5),
        'g_conv_out': 1.0 + nrm((L, CONV_DIM), 0.02),
        'g_nsa_out': 1.0 + nrm((L, NSA_DIM), 0.02),
        'w_o': nrm((L, MIX_DIM, D_MODEL), BETA * MIX_DIM ** -0.5),
        'ln1_g': 1.0 + nrm((L, D_MODEL), 0.02),
        'ln1_b': nrm((L, D_MODEL), 0.02),
        'ln2_g': 1.0 + nrm((L, D_MODEL), 0.02),
        'ln2_b': nrm((L, D_MODEL), 0.02),
        'w_ffn_gate': nrm((L, D_MODEL, D_FF), D_MODEL ** -0.5),
        'w_ffn_up': nrm((L, D_MODEL, D_FF), D_MODEL ** -0.5),
        'w_ffn_down': nrm((L, D_FF, D_MODEL), BETA * D_FF ** -0.5),
    }


def reference(x_prompt, x_sample, cache_cmp_kv, cache_slc_kv, cache_win_kv, state_conv, page_table,
              c_prompt, c_sample, w_ada, b_ada, w_in, conv_w, cmp_pe, cmp_w1, cmp_b1, cmp_w2,
              g_conv_out, g_nsa_out, w_o, ln1_g, ln1_b, ln2_g, ln2_b, w_ffn_gate, w_ffn_up, w_ffn_down):
    xp, xs = x_prompt, x_sample
    acc_p = [[], [], [], []]
    acc_s = [[], [], [], []]
    for l in range(DEPTH):
        cmp_params = (cmp_pe[l], cmp_w1[l], cmp_b1[l], cmp_w2[l])
        ffn = (ln1_g[l], ln1_b[l], ln2_g[l], ln2_b[l], w_ffn_gate[l], w_ffn_up[l], w_ffn_down[l])
        mods = _modulation(c_prompt, w_ada[l], b_ada[l])
        y_c, o_n, new = _prompt_mixer(xp * (1 + mods[1]) + mods[0], w_in[l], conv_w[l], cmp_params)
        xp = _post_block(xp, _merge_groups(y_c, o_n, g_conv_out[l], g_nsa_out[l], w_o[l]), mods, *ffn)
        for a, v in zip(acc_p, new):
            a.append(v)
        mods = _modulation(c_sample, w_ada[l], b_ada[l])
        y_c, o_n, new = _sample_mixer(xs * (1 + mods[1]) + mods[0], cache_cmp_kv[l], cache_slc_kv[l],
                                      cache_win_kv[l], state_conv[l], page_table, w_in[l], conv_w[l], cmp_params)
        xs = _post_block(xs, _merge_groups(y_c, o_n, g_conv_out[l], g_nsa_out[l], w_o[l]), mods, *ffn)
        for a, v in zip(acc_s, new):
            a.append(v)
    cmp_p, slc_p, win_p, conv_p = [jnp.stack(a) for a in acc_p]
    cmp_s, slc_s, win_s, conv_s = [jnp.stack(a) for a in acc_s]
    return (xp, xs, cmp_p, slc_p, win_p, conv_p, cmp_s, slc_s, win_s, conv_s)
```

```python
import numpy as np
from contextlib import ExitStack
import concourse.bass as bass
import concourse.mybir as mybir
from concourse.bass_utils import run_bass_kernel_spmd

F32 = mybir.dt.float32
BF16 = mybir.dt.bfloat16
I32 = mybir.dt.int32
AF = mybir.ActivationFunctionType
ALU = mybir.AluOpType
AX = mybir.AxisListType

D = 1024
SEQ = 2048
NT = 16
NPAGE = 128
NPHYS = 5120
PROJ = 2340
DFF = 2816
NFF = 22
ALPHA = 2 ** 0.25
EPS = 1e-5
SCALE = 0.125
N_CORES = 8
STAGE = 9
DO_PROMPT = 1
SUB = 9
DBG = 0


class T:
    def __init__(self, handle, name, space):
        self.h = handle
        self.name = name
        self.space = space
        self.w = None
        self.r = {}
        self.dsem = None
        self.dcount = 0

    def __getitem__(self, idx):
        return self.h[idx]


class Gen:
    ENG = ("pe", "act", "dve", "pool", "sp")

    def __init__(self, nc, stack):
        self.nc = nc
        self.stack = stack
        self.ops = {e: [] for e in self.ENG}
        self.cnt = {e: 0 for e in self.ENG}
        self.seen = {e: {} for e in self.ENG}
        self.sems = {}
        self.cur = {}
        for e in self.ENG[:4]:
            self.sems[e] = self.stack.enter_context(self.nc.semaphore("c_" + e))
        self.out_marks = {}

    def sb(self, name, shape, dtype=F32, stack=None):
        h = (stack or self.stack).enter_context(self.nc.sbuf_tensor("s_" + name, list(shape), dtype))
        return T(h, name, "sbuf")

    def ps(self, name, shape, dtype=F32):
        h = self.stack.enter_context(self.nc.psum_tensor("p_" + name, list(shape), dtype))
        return T(h, name, "psum")

    def _wait(self, e, k, v):
        if e == "pe" and k == "pe":
            return
        if self.seen[e].get(k, 0) < v:
            self.seen[e][k] = v
            sem = self.sems[k]
            self.ops[e].append(lambda eng, sem=sem, v=v: eng.wait_ge(sem, v))

    def _deps(self, e, reads, writes):
        deps = {}
        for t in reads:
            if t.w is not None:
                k, v = t.w
                deps[k] = max(deps.get(k, 0), v)
        for t in writes:
            if t.w is not None:
                k, v = t.w
                deps[k] = max(deps.get(k, 0), v)
            for k, v in t.r.items():
                deps[k] = max(deps.get(k, 0), v)
        for k, v in deps.items():
            self._wait(e, k, v)

    def _mark(self, key, val, reads, writes):
        self.cur[key] = max(self.cur.get(key, 0), val)
        for t in reads:
            t.r[key] = max(t.r.get(key, 0), val)
        for t in writes:
            t.w = (key, val)
            t.r = {}

    def emit(self, e, fn, reads=(), writes=(), signal=True):
        self._deps(e, reads, writes)
        if signal:
            self.cnt[e] += 1
            sem = self.sems[e]
            self.ops[e].append(lambda eng, fn=fn, sem=sem: fn(eng).then_inc(sem, 1))
            val = self.cnt[e]
        else:
            self.ops[e].append(lambda eng, fn=fn: fn(eng))
            val = self.cnt[e] + 1
        self._mark(e, val, reads, writes)

    def dma(self, q, fn, reads=(), writes=(), track=None, is_output=False):
        if track is None:
            for t in list(writes) + list(reads):
                if t.space == "sbuf":
                    track = t
                    break
            if track is None:
                track = (list(writes) + list(reads))[0]
        if track.dsem is None:
            key = "d_" + track.name
            assert key not in self.sems, key
            self.sems[key] = self.stack.enter_context(self.nc.semaphore(key))
            track.dsem = key
        key = track.dsem
        self._deps(q, reads, writes)
        track.dcount += 1
        val = 16 * track.dcount
        sem = self.sems[key]
        self.ops[q].append(lambda eng, fn=fn, sem=sem: fn(eng).then_inc(sem, 16))
        self._mark(key, val, reads, writes)
        if is_output:
            self.out_marks[key] = max(self.out_marks.get(key, 0), val)

    def barrier(self):
        for e in self.ENG:
            for k, v in list(self.cur.items()):
                self._wait(e, k, v)

    def mm(self, out, lhsT, rhs, start, stop, reads, writes, signal=None):
        if signal is None:
            signal = stop
        self.emit("pe", lambda e: e.matmul(out, lhsT, rhs, start=start, stop=stop),
                  reads=reads, writes=writes, signal=signal)

    def tr(self, out, in_, ident, reads, writes):
        self.emit("pe", lambda e: e.transpose(out, in_, ident), reads=reads, writes=writes)

    def act(self, out, in_, func, reads, writes, bias=None, scale=None, accum_out=None):
        kw = {}
        if bias is not None:
            kw["bias"] = bias
        if scale is not None:
            kw["scale"] = scale
        if accum_out is not None:
            kw["accum_out"] = accum_out
        self.emit("act", lambda e: e.activation(out=out, in_=in_, func=func, **kw), reads=reads, writes=writes)

    def tt(self, out, in0, in1, op, reads, writes, eng="dve"):
        self.emit(eng, lambda e: e.tensor_tensor(out=out, in0=in0, in1=in1, op=op), reads=reads, writes=writes)

    def ts(self, out, in0, s1, s2, op0, op1, reads, writes, eng="dve"):
        if op1 is None:
            self.emit(eng, lambda e: e.tensor_scalar(out=out, in0=in0, scalar1=s1, scalar2=None, op0=op0),
                      reads=reads, writes=writes)
        else:
            self.emit(eng, lambda e: e.tensor_scalar(out=out, in0=in0, scalar1=s1, scalar2=s2, op0=op0, op1=op1),
                      reads=reads, writes=writes)

    def stt(self, out, in0, scalar, in1, op0, op1, reads, writes, eng="dve"):
        self.emit(eng, lambda e: e.scalar_tensor_tensor(out=out, in0=in0, scalar=scalar, in1=in1, op0=op0, op1=op1),
                  reads=reads, writes=writes)

    def cp(self, out, in_, reads, writes, eng="dve"):
        if eng == "act":
            self.act(out, in_, AF.Copy, reads, writes)
        else:
            self.emit(eng, lambda e: e.tensor_copy(out=out, in_=in_), reads=reads, writes=writes)

    def rsqrt(self, ap, tiles):
        self.act(ap, ap, AF.Sqrt, tiles, tiles)
        self.emit("dve", lambda e: e.reciprocal(out=ap, in_=ap), reads=tiles, writes=tiles)

    def memset(self, ap, val, writes, eng="dve"):
        self.emit(eng, lambda e: e.memset(ap, val), writes=writes)

    def ld(self, q, out, in_, reads, writes, nc_ok=False, is_output=False, track=None):
        if nc_ok:
            self.dma(q, lambda e: e.dma_start(out=out, in_=in_, allow_slow_non_contiguous=True),
                     reads=reads, writes=writes, is_output=is_output, track=track)
        else:
            self.dma(q, lambda e: e.dma_start(out=out, in_=in_), reads=reads, writes=writes,
                     is_output=is_output, track=track)

    def finish(self):
        for k, v in self.out_marks.items():
            self._wait("sp", k, v)
        for e in ("pe", "act", "dve"):
            self._wait("sp", e, self.cnt[e])
        with self.nc.Block() as block:
            @block.tensor
            def _(eng):
                for f in self.ops["pe"]:
                    f(eng)

            @block.scalar
            def _(eng):
                for f in self.ops["act"]:
                    f(eng)

            @block.vector
            def _(eng):
                for f in self.ops["dve"]:
                    f(eng)

            @block.gpsimd
            def _(eng):
                for f in self.ops["pool"]:
                    f(eng)

            @block.sync
            def _(eng):
                for f in self.ops["sp"]:
                    f(eng)


def host_constants():
    c = {}
    c["ident"] = np.eye(128, dtype=np.float32)
    s = np.arange(128)[:, None]
    q = np.arange(128)[None, :]
    c["tri_le"] = (s <= q).astype(np.float32)
    c["tri_gt"] = (s > q).astype(np.float32)
    i = np.arange(128)[:, None]
    qq = np.arange(SEQ)[None, :]
    cm = ((16 * i + 31) <= qq).astype(np.float32)
    cm[127, :] = 0.0
    c["cmpmask"] = cm
    qpos = (np.arange(NT)[None, :, None] * 128 + np.arange(128)[:, None, None])
    j = np.arange(32)[None, None, :]
    cur = qpos // 64
    forced = (j == 0) | (j == cur) | (j == cur - 1)
    c["force"] = np.where(forced, 1e9 * (1.0 + j / 64.0), 0.0).astype(np.float32)
    c["validneg"] = np.where(j * 64 <= qpos, 0.0, -1e30).astype(np.float32)
    L = np.zeros((32, NT, 128), np.float32)
    for kt in range(NT):
        L[2 * kt, kt, :64] = 1.0
        L[2 * kt + 1, kt, 64:] = 1.0
    c["lsel"] = L
    A = np.zeros((128, 32), np.float32)
    for jj in range(32):
        for ii in range(4 * jj - 1, 4 * jj + 4):
            if 0 <= ii <= 126:
                A[ii, jj] = 1.0
    c["acmp"] = A
    As = np.zeros((128, 8, 257), np.float32)
    for jj in range(257):
        for ii in range(4 * jj - 1, 4 * jj + 4):
            if 0 <= ii <= 1022:
                As[ii % 128, ii // 128, jj] = 1.0
    c["as_"] = As
    fs = np.zeros((8, 257), np.float32)
    for jj in (0, 255, 256):
        fs[:, jj] = 1e9 * (1.0 + jj / 1024.0)
    c["force_s"] = fs
    qi = np.arange(48)
    tq = qi % 8
    c["gsum"] = (tq[:, None] == np.arange(8)[None, :]).astype(np.float32)
    c["gt"] = (np.arange(8)[:, None] == tq[None, :]).astype(np.float32)
    c["causal_new"] = (np.arange(8)[:, None] <= tq[None, :]).astype(np.float32)
    c["mw0"] = (np.arange(128)[:, None] > tq[None, :]).astype(np.float32)
    return c


CONST_SHAPES = {"as_": [128, 8, 257], "force_s": [8, 257], "gsum": [48, 8], "gt": [8, 48], "causal_new": [8, 48],
                "mw0": [128, 48], "ident": [128, 128], "tri_le": [128, 128], "tri_gt": [128, 128], "cmpmask": [128, SEQ],
                "force": [128, NT, 32], "validneg": [128, NT, 32], "lsel": [32, NT, 128], "acmp": [128, 32]}

IN_SHAPES = {
    "xp": ([SEQ, D], F32), "xs": ([32, D], F32),
    "cmp_pool": ([NPHYS * 16, 2048], F32), "slc_pool": ([NPHYS * 16, 2048], F32),
    "win": ([4, 512, 256], F32), "sconvT": ([128, 2, 4, 2], F32), "ptT": ([128, 4], I32),
    "cvecT": ([128, 8, 5], F32),
    "w_ada": ([D, 6 * D], F32), "b_ada": ([6 * D], F32), "b_adaT": ([128, 48], F32), "w_in": ([D, PROJ], F32),
    "conv_wT": ([128, 2, 3], F32), "peT": ([128, 64], F32), "w1rep": ([128, 2 * 32 * 128], F32),
    "b1T": ([128, 2], F32), "w2k": ([128, 128], F32), "w2v": ([128, 64], F32), "g_convT": ([128, 2], F32), "g_nsaT": ([128, 6], F32),
    "w_o": ([D, D], F32), "ln1_g": ([D], F32), "ln1_b": ([D], F32), "ln2_g": ([D], F32), "ln2_b": ([D], F32),
    "w_g": ([D, DFF], F32), "w_u": ([D, DFF], F32), "w_d": ([DFF, D], F32),
}
OUT_SHAPES = {
    "y_p": [SEQ, D], "y_s": [32, D], "cmp_p": [SEQ, 256], "slc_p": [SEQ, 256], "win_p": [512, 256],
    "conv_p": [128, 2, 2], "cmp_s": [32, 256], "slc_s": [32, 256], "win_s": [4, 512, 256], "conv_s": [128, 2, 4, 2],
}


def build_nc(do_attn=True, do_post=True):
    nc = bass.Bass("TRN2", target_bir_lowering=False)
    I = {}
    for k, (shp, dt) in IN_SHAPES.items():
        I[k] = nc.dram_tensor(k, shp, dt, kind="ExternalInput").ap()
    for k, shp in CONST_SHAPES.items():
        I[k] = nc.dram_tensor("k_" + k, shp, F32, kind="ExternalInput").ap()
    O = {}
    for k, shp in OUT_SHAPES.items():
        O[k] = nc.dram_tensor(k, shp, F32, kind="ExternalOutput").ap()

    if DBG:
        for k, shp in {"dbg_o": [32, 768], "dbg_oc": [4, 48, 130], "dbg_os": [4, 48, 130], "dbg_ow": [4, 48, 130],
                       "dbg_sc": [4, 2, 8, 257]}.items():
            O[k] = nc.dram_tensor(k, shp, F32, kind="ExternalOutput").ap()
    wscr = {}
    for nm in ("wg_bf", "wu_bf", "wd_bf"):
        wscr[nm] = nc.dram_tensor(nm, [NFF, 128, 1024], BF16, kind="Internal").ap()
    with ExitStack() as st:
        g = Gen(nc, st)
        WSC = {nm: [T(None, "%s_t%d" % (nm, i), "dram") for i in range(NFF)] for nm in wscr}
        odram = T(None, "odram", "dram")

        PS = [g.ps("ps%d" % i, [128, 512], F32) for i in range(8)]


        modT = g.sb("modT", [128, 48, 5], F32)
        gate_p = g.sb("gate_p", [128, 2, D], F32)
        ops1 = g.sb("ops1", [128, 8, 5], F32)
        ops2 = g.sb("ops2", [128, 8, 5], F32)
        qT = g.sb("qT", [128, 6, SEQ], BF16)
        ksT = g.sb("ksT", [128, SEQ], BF16)
        kwT = g.sb("kwT", [128, SEQ], BF16)
        ycT = g.sb("ycT", [128, 2, SEQ], BF16)
        v_aug = g.sb("v_aug", [128, NT, 2, 2, 65], BF16)
        sig = g.sb("sig", [128, NT, 36], F32)
        rc = g.sb("rc", [128, NT], F32)

        ident = g.sb("ident", [128, 128], F32)
        g.ld("sp", ident[:], I["ident"], [], [ident])
        tri_le = g.sb("tri_le", [128, 128], BF16)
        tri_gt = g.sb("tri_gt", [128, 128], BF16)
        g.ld("pool", tri_le[:], I["tri_le"], [], [tri_le])
        g.ld("pool", tri_gt[:], I["tri_gt"], [], [tri_gt])
        cmpmask = g.sb("cmpmask", [128, SEQ], BF16)
        g.ld("pool", cmpmask[:], I["cmpmask"], [], [cmpmask])
        force = g.sb("force", [128, NT, 32], F32)
        validneg = g.sb("validneg", [128, NT, 32], F32)
        g.ld("sp", force[:], I["force"], [], [force])
        g.ld("sp", validneg[:], I["validneg"], [], [validneg])
        lsel = g.sb("lsel", [32, NT, 128], BF16)
        g.ld("pool", lsel[:], I["lsel"], [], [lsel])
        ones_f = g.sb("ones_f", [128, 1], F32)
        g.memset(ones_f[:], 1.0, [ones_f])

        b_adaT = g.sb("b_adaT", [128, 48], F32)
        g.ld("sp", b_adaT[:], I["b_adaT"], [], [b_adaT])
        conv_wT = g.sb("conv_wT", [128, 2, 3], F32)
        g.ld("sp", conv_wT[:], I["conv_wT"], [], [conv_wT])
        g_convT = g.sb("g_convT", [128, 2], F32)
        g.ld("sp", g_convT[:], I["g_convT"], [], [g_convT])
        g_nsaT = g.sb("g_nsaT", [128, 6], F32)
        g.ld("sp", g_nsaT[:], I["g_nsaT"], [], [g_nsaT])
        b1T = g.sb("b1T", [128, 2], F32)
        g.ld("sp", b1T[:], I["b1T"], [], [b1T])
        stC = ExitStack()
        kcT = g.sb("kcT", [128, 2, SEQ], BF16, stack=stC)
        stS = ExitStack()
        ycTs = g.sb("ycTs", [128, 2, 32], BF16, stack=stS)
        ysqs = g.sb("ysqs", [128, 2, 32], F32, stack=stS)
        gate_s = g.sb("gate_s", [32, 2, D], F32, stack=stS)
        Ps = g.sb("Ps", [32, PROJ], F32, stack=stS)
        sigS = g.sb("sigS", [32, 36], F32, stack=stS)
        qTs = g.sb("qTs", [128, 4, 6, 8], BF16, stack=stS)
        ksTs = g.sb("ksTs", [128, 32], BF16, stack=stS)
        kwTs = g.sb("kwTs", [128, 32], BF16, stack=stS)
        Pv = g.sb("Pv", [32, 2, 128], BF16, stack=stS)
        vnew = g.sb("vnew", [8, 4, 2, 2, 65], BF16, stack=stS)
        gq = g.sb("gq", [48, 4, 2, 3], F32, stack=stS)
        rc_s = g.sb("rc_s", [32, 1], F32, stack=stS)
        o_tok = g.sb("o_tok", [32, 768], F32, stack=stS)
        stA = ExitStack()
        bgate = g.sb("bgate", [128, 2, D], F32, stack=stA)
        for i, m in enumerate((2, 5)):
            g.ld("sp", bgate[:, i, :], I["b_ada"][m * D:(m + 1) * D].rearrange("(o d) -> o d", o=1).to_broadcast([128, D]),
                 [], [bgate])

        cT = g.sb("cT", [128, 8, 5], F32, stack=stA)
        g.ld("sp", cT[:], I["cvecT"], [], [cT])
        scT = g.sb("scT", [128, 8, 5], BF16, stack=stA)
        g.act(scT[:], cT[:], AF.Silu, [cT], [scT])
        rep_p = g.sb("rep_p", [128, 8, 128], BF16, stack=stA)
        g.cp(rep_p[:], scT[:, :, 0:1].to_broadcast([128, 8, 128]), [scT], [rep_p])
        rep_s = g.sb("rep_s", [128, 8, 32], BF16, stack=stA)
        for b in range(4):
            g.cp(rep_s[:, :, b * 8:(b + 1) * 8], scT[:, :, 1 + b:2 + b].to_broadcast([128, 8, 8]), [scT], [rep_s])
        wa = [g.sb("wa%d" % i, [128, 8, 512], BF16, stack=stA) for i in range(2)]
        w_ada_v = I["w_ada"].rearrange("(k p) n -> p k n", p=128)
        for nb in range(12):
            w = wa[nb % 2]
            g.ld("pool", w[:, :, :], w_ada_v[:, :, nb * 512:(nb + 1) * 512], [], [w])
            pm = PS[nb % 2]
            for sub in range(4):
                for kc in range(8):
                    g.mm(pm[:, sub * 5:(sub + 1) * 5], w[:, kc, sub * 128:(sub + 1) * 128], scT[:, kc, :],
                         kc == 0, kc == 7, [w, scT], [pm])
            g.tt(modT[:, nb * 4:(nb + 1) * 4, :], pm[:, 0:20].rearrange("p (s m) -> p s m", m=5),
                 b_adaT[:, nb * 4:(nb + 1) * 4].rearrange("p (s o) -> p s o", o=1).to_broadcast([128, 4, 5]), ALU.add,
                 [pm, b_adaT], [modT])
            if nb in (4, 5, 10, 11):
                gi = 0 if nb < 6 else 1
                half = nb % 2
                pg = PS[2 + nb % 2]
                for kc in range(8):
                    g.mm(pg[:, :], rep_p[:, kc, :], w[:, kc, :], kc == 0, kc == 7, [rep_p, w], [pg])
                g.tt(gate_p[:, gi, half * 512:(half + 1) * 512], pg[:, :], bgate[:, gi, half * 512:(half + 1) * 512],
                     ALU.add, [pg, bgate], [gate_p])
                pg2 = PS[4 + nb % 2]
                for kc in range(8):
                    g.mm(pg2[0:32, :], rep_s[:, kc, :], w[:, kc, :], kc == 0, kc == 7, [rep_s, w], [pg2])
                g.tt(gate_s[:, gi, half * 512:(half + 1) * 512], pg2[0:32, :], bgate[0:32, gi, half * 512:(half + 1) * 512],
                     ALU.add, [pg2, bgate], [gate_s])
        g.ts(ops1[:], modT[:, 8:16, :], 1.0, None, ALU.add, None, [modT], [ops1])
        g.ts(ops2[:], modT[:, 32:40, :], 1.0, None, ALU.add, None, [modT], [ops2])

        g.barrier()
        stA.close()
        stB = ExitStack()
        w_in_sb = g.sb("w_in_sb", [128, 8, PROJ], BF16, stack=stB)
        w_in_v = I["w_in"].rearrange("(k p) n -> p k n", p=128)
        w_in_k = [T(None, "w_in_k%d" % kc, "sbuf") for kc in range(8)]
        for kc in range(8):
            g.ld("pool", w_in_sb[:, kc, :], w_in_v[:, kc, :], [], [w_in_k[kc]])
        hT = g.sb("hT", [128, 8, SEQ], BF16, stack=stB)
        hT_t = [T(None, "hT_t%d" % i, "sbuf") for i in range(NT)]
        xin = [g.sb("xin%d" % i, [128, D], F32, stack=stB) for i in range(1)]
        for tt in range(NT):
            xt = xin[0]
            g.ld("sp", xt[:], I["xp"][tt * 128:(tt + 1) * 128, :], [], [xt])
            for hf in range(2):
                pb = PS[hf]
                for k4 in range(4):
                    kc = hf * 4 + k4
                    g.tr(pb[:, k4 * 128:(k4 + 1) * 128], xt[:, kc * 128:(kc + 1) * 128], ident[:], [xt, ident], [pb])
                for k4 in range(4):
                    kc = hf * 4 + k4
                    g.act(hT[:, kc, tt * 128:(tt + 1) * 128], pb[:, k4 * 128:(k4 + 1) * 128], AF.Identity,
                          [pb, ops1, modT], [hT_t[tt]], bias=modT[:, kc, 0:1], scale=ops1[:, kc, 0:1])

        hTs = g.sb("hTs", [128, 8, 32], BF16, stack=stB)
        xs_sb = g.sb("xs_sb", [32, D], F32, stack=stB)
        g.ld("sp", xs_sb[:], I["xs"], [], [xs_sb])
        for hf in range(2):
            pb = PS[2 + hf]
            for k4 in range(4):
                kc = hf * 4 + k4
                g.tr(pb[:, k4 * 32:(k4 + 1) * 32], xs_sb[:, kc * 128:(kc + 1) * 128], ident[0:32, 0:32], [xs_sb, ident], [pb])
            for k4 in range(4):
                kc = hf * 4 + k4
                for b in range(4):
                    g.act(hTs[:, kc, b * 8:(b + 1) * 8], pb[:, k4 * 32 + b * 8:k4 * 32 + (b + 1) * 8], AF.Identity,
                          [pb, ops1, modT], [hTs], bias=modT[:, kc, 1 + b:2 + b], scale=ops1[:, kc, 1 + b:2 + b])

        g.memset(v_aug[:, :, :, :, 64:65], 1.0, [v_aug])

        def fm_cols(base, r=None):
            return None

        uT = g.sb("uT", [128, 2, 514], F32, stack=stB)
        g.memset(uT[:], 0.0, [uT])
        hc_sb = g.sb("hc_sb", [128, 512], F32, stack=stB)
        acc = g.sb("acc", [128, 512], F32, stack=stB)
        ycf = g.sb("ycf", [128, 512], F32, stack=stB)
        ysq = g.sb("ysq", [128, 2, 512], F32, stack=stB)
        psi = [0]

        def next_ps():
            psi[0] = (psi[0] + 1) % 8
            return PS[psi[0]]

        def fm_proj(col_ap_fn, tc):
            p = next_ps()
            for kc in range(8):
                g.mm(p[:, :], col_ap_fn(kc), hT[:, kc, tc * 512:(tc + 1) * 512], kc == 0, kc == 7,
                     [w_in_k[kc]] + hT_t[tc * 4:(tc + 1) * 4], [p])
            return p

        for tc in range(4):
            tsl = slice(tc * 512, (tc + 1) * 512)
            for blk in range(2):
                c0 = blk * 128
                p_hc = fm_proj(lambda kc, c0=c0: w_in_sb[:, kc, c0:c0 + 128], tc)
                g.cp(hc_sb[:], p_hc[:, :], [p_hc], [hc_sb], eng="act")
                p_gc = fm_proj(lambda kc, c0=c0: w_in_sb[:, kc, 512 + c0:512 + c0 + 128], tc)
                g.tt(uT[:, blk, 2:514], p_gc[:, :], hc_sb[:], ALU.mult, [p_gc, hc_sb], [uT])
                g.ts(acc[:], uT[:, blk, 2:514], conv_wT[:, blk, 2:3], None, ALU.mult, None, [uT, conv_wT], [acc])
                g.stt(acc[:], uT[:, blk, 1:513], conv_wT[:, blk, 1:2], acc[:], ALU.mult, ALU.add, [uT, conv_wT, acc], [acc])
                g.stt(acc[:], uT[:, blk, 0:512], conv_wT[:, blk, 0:1], acc[:], ALU.mult, ALU.add, [uT, conv_wT, acc], [acc])
                p_gb = fm_proj(lambda kc, c0=c0: w_in_sb[:, kc, 256 + c0:256 + c0 + 128], tc)
                g.tt(ycf[:], p_gb[:, :], acc[:], ALU.mult, [p_gb, acc], [ycf])
                g.act(ysq[:, blk, :], ycf[:], AF.Square, [ycf], [ysq])
                g.act(ycT[:, blk, tsl], ycf[:], AF.Copy, [ycf, g_convT], [ycT], scale=g_convT[:, blk:blk + 1])
                if tc == 3:
                    g.ld("sp", O["conv_p"][:, blk, :], uT[:, blk, 512:514], [uT], [], is_output=True)
                else:
                    g.cp(uT[:, blk, 0:2], uT[:, blk, 512:514], [uT], [uT])
            for t4 in range(4):
                tt = tc * 4 + t4
                p = next_ps()
                for blk in range(2):
                    g.mm(p[:, 0:1], ysq[:, blk, t4 * 128:(t4 + 1) * 128], ones_f[:, 0:1], blk == 0, blk == 1, [ysq, ones_f], [p])
                g.ts(rc[:, tt:tt + 1], p[:, 0:1], 1.0 / 256.0, EPS, ALU.mult, ALU.add, [p], [rc])
            for r in range(6):
                p = fm_proj(lambda kc, r=r: w_in_sb[:, kc, 768 + r * 128:768 + (r + 1) * 128], tc)
                g.cp(qT[:, r, tsl], p[:, :], [p], [qT], eng=("act" if r % 2 else "dve"))
            for e in range(2):
                p = fm_proj(lambda kc, e=e: w_in_sb[:, kc, 1536 + e * 128:1536 + (e + 1) * 128], tc)
                g.cp(kcT[:, e, tsl], p[:, :], [p], [kcT], eng=("act" if e else "dve"))
            p = fm_proj(lambda kc: w_in_sb[:, kc, 1792:1920], tc)
            g.cp(ksT[:, tsl], p[:, :], [p], [ksT], eng="act")
            p = fm_proj(lambda kc: w_in_sb[:, kc, 2048:2176], tc)
            g.cp(kwT[:, tsl], p[:, :], [p], [kwT], eng="dve")
        g.rsqrt(rc[:], [rc])

        kvraw = [g.sb("kvraw%d" % i, [128, 768], F32, stack=stB) for i in range(1)]
        for tt in range(NT):
            pa = next_ps()
            pbk = next_ps()
            for kc in range(8):
                g.mm(pa[:, :], hT[:, kc, tt * 128:(tt + 1) * 128], w_in_sb[:, kc, 1536:2048], kc == 0, kc == 7,
                     [hT_t[tt], w_in_k[kc]], [pa])
            for kc in range(8):
                g.mm(pbk[:, 0:292], hT[:, kc, tt * 128:(tt + 1) * 128], w_in_sb[:, kc, 2048:2340], kc == 0, kc == 7,
                     [hT_t[tt], w_in_k[kc]], [pbk])
            kv = kvraw[0]
            g.cp(kv[:, 0:512], pa[:, :], [pa], [kv], eng="dve")
            g.cp(kv[:, 512:768], pbk[:, 0:256], [pbk], [kv], eng="act")
            g.act(sig[:, tt, :], pbk[:, 256:292], AF.Sigmoid, [pbk], [sig])
            g.cp(v_aug[:, tt, 0, :, 0:64], kv[:, 384:512].rearrange("p (g d) -> p g d", g=2), [kv], [v_aug], eng="dve")
            g.cp(v_aug[:, tt, 1, :, 0:64], kv[:, 640:768].rearrange("p (g d) -> p g d", g=2), [kv], [v_aug], eng="dve")
            g.ld("sp", O["cmp_p"][tt * 128:(tt + 1) * 128, :], kv[:, 0:256], [kv], [], is_output=True)
            g.ld("sp", O["slc_p"][tt * 128:(tt + 1) * 128, :], kv[:, 256:512], [kv], [], is_output=True)
            if tt >= 12:
                g.ld("sp", O["win_p"][(tt - 12) * 128:(tt - 11) * 128, :], kv[:, 512:768], [kv], [], is_output=True)

        for cb in range(5):
            c0 = cb * 512
            c1 = min(PROJ, c0 + 512)
            p = next_ps()
            for kc in range(8):
                g.mm(p[0:32, 0:c1 - c0], hTs[:, kc, :], w_in_sb[:, kc, c0:c1], kc == 0, kc == 7, [hTs, w_in_k[kc]], [p])
            g.cp(Ps[:, c0:c1], p[0:32, 0:c1 - c0], [p], [Ps], eng=("act" if cb % 2 else "dve"))
        g.ld("sp", O["cmp_s"], Ps[:, 1536:1792], [Ps], [], is_output=True)
        g.ld("sp", O["slc_s"], Ps[:, 1792:2048], [Ps], [], is_output=True)
        for b in range(4):
            g.ld("sp", O["win_s"][b, 504:512, :], Ps[b * 8:(b + 1) * 8, 2048:2304], [Ps], [], is_output=True)
            g.ld("sp", O["win_s"][b, 0:504, :], I["win"][b, 8:512, :], [], [], is_output=True, track=odram)
        us = g.sb("us", [128, 2, 4, 10], F32, stack=stB)
        g.ld("sp", us[:, :, :, 0:2], I["sconvT"], [], [us])
        ycs = g.sb("ycs", [128, 2, 32], F32, stack=stB)
        hcs = g.sb("hcs", [128, 32], F32, stack=stB)
        accs = g.sb("accs", [128, 32], F32, stack=stB)
        for blk in range(2):
            c0 = blk * 128

            def fm_s(cofs):
                p = next_ps()
                for kc in range(8):
                    g.mm(p[:, 0:32], w_in_sb[:, kc, cofs:cofs + 128], hTs[:, kc, :], kc == 0, kc == 7, [w_in_k[kc], hTs], [p])
                return p
            p_hc = fm_s(c0)
            g.cp(hcs[:], p_hc[:, 0:32], [p_hc], [hcs], eng="act")
            p_gc = fm_s(512 + c0)
            g.tt(us[:, blk, :, 2:10], p_gc[:, 0:32].rearrange("p (b t) -> p b t", b=4),
                 hcs[:].rearrange("p (b t) -> p b t", b=4), ALU.mult, [p_gc, hcs], [us])
            a3 = accs[:].rearrange("p (b t) -> p b t", b=4)
            g.ts(a3, us[:, blk, :, 2:10], conv_wT[:, blk, 2:3], None, ALU.mult, None, [us, conv_wT], [accs])
            g.stt(a3, us[:, blk, :, 1:9], conv_wT[:, blk, 1:2], a3, ALU.mult, ALU.add, [us, conv_wT, accs], [accs])
            g.stt(a3, us[:, blk, :, 0:8], conv_wT[:, blk, 0:1], a3, ALU.mult, ALU.add, [us, conv_wT, accs], [accs])
            p_gb = fm_s(256 + c0)
            g.tt(ycs[:, blk, :], p_gb[:, 0:32], accs[:], ALU.mult, [p_gb, accs], [ycs])
            g.act(ysqs[:, blk, :], ycs[:, blk, :], AF.Square, [ycs], [ysqs])
            g.act(ycTs[:, blk, :], ycs[:, blk, :], AF.Copy, [ycs, g_convT], [ycTs], scale=g_convT[:, blk:blk + 1])
            g.ld("sp", O["conv_s"][:, blk, :, :], us[:, blk, :, 8:10], [us], [], is_output=True)

        def fm_s2(cofs):
            p = next_ps()
            for kc in range(8):
                g.mm(p[:, 0:32], w_in_sb[:, kc, cofs:cofs + 128], hTs[:, kc, :], kc == 0, kc == 7, [w_in_k[kc], hTs], [p])
            return p
        for r in range(6):
            p = fm_s2(768 + r * 128)
            g.cp(qTs[:, :, r, :], p[:, 0:32].rearrange("p (b t) -> p b t", b=4), [p], [qTs])
        p = fm_s2(1792)
        g.cp(ksTs[:], p[:, 0:32], [p], [ksTs])
        p = fm_s2(2048)
        g.cp(kwTs[:], p[:, 0:32], [p], [kwTs])
        g.act(sigS[:], Ps[:, 2304:2340], AF.Sigmoid, [Ps], [sigS])
        g.cp(Pv[:, 0, :], Ps[:, 1920:2048], [Ps], [Pv])
        g.cp(Pv[:, 1, :], Ps[:, 2176:2304], [Ps], [Pv])
        g.memset(vnew[:], 1.0, [vnew])
        for b in range(4):
            for br in range(2):
                g.ld("sp", vnew[0:8, b, br, :, 0:64], Pv[b * 8:(b + 1) * 8, br, :].rearrange("p (g d) -> p g d", g=2), [Pv], [vnew])
            for r in range(6):
                for gg in range(2):
                    g.ld("sp", gq[r * 8:(r + 1) * 8, b, gg, :], sigS[b * 8:(b + 1) * 8, gg * 6 + r:36:12], [sigS], [gq], nc_ok=True)
        p = next_ps()
        for blk in range(2):
            g.mm(p[0:32, 0:1], ysqs[:, blk, :], ones_f[:, 0:1], blk == 0, blk == 1, [ysqs, ones_f], [p])
        g.ts(rc_s[:], p[0:32, 0:1], 1.0 / 256.0, EPS, ALU.mult, ALU.add, [p], [rc_s])
        g.rsqrt(rc_s[:], [rc_s])
        g.barrier()
        stB.close()

        def load_cmp_w(stk, tag):
            w1rep = g.sb("w1rep" + tag, [128, 2, 32, 128], BF16, stack=stk)
            g.ld("pool", w1rep[:].rearrange("p e j h -> p (e j h)"), I["w1rep"], [], [w1rep])
            peT = g.sb("peT" + tag, [128, 2, 32], BF16, stack=stk)
            g.ld("pool", peT[:].rearrange("p e j -> p (e j)"), I["peT"], [], [peT])
            w2k = g.sb("w2k" + tag, [128, 128], BF16, stack=stk)
            g.ld("pool", w2k[:], I["w2k"], [], [w2k])
            w2v = g.sb("w2v" + tag, [128, 64], BF16, stack=stk)
            g.ld("pool", w2v[:], I["w2v"], [], [w2v])
            biasT = g.sb("biasT" + tag, [128, 2], F32, stack=stk)
            pbias = next_ps()
            for e in range(2):
                for j in range(32):
                    g.mm(pbias[:, e:e + 1], w1rep[0:64, e, j, :], peT[0:64, e, j:j + 1], j == 0, j == 31, [w1rep, peT], [pbias])
            g.tt(biasT[:], pbias[:, 0:2], b1T[:], ALU.add, [pbias, b1T], [biasT])
            return w1rep, w2k, w2v, biasT

        def gelu_tanh(hx_ap, hy_ap, out_ap, hx_t, hy_t, out_t):
            g.tt(hy_ap, hx_ap, hx_ap, ALU.mult, [hx_t], [hy_t])
            g.ts(hy_ap, hy_ap, 0.044715, 1.0, ALU.mult, ALU.add, [hy_t], [hy_t])
            g.tt(hy_ap, hy_ap, hx_ap, ALU.mult, [hy_t, hx_t], [hy_t])
            g.act(hy_ap, hy_ap, AF.Tanh, [hy_t], [hy_t], scale=0.7978845608028654)
            g.ts(hy_ap, hy_ap, 0.5, 0.5, ALU.mult, ALU.add, [hy_t], [hy_t])
            g.tt(out_ap, hy_ap, hx_ap, ALU.mult, [hy_t, hx_t], [out_t])

        def layernorm(src, gi, dst, stats, mv, lnbc, n=128):
            src_ap, src_t = src
            dst_ap, dst_t = dst
            for c2 in range(2):
                g.emit("dve", lambda e, c2=c2: e.bn_stats(out=stats[0:n, c2, :], in_=src_ap[:, c2 * 512:(c2 + 1) * 512]),
                       reads=[src_t], writes=[stats])
            g.emit("dve", lambda e: e.bn_aggr(out=mv[0:n, :], in_=stats[0:n, :, :]), reads=[stats], writes=[mv])
            g.ts(mv[0:n, 1:2], mv[0:n, 1:2], EPS, None, ALU.add, None, [mv], [mv])
            g.rsqrt(mv[0:n, 1:2], [mv])
            g.ts(dst_ap, src_ap, mv[0:n, 0:1], mv[0:n, 1:2], ALU.subtract, ALU.mult, [src_t, mv], [dst_t])
            g.tt(dst_ap, dst_ap, lnbc[0:n, gi, :], ALU.mult, [dst_t, lnbc], [dst_t])
            g.tt(dst_ap, dst_ap, lnbc[0:n, gi + 1, :], ALU.add, [dst_t, lnbc], [dst_t])

        w_o_v = I["w_o"].rearrange("(k p) n -> p k n", p=128)
        w_g_v = I["w_g"].rearrange("(k p) n -> p k n", p=128)
        w_u_v = I["w_u"].rearrange("(k p) n -> p k n", p=128)

        stS2 = ExitStack()
        w1rep_s, w2k_s, w2v_s, biasT_s = load_cmp_w(stS2, "_s")
        ident_bf = g.sb("ident_bf", [128, 128], BF16, stack=stS2)
        g.ld("pool", ident_bf[:], I["ident"], [], [ident_bf])
        ones_bf = g.sb("ones_bf", [128, 1], BF16, stack=stS2)
        g.memset(ones_bf[:], 1.0, [ones_bf])
        force_s = g.sb("force_s", [8, 257], F32, stack=stS2)
        g.ld("sp", force_s[:], I["force_s"], [], [force_s])
        gsum = g.sb("gsum", [48, 8], F32, stack=stS2)
        g.ld("sp", gsum[:], I["gsum"], [], [gsum])
        gtb = g.sb("gtb", [8, 48], BF16, stack=stS2)
        g.ld("pool", gtb[:], I["gt"], [], [gtb])
        cnew = g.sb("cnew", [8, 48], BF16, stack=stS2)
        g.ld("pool", cnew[:], I["causal_new"], [], [cnew])
        mw0 = g.sb("mw0", [128, 48], BF16, stack=stS2)
        g.ld("pool", mw0[:], I["mw0"], [], [mw0])
        ptT_sb = g.sb("ptT_sb", [128, 4], I32, stack=stS2)
        g.ld("sp", ptT_sb[:], I["ptT"], [], [ptT_sb])
        idx8 = g.sb("idx8", [128, 4, 16], I32, stack=stS2)
        for b in range(4):
            for k in range(16):
                g.ts(idx8[:, b, k:k + 1], ptT_sb[:, b:b + 1], 16, k, ALU.mult, ALU.add, [ptT_sb], [idx8])
        vaug_s = g.sb("vaug_s", [128, 8, 2, 322], BF16, stack=stS2)
        g.memset(vaug_s[:], 1.0, [vaug_s])
        for gg in range(2):
            g.ld("pool", vaug_s[:, :, gg, 65:322], I["as_"], [], [vaug_s])
        Xg = [g.sb("Xg%d" % i, [128, 8, 256], BF16, stack=stS2) for i in range(2)]
        XT = g.sb("XT", [128, 2, 16, 128], BF16, stack=stS2)
        TB = g.sb("TB", [128, 2, 4, 1024], F32, stack=stS2)
        hidTs = g.sb("hidTs", [128, 4, 1024], BF16, stack=stS2)
        g.memset(hidTs[:], 0.0, [hidTs])
        kcmpTs = g.sb("kcmpTs", [128, 1024], BF16, stack=stS2)
        ecs = g.sb("ecs", [128, 8, 48], BF16, stack=stS2)
        pss = g.sb("pss", [128, 8, 48], BF16, stack=stS2)
        rs_s = g.sb("rs_s", [48, 3], F32, stack=stS2)
        scn = g.sb("scn", [48, 257], F32, stack=stS2)
        sc2s = g.sb("sc2s", [8, 257], F32, stack=stS2)
        scw = g.sb("scw", [8, 257], F32, stack=stS2)
        m8s = g.sb("m8s", [8, 8], F32, stack=stS2)
        m8t = g.sb("m8t", [8, 8], F32, stack=stS2)
        sel_s = g.sb("sel_s", [8, 258], BF16, stack=stS2)
        maskTs = g.sb("maskTs", [128, 2, 2, 48], BF16, stack=stS2)
        oc_sb = g.sb("oc_sb", [48, 2, 65], F32, stack=stS2)
        pnew = g.sb("pnew", [8, 48], BF16, stack=stS2)
        Wb = g.sb("Wb", [128, 4, 256], BF16, stack=stS2)
        vx = g.sb("vx", [128, 16, 2, 65], BF16, stack=stS2)
        g.memset(vx[:], 1.0, [vx])
        Wv = g.sb("Wv", [128, 4, 2, 65], BF16, stack=stS2)
        g.memset(Wv[:], 1.0, [Wv])
        KTw = g.sb("KTw", [128, 4, 128], BF16, stack=stS2)
        cfs = g.sb("cfs", [48, 3], F32, stack=stS2)
        og_sb = g.sb("og_sb", [48, 2, 64], F32, stack=stS2)
        psb = [PS[0].h.bitcast(BF16), PS[1].h.bitcast(BF16)]
        tcount = [0]

        def transposes8(srcs, src_t, dst_ap, dst_t):
            i = tcount[0] % 2
            tcount[0] += 1
            for k, sap in enumerate(srcs):
                g.tr(psb[i][:, k * 128:(k + 1) * 128], sap, ident_bf[:], [src_t, ident_bf], [PS[i]])
            n = len(srcs)
            g.cp(dst_ap, psb[i][:, 0:n * 128].rearrange("p (k q) -> p k q", q=128), [PS[i]], [dst_t],
                 eng=("act" if tcount[0] % 2 else "dve"))

        def gather(pool_name, b, k, dst):
            g.dma("pool", lambda e: e.indirect_dma_start(
                out=dst[:].rearrange("p r c -> p (r c)"), out_offset=None, in_=I[pool_name],
                in_offset=bass.IndirectOffsetOnAxis(ap=idx8[:, b, k:k + 1], axis=0)), reads=[idx8], writes=[dst])

        dbt_all = g.sb("dbt_all", [48, 3, 130], F32, stack=stS2) if DBG else None
        gcount = [0]
        for b in range(4 if STAGE >= 2 else 0):
            qb = [qTs[0:64, b, :, :], qTs[64:128, b, :, :]]
            for cl in range(8):
                for jh in range(2):
                    gather("cmp_pool", b, cl * 2 + jh, Xg[jh])
                    for e in range(2):
                        transposes8([Xg[jh][:, j8, e * 128:(e + 1) * 128] for j8 in range(8)], Xg[jh],
                                    XT[:, e, jh * 8:(jh + 1) * 8, :], XT)
                if SUB < 2:
                    continue
                for tb in range(2):
                    for gg in range(2):
                        pb = PS[2 + gg] if tb == 0 else PS[4 + gg]
                        gs = slice(gg * 64, (gg + 1) * 64)
                        for e in range(2):
                            col = e * 128
                            for j in range(16):
                                g.mm(pb[:, col:col + 128], w1rep_s[gs, e, tb * 16 + j, :], XT[gs, e, j, :], j == 0, j == 15,
                                     [w1rep_s, XT], [pb])
                        g.cp(TB[:, tb, gg:4:2, cl:1024:8], pb[:, 0:256].rearrange("p (a q) -> p a q", q=128), [pb], [TB],
                             eng=("act" if (tb + gg) % 2 else "dve"))
            if SUB < 3:
                continue
            hx_ap = TB[:, 0, :, 0:1023]
            hy_ap = TB[:, 1, :, 0:1023]
            g.tt(hx_ap, TB[:, 0, :, 0:1023], TB[:, 1, :, 1:1024], ALU.add, [TB], [TB])
            for e in range(2):
                g.ts(TB[:, 0, e * 2:(e + 1) * 2, 0:1023], TB[:, 0, e * 2:(e + 1) * 2, 0:1023], biasT_s[:, e:e + 1], None, ALU.add, None,
                     [TB, biasT_s], [TB])
            gelu_tanh(hx_ap, hy_ap, hidTs[:, :, 0:1023], TB, TB, hidTs)
            if SUB < 4:
                continue
            for gg in range(2):
                gs = slice(gg * 64, (gg + 1) * 64)
                for hf in range(2):
                    po = PS[2 + hf]
                    g.mm(po[:, :], w2k_s[:], hidTs[:, gg, hf * 512:(hf + 1) * 512], True, True, [w2k_s, hidTs], [po])
                    g.cp(kcmpTs[gs, hf * 512:(hf + 1) * 512], po[gs, :], [po], [kcmpTs], eng="act")
                po = PS[4]
                for it in range(8):
                    g.mm(po[:, it * 64:(it + 1) * 64], hidTs[:, 2 + gg, it * 128:(it + 1) * 128], w2v_s[:], True, True,
                         [hidTs, w2v_s], [po])
                g.cp(vaug_s[:, :, gg, 0:64], po[:, :].rearrange("p (i d) -> p i d", d=64), [po], [vaug_s], eng="dve")
            if STAGE < 3:
                continue
            for gg in range(2):
                gs = slice(gg * 64, (gg + 1) * 64)
                pS = PS[2]
                for it in range(8):
                    g.mm(pS[:, it * 48:(it + 1) * 48], kcmpTs[gs, it * 128:(it + 1) * 128], qb[gg], True, True, [kcmpTs, qTs], [pS])
                g.act(ecs[:], pS[:, 0:384].rearrange("p (i q) -> p i q", q=48), AF.Exp, [pS], [ecs], scale=SCALE)
                pO = PS[3]
                for it in range(8):
                    n = 127 if it == 7 else 128
                    g.mm(pO[0:48, 0:322], ecs[0:n, it, :], vaug_s[0:n, it, gg, :], it == 0, it == 7, [ecs, vaug_s], [pO], signal=True)
                g.cp(oc_sb[:, gg, :], pO[0:48, 0:65], [pO], [oc_sb], eng="act")
                g.ts(rs_s[:, 0:1], pO[0:48, 64:65], 1e-30, None, ALU.max, None, [pO], [rs_s])
                g.emit("dve", lambda e: e.reciprocal(out=rs_s[:, 0:1], in_=rs_s[:, 0:1]), reads=[rs_s], writes=[rs_s])
                g.ts(scn[:], pO[0:48, 65:322], rs_s[:, 0:1], None, ALU.mult, None, [pO, rs_s], [scn])
                pI = PS[4]
                g.mm(pI[0:8, 0:257], gsum[:], scn[:], True, True, [gsum, scn], [pI])
                g.tt(sc2s[:], pI[0:8, 0:257], force_s[:], ALU.max, [pI, force_s], [sc2s])
                if DBG:
                    g.ld("sp", O["dbg_sc"][b, gg], sc2s[:], [sc2s], [], is_output=True)
                g.emit("dve", lambda e: e.max(out=m8s[:], in_=sc2s[:]), reads=[sc2s], writes=[m8s])
                g.emit("dve", lambda e: e.match_replace(out=scw[:], in_to_replace=m8s[:], in_values=sc2s[:], imm_value=-3e38),
                       reads=[m8s, sc2s], writes=[scw])
                g.emit("dve", lambda e: e.max(out=m8t[:], in_=scw[:]), reads=[scw], writes=[m8t])
                g.ts(sel_s[:, 0:257], sc2s[:], m8t[:, 7:8], None, ALU.is_ge, None, [sc2s, m8t], [sel_s])
                for par in range(2):
                    pM = PS[4]
                    g.mm(pM[:, 0:48], sel_s[:, par:256:2], gtb[:], True, True, [sel_s, gtb], [pM])
                    g.cp(maskTs[:, gg, par, :], pM[:, 0:48], [pM], [maskTs], eng="act")
            if STAGE < 4:
                continue
            pOs2 = [PS[4], PS[5]]
            pOw2 = [PS[6], PS[7]]
            for rg in range(8):
                for rh in range(2):
                    gather("slc_pool", b, rg * 2 + rh, Xg[rh])
                    transposes8([Xg[rh][:, r8, 0:128] for r8 in range(8)], Xg[rh], XT[:, 0, rh * 8:(rh + 1) * 8, :], XT)
                    g.cp(vx[:, rh * 8:(rh + 1) * 8, :, 0:64], Xg[rh][:, :, 128:256].rearrange("p r (g d) -> p r g d", g=2),
                         [Xg[rh]], [vx], eng="dve")
                par = rg // 4
                for gg in range(2):
                    gs = slice(gg * 64, (gg + 1) * 64)
                    for rh in range(2):
                        pS = PS[2 + rh]
                        for r8 in range(8):
                            g.mm(pS[:, r8 * 48:(r8 + 1) * 48], XT[gs, 0, rh * 8 + r8, :], qb[gg], True, True, [XT, qTs], [pS])
                        g.act(ecs[:], pS[:, 0:384].rearrange("p (i q) -> p i q", q=48), AF.Exp, [pS], [ecs], scale=SCALE)
                        g.tt(pss[:], ecs[:], maskTs[:, gg, par, :].rearrange("p (o q) -> p o q", o=1).to_broadcast([128, 8, 48]),
                             ALU.mult, [ecs, maskTs], [pss])
                        for r8 in range(8):
                            rr = rh * 8 + r8
                            first = (rg == 0 and rr == 0)
                            g.mm(pOs2[gg][0:48, 0:65], pss[:, r8, :], vx[:, rr, gg, :], first, False,
                                 [pss, vx], [pOs2[gg]], signal=True)
            for gg in range(2):
                gs = slice(gg * 64, (gg + 1) * 64)
                for br, (kTn, pOx) in enumerate(((ksTs, pOs2[gg]), (kwTs, pOw2[gg]))):
                    if br == 1:
                        continue
                    pS = PS[2]
                    g.mm(pS[0:8, 0:48], kTn[gs, b * 8:(b + 1) * 8], qb[gg], True, True, [kTn, qTs], [pS])
                    g.act(pnew[:], pS[0:8, 0:48], AF.Exp, [pS], [pnew], scale=SCALE)
                    g.tt(pnew[:], pnew[:], cnew[:], ALU.mult, [pnew, cnew], [pnew])
                    g.mm(pOx[0:48, 0:65], pnew[:], vnew[0:8, b, br, gg, :], False, True, [pnew, vnew], [pOx], signal=True)
            if STAGE < 5:
                continue
            g.ld("pool", Wb[:], I["win"][b].rearrange("(w p) c -> p w c", p=128), [], [Wb])
            transposes8([Wb[:, w, 0:128] for w in range(4)], Wb, KTw[:, :, :], KTw)
            g.cp(Wv[:, :, :, 0:64], Wb[:, :, 128:256].rearrange("p w (g d) -> p w g d", g=2), [Wb], [Wv], eng="dve")
            for gg in range(2):
                gs = slice(gg * 64, (gg + 1) * 64)
                pS = PS[2]
                for w in range(4):
                    g.mm(pS[:, w * 48:(w + 1) * 48], KTw[gs, w, :], qb[gg], True, True, [KTw, qTs], [pS])
                g.act(ecs[:, 0:4, :], pS[:, 0:192].rearrange("p (i q) -> p i q", q=48), AF.Exp, [pS], [ecs], scale=SCALE)
                g.tt(ecs[:, 0, :], ecs[:, 0, :], mw0[:], ALU.mult, [ecs, mw0], [ecs])
                for w in range(4):
                    g.mm(pOw2[gg][0:48, 0:65], ecs[:, w, :], Wv[:, w, gg, :], w == 0, False, [ecs, Wv], [pOw2[gg]], signal=True)
                pS = PS[3]
                g.mm(pS[0:8, 0:48], kwTs[gs, b * 8:(b + 1) * 8], qb[gg], True, True, [kwTs, qTs], [pS])
                g.act(pnew[:], pS[0:8, 0:48], AF.Exp, [pS], [pnew], scale=SCALE)
                g.tt(pnew[:], pnew[:], cnew[:], ALU.mult, [pnew, cnew], [pnew])
                g.mm(pOw2[gg][0:48, 0:65], pnew[:], vnew[0:8, b, 1, gg, :], False, True, [pnew, vnew], [pOw2[gg]], signal=True)
            if STAGE < 6:
                continue
            if DBG:
                dbt = dbt_all
                g.cp(dbt[:, 0, :], oc_sb[:].rearrange("p g c -> p (g c)"), [oc_sb], [dbt])
                for gg_ in range(2):
                    g.cp(dbt[:, 1, gg_ * 65:(gg_ + 1) * 65], pOs2[gg_][0:48, 0:65], [pOs2[gg_]], [dbt])
                    g.cp(dbt[:, 2, gg_ * 65:(gg_ + 1) * 65], pOw2[gg_][0:48, 0:65], [pOw2[gg_]], [dbt])
                g.ld("sp", O["dbg_oc"][b], dbt[:, 0, :], [dbt], [], is_output=True)
                g.ld("sp", O["dbg_os"][b], dbt[:, 1, :], [dbt], [], is_output=True)
                g.ld("sp", O["dbg_ow"][b], dbt[:, 2, :], [dbt], [], is_output=True)
            for gg in range(2):
                g.ts(rs_s[:, 0:1], oc_sb[:, gg, 64:65], 1e-30, None, ALU.max, None, [oc_sb], [rs_s])
                pOs = pOs2[gg]
                pOw = pOw2[gg]
                g.ts(rs_s[:, 1:2], pOs[0:48, 64:65], 1e-30, None, ALU.max, None, [pOs], [rs_s])
                g.ts(rs_s[:, 2:3], pOw[0:48, 64:65], 1e-30, None, ALU.max, None, [pOw], [rs_s])
                g.emit("dve", lambda e: e.reciprocal(out=rs_s[:], in_=rs_s[:]), reads=[rs_s], writes=[rs_s])
                g.tt(cfs[:], rs_s[:], gq[:, b, gg, :], ALU.mult, [rs_s, gq], [cfs])
                g.ts(og_sb[:, gg, :], oc_sb[:, gg, 0:64], cfs[:, 0:1], None, ALU.mult, None, [oc_sb, cfs], [og_sb])
                g.stt(og_sb[:, gg, :], pOs[0:48, 0:64], cfs[:, 1:2], og_sb[:, gg, :], ALU.mult, ALU.add,
                      [pOs, cfs, og_sb], [og_sb])
                g.stt(og_sb[:, gg, :], pOw[0:48, 0:64], cfs[:, 2:3], og_sb[:, gg, :], ALU.mult, ALU.add,
                      [pOw, cfs, og_sb], [og_sb])
                for r in range(6):
                    g.ld("sp", o_tok[b * 8:(b + 1) * 8, gg * 384 + r * 64:gg * 384 + (r + 1) * 64], og_sb[r * 8:(r + 1) * 8, gg, :],
                         [og_sb], [o_tok])

        if DBG:
            g.ld("sp", O["dbg_o"], o_tok[:], [o_tok], [], is_output=True)
        if STAGE >= 7:
            g.barrier()
            stS2.close()
            stS2 = ExitStack()
            w_o_s = g.sb("w_o_s", [128, 8, D], BF16, stack=stS2)
            for kc in range(8):
                g.ld("pool", w_o_s[:, kc, :], w_o_v[:, kc, :], [], [w_o_s])
            lnbc_s = g.sb("lnbc_s", [32, 4, D], F32, stack=stS2)
            for i, nm in enumerate(("ln1_g", "ln1_b", "ln2_g", "ln2_b")):
                g.ld("sp", lnbc_s[:, i, :], I[nm].rearrange("(o d) -> o d", o=1).to_broadcast([32, D]), [], [lnbc_s])
            stats_s = g.sb("stats_s", [32, 2, 6], F32, stack=stS2)
            mv_s = g.sb("mv_s", [32, 2], F32, stack=stS2)
            osq_s = g.sb("osq_s", [32, 768], F32, stack=stS2)
            rn_s = g.sb("rn_s", [32, 1], F32, stack=stS2)
            oTs = g.sb("oTs", [128, 6, 32], BF16, stack=stS2)
            t1s = g.sb("t1s", [32, D], F32, stack=stS2)
            xs2 = g.sb("xs2", [32, D], F32, stack=stS2)
            x1s = g.sb("x1s", [32, D], F32, stack=stS2)
            h2Ts = g.sb("h2Ts", [128, 8, 32], BF16, stack=stS2)
            aTs = g.sb("aTs", [128, NFF, 32], BF16, stack=stS2)
            sgs = g.sb("sgs", [128, 32], F32, stack=stS2)
            wgs = [g.sb("wgs%d" % i, [128, 8, 128], BF16, stack=stS2) for i in range(2)]
            wus = [g.sb("wus%d" % i, [128, 8, 128], BF16, stack=stS2) for i in range(2)]
            wds = [g.sb("wds%d" % i, [128, D], BF16, stack=stS2) for i in range(2)]
            g.tt(osq_s[:], o_tok[:], o_tok[:], ALU.mult, [o_tok], [osq_s])
            g.emit("dve", lambda e: e.reduce_sum(out=rn_s[:], in_=osq_s[:], axis=AX.X), reads=[osq_s], writes=[rn_s])
            g.ts(rn_s[:], rn_s[:], 1.0 / 768.0, EPS, ALU.mult, ALU.add, [rn_s], [rn_s])
            g.rsqrt(rn_s[:], [rn_s])
            pb = PS[7]
            for blk in range(6):
                g.tr(pb[:, blk * 32:(blk + 1) * 32], o_tok[:, blk * 128:(blk + 1) * 128], ident[0:32, 0:32], [o_tok, ident], [pb])
            for blk in range(6):
                g.act(oTs[:, blk, :], pb[:, blk * 32:(blk + 1) * 32], AF.Copy, [pb, g_nsaT], [oTs], scale=g_nsaT[:, blk:blk + 1])
            for hf in range(2):
                cs_ = slice(hf * 512, (hf + 1) * 512)
                pc = PS[0 + hf]
                pn = PS[2 + hf]
                for blk in range(2):
                    g.mm(pc[0:32, :], ycTs[:, blk, :], w_o_s[:, blk, cs_], blk == 0, blk == 1, [ycTs, w_o_s], [pc])
                for blk in range(6):
                    g.mm(pn[0:32, :], oTs[:, blk, :], w_o_s[:, 2 + blk, cs_], blk == 0, blk == 5, [oTs, w_o_s], [pn])
                g.ts(t1s[:, cs_], pc[0:32, :], rc_s[:, 0:1], None, ALU.mult, None, [pc, rc_s], [t1s])
                g.stt(t1s[:, cs_], pn[0:32, :], rn_s[:, 0:1], t1s[:, cs_], ALU.mult, ALU.add, [pn, rn_s, t1s], [t1s])
            g.tt(t1s[:], t1s[:], gate_s[:, 0, :], ALU.mult, [t1s, gate_s], [t1s])
            g.ld("sp", xs2[:], I["xs"], [], [xs2])
            g.stt(t1s[:], xs2[:], ALPHA, t1s[:], ALU.mult, ALU.add, [xs2, t1s], [t1s])
            layernorm((t1s[:], t1s), 0, (x1s[:], x1s), stats_s, mv_s, lnbc_s, n=32)
            for hf in range(2):
                pb = PS[6 + hf]
                for k4 in range(4):
                    kc = hf * 4 + k4
                    g.tr(pb[:, k4 * 32:(k4 + 1) * 32], x1s[:, kc * 128:(kc + 1) * 128], ident[0:32, 0:32], [x1s, ident], [pb])
                for k4 in range(4):
                    kc = hf * 4 + k4
                    for b in range(4):
                        g.act(h2Ts[:, kc, b * 8:(b + 1) * 8], pb[:, k4 * 32 + b * 8:k4 * 32 + (b + 1) * 8], AF.Identity,
                              [pb, ops2, modT], [h2Ts], bias=modT[:, 24 + kc, 1 + b:2 + b], scale=ops2[:, kc, 1 + b:2 + b])
            for ffc in range(NFF):
                wg_t = wgs[ffc % 2]
                wu_t = wus[ffc % 2]
                fs = slice(ffc * 128, (ffc + 1) * 128)
                g.ld("pool", wg_t[:], w_g_v[:, :, fs], [], [wg_t])
                g.ld("pool", wu_t[:], w_u_v[:, :, fs], [], [wu_t])
                g.ld("sp", wscr["wg_bf"][ffc], wg_t[:].rearrange("p k n -> p (k n)"), [wg_t], [WSC["wg_bf"][ffc]])
                g.ld("sp", wscr["wu_bf"][ffc], wu_t[:].rearrange("p k n -> p (k n)"), [wu_t], [WSC["wu_bf"][ffc]])
                pG = PS[(2 * ffc) % 4]
                pU = PS[(2 * ffc + 1) % 4]
                for kc in range(8):
                    g.mm(pG[:, 0:32], wg_t[:, kc, :], h2Ts[:, kc, :], kc == 0, kc == 7, [wg_t, h2Ts], [pG])
                for kc in range(8):
                    g.mm(pU[:, 0:32], wu_t[:, kc, :], h2Ts[:, kc, :], kc == 0, kc == 7, [wu_t, h2Ts], [pU])
                g.act(sgs[:], pG[:, 0:32], AF.Silu, [pG], [sgs])
                g.tt(aTs[:, ffc, :], sgs[:], pU[:, 0:32], ALU.mult, [sgs, pU], [aTs])
            for ffc in range(NFF):
                wd_t = wds[ffc % 2]
                g.ld("pool", wd_t[:], I["w_d"][ffc * 128:(ffc + 1) * 128, :], [], [wd_t])
                g.ld("sp", wscr["wd_bf"][ffc], wd_t[:], [wd_t], [WSC["wd_bf"][ffc]])
                for hf in range(2):
                    g.mm(PS[4 + hf][0:32, :], aTs[:, ffc, :], wd_t[:, hf * 512:(hf + 1) * 512], ffc == 0, ffc == NFF - 1,
                         [aTs, wd_t], [PS[4 + hf]], signal=True)
            for hf in range(2):
                cs_ = slice(hf * 512, (hf + 1) * 512)
                g.tt(t1s[:, cs_], PS[4 + hf][0:32, :], gate_s[:, 1, cs_], ALU.mult, [PS[4 + hf], gate_s], [t1s])
            g.stt(t1s[:], x1s[:], ALPHA, t1s[:], ALU.mult, ALU.add, [x1s, t1s], [t1s])
            layernorm((t1s[:], t1s), 2, (xs2[:], xs2), stats_s, mv_s, lnbc_s, n=32)
            g.ld("sp", O["y_s"], xs2[:], [xs2], [], is_output=True)
        g.barrier()
        stS2.close()
        stS.close()


        if DO_PROMPT:
            kcmpT = g.sb("kcmpT", [128, 128], BF16, stack=stC)
            vcmp = g.sb("vcmp", [128, 2, 97], BF16, stack=stC)
            stC2 = ExitStack()
            w1rep, w2k, w2v, biasT = load_cmp_w(stC2, "_p")
            acmp_f = g.sb("acmp_f", [128, 32], F32, stack=stC2)
            g.ld("sp", acmp_f[:], I["acmp"], [], [acmp_f])
            g.memset(vcmp[:], 0.0, [vcmp])
            g.memset(vcmp[:, :, 64:65], 1.0, [vcmp])
            for gg in range(2):
                g.cp(vcmp[:, gg, 65:97], acmp_f[:], [acmp_f, vcmp], [vcmp])
            bot_sb = g.sb("bot_sb", [128, 128], F32, stack=stC2)
            hx = g.sb("hx", [128, 127], F32, stack=stC2)
            hy = g.sb("hy", [128, 127], F32, stack=stC2)
            hidT = g.sb("hidT", [128, 127], BF16, stack=stC2)
            for e in range(2):
                for gg in range(2):
                    gs = slice(gg * 64, (gg + 1) * 64)
                    ptop = next_ps()
                    pbot = next_ps()
                    for j in range(16):
                        g.mm(ptop[:, 0:128], w1rep[gs, e, j, :], kcT[gs, e, j:SEQ:16], j == 0, j == 15, [w1rep, kcT], [ptop])
                    for j in range(16):
                        g.mm(pbot[:, 0:128], w1rep[gs, e, 16 + j, :], kcT[gs, e, j:SEQ:16], j == 0, j == 15, [w1rep, kcT], [pbot])
                    g.cp(bot_sb[:], pbot[:, 0:128], [pbot], [bot_sb], eng="act")
                    g.tt(hx[:], ptop[:, 0:127], bot_sb[:, 1:128], ALU.add, [ptop, bot_sb], [hx])
                    g.ts(hx[:], hx[:], biasT[:, e:e + 1], None, ALU.add, None, [hx, biasT], [hx])
                    g.tt(hy[:], hx[:], hx[:], ALU.mult, [hx], [hy])
                    g.ts(hy[:], hy[:], 0.044715, 1.0, ALU.mult, ALU.add, [hy], [hy])
                    g.tt(hy[:], hy[:], hx[:], ALU.mult, [hy, hx], [hy])
                    g.act(hy[:], hy[:], AF.Tanh, [hy], [hy], scale=0.7978845608028654)
                    g.ts(hy[:], hy[:], 0.5, 0.5, ALU.mult, ALU.add, [hy], [hy])
                    g.tt(hidT[:], hy[:], hx[:], ALU.mult, [hy, hx], [hidT])
                    po = next_ps()
                    if e == 0:
                        g.mm(po[:, 0:127], w2k[:], hidT[:], True, True, [w2k, hidT], [po])
                        g.cp(kcmpT[gs, 0:127], po[gs, 0:127], [po], [kcmpT], eng="dve")
                    else:
                        g.mm(po[0:127, 0:64], hidT[:], w2v[:], True, True, [w2v, hidT], [po])
                        g.cp(vcmp[0:127, gg, 0:64], po[0:127, 0:64], [po], [vcmp], eng="dve")
            g.barrier()
            stC2.close()

            stD = ExitStack()
            w_o_sb = g.sb("w_o_sb", [128, 8, D], BF16, stack=stD)
            for kc in range(8):
                g.ld("pool", w_o_sb[:, kc, :], w_o_v[:, kc, :], [], [w_o_sb])
            lnbc = g.sb("lnbc", [128, 4, D], F32, stack=stD)
            for i, nm in enumerate(("ln1_g", "ln1_b", "ln2_g", "ln2_b")):
                g.ld("sp", lnbc[:, i, :], I[nm].rearrange("(o d) -> o d", o=1).to_broadcast([128, D]), [], [lnbc])
            ec = g.sb("ec", [128, 6, 128], BF16, stack=stD)
            es = [g.sb("es%d" % i, [128, 4, 128], BF16, stack=stD) for i in range(3)]
            pTt = [g.sb("pTt%d" % i, [128, 4, 128], BF16, stack=stD) for i in range(3)]
            maskT = g.sb("maskT", [128, NT, 128], BF16, stack=stD)
            rsc = g.sb("rsc", [128, 6], F32, stack=stD)
            score = g.sb("score", [128, 32], F32, stack=stD)
            sc2 = g.sb("sc2", [128, 32], F32, stack=stD)
            m8a = g.sb("m8a", [128, 8], F32, stack=stD)
            m8b = g.sb("m8b", [128, 8], F32, stack=stD)
            sel = g.sb("sel", [128, 32], F32, stack=stD)
            selT = g.sb("selT", [32, 128], BF16, stack=stD)
            rs2 = g.sb("rs2", [128, 2], F32, stack=stD)
            cf = g.sb("cf", [128, 3], F32, stack=stD)
            o_sb = g.sb("o_sb", [128, 768], F32, stack=stD)
            osq = g.sb("osq", [128, 768], F32, stack=stD)
            rn = g.sb("rn", [128, 1], F32, stack=stD)
            oT = g.sb("oT", [128, 6, 128], BF16, stack=stD)
            t1 = g.sb("t1", [128, D], F32, stack=stD)
            xt2 = g.sb("xt2", [128, D], F32, stack=stD)
            stats = g.sb("stats", [128, 2, 6], F32, stack=stD)
            mv = g.sb("mv", [128, 2], F32, stack=stD)
            x1g = g.sb("x1g", [128, 4, D], F32, stack=stD)
            h2T = g.sb("h2T", [128, 8, 512], BF16, stack=stD)
            aT = g.sb("aT", [128, NFF, 512], BF16, stack=stD)
            sg_sb = g.sb("sg_sb", [128, 512], F32, stack=stD)
            wgc = [g.sb("wgc%d" % i, [128, 8, 128], BF16, stack=stD) for i in range(2)]
            wuc = [g.sb("wuc%d" % i, [128, 8, 128], BF16, stack=stD) for i in range(2)]
            wdc = [g.sb("wdc%d" % i, [128, D], BF16, stack=stD) for i in range(2)]
            w_g_v = I["w_g"].rearrange("(k p) n -> p k n", p=128)
            w_u_v = I["w_u"].rearrange("(k p) n -> p k n", p=128)

            for grp in range(NT // 4):
                for t4 in range(4):
                    qt = grp * 4 + t4
                    qs = slice(qt * 128, (qt + 1) * 128)
                    for gg in range(2):
                        gs = slice(gg * 64, (gg + 1) * 64)
                        for r in range(6):
                            pS = PS[2 + (r // 4)]
                            g.mm(pS[0:127, (r % 4) * 128:(r % 4 + 1) * 128], kcmpT[gs, 0:127], qT[gs, r, qs], True, True,
                                 [kcmpT, qT], [pS])
                        g.act(ec[0:127, 0:4, :], PS[2][0:127, :].rearrange("p (r q) -> p r q", r=4), AF.Exp, [PS[2]], [ec], scale=SCALE)
                        g.act(ec[0:127, 4:6, :], PS[3][0:127, 0:256].rearrange("p (r q) -> p r q", r=2), AF.Exp, [PS[3]], [ec], scale=SCALE)
                        g.tt(ec[0:127, :, :], ec[0:127, :, :],
                             cmpmask[0:127, qs].rearrange("p (o q) -> p o q", o=1).to_broadcast([127, 6, 128]), ALU.mult,
                             [ec, cmpmask], [ec])
                        for r in range(6):
                            pO = PS[r // 3]
                            g.mm(pO[:, (r % 3) * 97:(r % 3 + 1) * 97], ec[0:127, r, :], vcmp[0:127, gg, :], True, True, [ec, vcmp], [pO])
                        for hh in range(2):
                            g.ts(rsc[:, hh * 3:(hh + 1) * 3], PS[hh][:, 0:291].rearrange("p (r c) -> p r c", c=97)[:, :, 64],
                                 1e-30, None, ALU.max, None, [PS[hh]], [rsc])
                        g.emit("dve", lambda e: e.reciprocal(out=rsc[:], in_=rsc[:]), reads=[rsc], writes=[rsc])
                        for r in range(6):
                            src = PS[r // 3][:, (r % 3) * 97 + 65:(r % 3) * 97 + 97]
                            if r == 0:
                                g.ts(score[:], src, rsc[:, 0:1], None, ALU.mult, None, [PS[0], rsc], [score])
                            else:
                                g.stt(score[:], src, rsc[:, r:r + 1], score[:], ALU.mult, ALU.add, [PS[r // 3], rsc, score], [score])
                        g.tt(sc2[:], score[:], force[:, qt, :], ALU.max, [score, force], [sc2])
                        g.tt(sc2[:], sc2[:], validneg[:, qt, :], ALU.add, [sc2, validneg], [sc2])
                        g.emit("dve", lambda e: e.max(out=m8a[:], in_=sc2[:]), reads=[sc2], writes=[m8a])
                        g.emit("dve", lambda e: e.match_replace(out=score[:], in_to_replace=m8a[:], in_values=sc2[:], imm_value=-3e38),
                               reads=[m8a, sc2], writes=[score])
                        g.emit("dve", lambda e: e.max(out=m8b[:], in_=score[:]), reads=[score], writes=[m8b])
                        g.ts(sel[:], sc2[:], m8b[:, 7:8], None, ALU.is_ge, None, [sc2, m8b], [sel])
                        g.tr(PS[4][0:32, 0:128], sel[:], ident[:], [sel, ident], [PS[4]])
                        g.cp(selT[:], PS[4][0:32, 0:128], [PS[4]], [selT], eng="act")
                        for k0 in range(0, qt + 1, 4):
                            nk = min(4, qt + 1 - k0)
                            for k4 in range(nk):
                                g.mm(PS[4][:, k4 * 128:(k4 + 1) * 128], lsel[:, k0 + k4, :], selT[:], True, True, [lsel, selT], [PS[4]])
                            g.cp(maskT[:, k0:k0 + nk, :], PS[4][:, 0:nk * 128].rearrange("p (k q) -> p k q", q=128), [PS[4]], [maskT],
                                 eng="act")
                        g.tt(maskT[:, qt, :], maskT[:, qt, :], tri_le[:], ALU.mult, [maskT, tri_le], [maskT])
                        rounds = []
                        wks = list(range(max(0, qt - 4), qt + 1))
                        for r in range(6):
                            for k0 in range(0, qt + 1, 4):
                                rounds.append(("slc", r, list(range(k0, min(k0 + 4, qt + 1))), False))
                            for c0 in range(0, len(wks), 4):
                                rounds.append(("win", r, wks[c0:c0 + 4], c0 + 4 >= len(wks)))
                        SBK = [PS[2], PS[3], PS[7]]
                        LOOK = 2

                        def emit_scores(i):
                            kind, r, kts, _ = rounds[i]
                            pS = SBK[i % 3]
                            kT = ksT if kind == "slc" else kwT
                            for k4, kt in enumerate(kts):
                                g.mm(pS[:, k4 * 128:(k4 + 1) * 128], kT[gs, kt * 128:(kt + 1) * 128], qT[gs, r, qs], True, True,
                                     [kT, qT], [pS])

                        def emit_rest(i):
                            kind, r, kts, last = rounds[i]
                            pS = SBK[i % 3]
                            e_t = es[i % 3]
                            p_t = pTt[i % 3]
                            nk = len(kts)
                            pOW = PS[5 + (r % 2)]
                            g.act(e_t[:, 0:nk, :], pS[:, 0:nk * 128].rearrange("p (k q) -> p k q", q=128), AF.Exp, [pS], [e_t], scale=SCALE)
                            if kind == "slc":
                                g.tt(p_t[:, 0:nk, :], e_t[:, 0:nk, :], maskT[:, kts[0]:kts[0] + nk, :], ALU.mult, [e_t, maskT], [p_t])
                                for k4, kt in enumerate(kts):
                                    g.mm(pOW[:, 0:65], p_t[:, k4, :], v_aug[:, kt, 0, gg, :], kt == 0, kt == qt, [p_t, v_aug], [pOW],
                                         signal=True)
                            else:
                                for k4, kt in enumerate(kts):
                                    if kt == qt:
                                        g.tt(e_t[:, k4, :], e_t[:, k4, :], tri_le[:], ALU.mult, [e_t, tri_le], [e_t])
                                    elif kt == qt - 4:
                                        g.tt(e_t[:, k4, :], e_t[:, k4, :], tri_gt[:], ALU.mult, [e_t, tri_gt], [e_t])
                                for k4, kt in enumerate(kts):
                                    g.mm(pOW[:, 65:130], e_t[:, k4, :], v_aug[:, kt, 1, gg, :], kt == wks[0], kt == qt, [e_t, v_aug], [pOW],
                                         signal=True)
                            if last:
                                g.ts(rs2[:], pOW[:, 0:130].rearrange("p (b c) -> p b c", c=65)[:, :, 64], 1e-30, None, ALU.max, None,
                                     [pOW], [rs2])
                                g.emit("dve", lambda e: e.reciprocal(out=rs2[:], in_=rs2[:]), reads=[rs2], writes=[rs2])
                                gi0 = gg * 6 + r
                                g.tt(cf[:, 0:1], rsc[:, r:r + 1], sig[:, qt, gi0:gi0 + 1], ALU.mult, [rsc, sig], [cf])
                                g.tt(cf[:, 1:2], rs2[:, 0:1], sig[:, qt, 12 + gi0:13 + gi0], ALU.mult, [rs2, sig], [cf])
                                g.tt(cf[:, 2:3], rs2[:, 1:2], sig[:, qt, 24 + gi0:25 + gi0], ALU.mult, [rs2, sig], [cf])
                                oc = o_sb[:, gg * 384 + r * 64:gg * 384 + (r + 1) * 64]
                                g.ts(oc, PS[r // 3][:, (r % 3) * 97:(r % 3) * 97 + 64], cf[:, 0:1], None, ALU.mult, None,
                                     [PS[r // 3], cf], [o_sb])
                                g.stt(oc, pOW[:, 0:64], cf[:, 1:2], oc, ALU.mult, ALU.add, [pOW, cf, o_sb], [o_sb])
                                g.stt(oc, pOW[:, 65:129], cf[:, 2:3], oc, ALU.mult, ALU.add, [pOW, cf, o_sb], [o_sb])

                        nr = len(rounds)
                        for i in range(min(LOOK, nr)):
                            emit_scores(i)
                        for i in range(nr):
                            if i + LOOK < nr:
                                emit_scores(i + LOOK)
                            emit_rest(i)
                    g.tt(osq[:], o_sb[:], o_sb[:], ALU.mult, [o_sb], [osq])
                    g.emit("dve", lambda e: e.reduce_sum(out=rn[:], in_=osq[:], axis=AX.X), reads=[osq], writes=[rn])
                    g.ts(rn[:], rn[:], 1.0 / 768.0, EPS, ALU.mult, ALU.add, [rn], [rn])
                    g.rsqrt(rn[:], [rn])
                    for hf in range(2):
                        pb = PS[6 + hf]
                        nb_ = 4 if hf == 0 else 2
                        for k4 in range(nb_):
                            blk = hf * 4 + k4
                            g.tr(pb[:, k4 * 128:(k4 + 1) * 128], o_sb[:, blk * 128:(blk + 1) * 128], ident[:], [o_sb, ident], [pb])
                        for k4 in range(nb_):
                            blk = hf * 4 + k4
                            g.act(oT[:, blk, :], pb[:, k4 * 128:(k4 + 1) * 128], AF.Copy, [pb, g_nsaT], [oT], scale=g_nsaT[:, blk:blk + 1])
                    for hf in range(2):
                        cs_ = slice(hf * 512, (hf + 1) * 512)
                        pc = PS[0 + hf]
                        pn = PS[2 + hf]
                        for blk in range(2):
                            g.mm(pc[:, :], ycT[:, blk, qs], w_o_sb[:, blk, cs_], blk == 0, blk == 1, [ycT, w_o_sb], [pc])
                        for blk in range(6):
                            g.mm(pn[:, :], oT[:, blk, :], w_o_sb[:, 2 + blk, cs_], blk == 0, blk == 5, [oT, w_o_sb], [pn])
                        g.ts(t1[:, cs_], pc[:, :], rc[:, qt:qt + 1], None, ALU.mult, None, [pc, rc], [t1])
                        g.stt(t1[:, cs_], pn[:, :], rn[:, 0:1], t1[:, cs_], ALU.mult, ALU.add, [pn, rn, t1], [t1])
                    g.tt(t1[:], t1[:], gate_p[:, 0, :], ALU.mult, [t1, gate_p], [t1])
                    g.ld("sp", xt2[:], I["xp"][qs, :], [], [xt2])
                    g.stt(t1[:], xt2[:], ALPHA, t1[:], ALU.mult, ALU.add, [xt2, t1], [t1])
                    layernorm((t1[:], t1), 0, (x1g[:, t4, :], x1g), stats, mv, lnbc)
                    for hf in range(2):
                        pb = PS[6 + hf]
                        for k4 in range(4):
                            kc = hf * 4 + k4
                            g.tr(pb[:, k4 * 128:(k4 + 1) * 128], x1g[:, t4, kc * 128:(kc + 1) * 128], ident[:], [x1g, ident], [pb])
                        for k4 in range(4):
                            kc = hf * 4 + k4
                            g.act(h2T[:, kc, t4 * 128:(t4 + 1) * 128], pb[:, k4 * 128:(k4 + 1) * 128], AF.Identity,
                                  [pb, ops2, modT], [h2T], bias=modT[:, 24 + kc, 0:1], scale=ops2[:, kc, 0:1])
                for ffc in range(NFF):
                    wg_t = wgc[ffc % 2]
                    wu_t = wuc[ffc % 2]
                    fs = slice(ffc * 128, (ffc + 1) * 128)
                    g.ld("sp", wg_t[:].rearrange("p k n -> p (k n)"), wscr["wg_bf"][ffc], [WSC["wg_bf"][ffc]], [wg_t])
                    g.ld("pool", wu_t[:].rearrange("p k n -> p (k n)"), wscr["wu_bf"][ffc], [WSC["wu_bf"][ffc]], [wu_t])
                    pG = PS[(2 * ffc) % 8]
                    pU = PS[(2 * ffc + 1) % 8]
                    for kc in range(8):
                        g.mm(pG[:, :], wg_t[:, kc, :], h2T[:, kc, :], kc == 0, kc == 7, [wg_t, h2T], [pG])
                    for kc in range(8):
                        g.mm(pU[:, :], wu_t[:, kc, :], h2T[:, kc, :], kc == 0, kc == 7, [wu_t, h2T], [pU])
                    g.act(sg_sb[:], pG[:, :], AF.Silu, [pG], [sg_sb])
                    g.tt(aT[:, ffc, :], sg_sb[:], pU[:, :], ALU.mult, [sg_sb, pU], [aT])
                for ffc in range(NFF):
                    wd_t = wdc[ffc % 2]
                    g.ld("sp" if ffc % 2 else "pool", wd_t[:], wscr["wd_bf"][ffc], [WSC["wd_bf"][ffc]], [wd_t])
                    for t4 in range(4):
                        for hf in range(2):
                            g.mm(PS[t4 * 2 + hf][:, :], aT[:, ffc, t4 * 128:(t4 + 1) * 128], wd_t[:, hf * 512:(hf + 1) * 512],
                                 ffc == 0, ffc == NFF - 1, [aT, wd_t], [PS[t4 * 2 + hf]], signal=True)
                for t4 in range(4):
                    qt = grp * 4 + t4
                    for hf in range(2):
                        cs_ = slice(hf * 512, (hf + 1) * 512)
                        g.tt(t1[:, cs_], PS[t4 * 2 + hf][:, :], gate_p[:, 1, cs_], ALU.mult, [PS[t4 * 2 + hf], gate_p], [t1])
                    g.stt(t1[:], x1g[:, t4, :], ALPHA, t1[:], ALU.mult, ALU.add, [x1g, t1], [t1])
                    layernorm((t1[:], t1), 2, (xt2[:], xt2), stats, mv, lnbc)
                    g.ld("sp", O["y_p"][qt * 128:(qt + 1) * 128, :], xt2[:], [xt2], [], is_output=True)
            g.barrier()
            stD.close()

        g.finish()
        stC.close()
    return nc


_NC_CACHE = {}


def _perm_w_in(w):
    w = w.copy()
    q = w[:, 768:1536].reshape(D, 2, 6, 64).transpose(0, 2, 1, 3).reshape(D, 768)
    w[:, 768:1536] = q
    return w


def kernel(x_prompt, x_sample, cache_cmp_kv, cache_slc_kv, cache_win_kv, state_conv, page_table,
           c_prompt, c_sample, w_ada, b_ada, w_in, conv_w, cmp_pe, cmp_w1, cmp_b1, cmp_w2,
           g_conv_out, g_nsa_out, w_o, ln1_g, ln1_b, ln2_g, ln2_b, w_ffn_gate, w_ffn_up, w_ffn_down,
           _cores=None):
    f = lambda a: np.ascontiguousarray(np.asarray(a, dtype=np.float32))
    if "nc" not in _NC_CACHE:
        _NC_CACHE["nc"] = build_nc()
    nc = _NC_CACHE["nc"]
    consts = host_constants()
    cores = list(range(N_CORES)) if _cores is None else _cores
    shared = {
        "cmp_pool": f(cache_cmp_kv).reshape(NPHYS * 16, 2048), "slc_pool": f(cache_slc_kv).reshape(NPHYS * 16, 2048),
        "w_ada": f(w_ada)[0], "b_ada": f(b_ada)[0], "w_in": _perm_w_in(f(w_in)[0]), "conv_wT": np.ascontiguousarray(f(conv_w)[0].reshape(3, 2, 128).transpose(2, 1, 0)),
        "b_adaT": np.ascontiguousarray(f(b_ada)[0].reshape(48, 128).T),
        "peT": np.ascontiguousarray(np.tile(f(cmp_pe)[0].transpose(2, 0, 1).reshape(64, 64), (2, 1))),
        "w1rep": np.ascontiguousarray(np.tile(f(cmp_w1)[0].transpose(2, 0, 1, 3).reshape(64, 2 * 32 * 128), (2, 1))), "b1T": np.ascontiguousarray(f(cmp_b1)[0].T), "w2k": np.ascontiguousarray(np.tile(f(cmp_w2)[0][0], (1, 2))), "w2v": f(cmp_w2)[0][1],
        "g_convT": np.ascontiguousarray(f(g_conv_out)[0].reshape(2, 128).T), "g_nsaT": np.ascontiguousarray(f(g_nsa_out)[0].reshape(6, 128).T), "w_o": f(w_o)[0],
        "ln1_g": f(ln1_g)[0], "ln1_b": f(ln1_b)[0], "ln2_g": f(ln2_g)[0], "ln2_b": f(ln2_b)[0],
        "w_g": f(w_ffn_gate)[0], "w_u": f(w_ffn_up)[0], "w_d": f(w_ffn_down)[0],
    }
    for k, v in consts.items():
        shared["k_" + k] = v
    xp = f(x_prompt)
    xs = f(x_sample)
    win = f(cache_win_kv)[0].reshape(32, 512, 256)
    sconv = f(state_conv)[0]
    pt = np.ascontiguousarray(np.asarray(page_table, dtype=np.int32))
    cp = f(c_prompt)
    cs = f(c_sample)
    in_maps = []
    for c in cores:
        m = dict(shared)
        m["xp"] = xp[c]
        m["xs"] = xs[4 * c:4 * c + 4].reshape(32, D)
        m["win"] = win[4 * c:4 * c + 4]
        m["sconvT"] = np.ascontiguousarray(sconv[4 * c:4 * c + 4].reshape(4, 2, 2, 128).transpose(3, 2, 0, 1))
        m["ptT"] = np.ascontiguousarray(pt[4 * c:4 * c + 4].T)
        m["cvecT"] = np.ascontiguousarray(np.concatenate([cp[c:c + 1], cs[4 * c:4 * c + 4]], axis=0).reshape(5, 8, 128).transpose(2, 1, 0))
        in_maps.append(m)
    res = run_bass_kernel_spmd(nc, in_maps, core_ids=list(range(len(cores))))
    R = res.results
    n = len(cores)
    y_p = np.stack([R[i]["y_p"] for i in range(n)])
    y_s = np.concatenate([R[i]["y_s"].reshape(4, 8, D) for i in range(n)])
    cmp_p = np.stack([R[i]["cmp_p"].reshape(SEQ, 2, 2, 64) for i in range(n)])[None]
    slc_p = np.stack([R[i]["slc_p"].reshape(SEQ, 2, 2, 64) for i in range(n)])[None]
    win_p = np.stack([R[i]["win_p"].reshape(512, 2, 2, 64) for i in range(n)])[None]
    conv_p = np.stack([R[i]["conv_p"].transpose(2, 1, 0).reshape(2, 256) for i in range(n)])[None]
    cmp_s = np.concatenate([R[i]["cmp_s"].reshape(4, 8, 2, 2, 64) for i in range(n)])[None]
    slc_s = np.concatenate([R[i]["slc_s"].reshape(4, 8, 2, 2, 64) for i in range(n)])[None]
    win_s = np.concatenate([R[i]["win_s"].reshape(4, 512, 2, 2, 64) for i in range(n)])[None]
    conv_s = np.concatenate([R[i]["conv_s"].transpose(2, 3, 1, 0).reshape(4, 2, 256) for i in range(n)])[None]
    return (y_p, y_s, cmp_p, slc_p, win_p, conv_p, cmp_s, slc_s, win_s, conv_s)
```

```python
import numpy as np
from contextlib import ExitStack
import concourse.bass as bass
import concourse.mybir as mybir
from concourse.bass_utils import run_bass_kernel_spmd

F32 = mybir.dt.float32
BF16 = mybir.dt.bfloat16
I32 = mybir.dt.int32
AF = mybir.ActivationFunctionType
ALU = mybir.AluOpType
AX = mybir.AxisListType

D = 1024
SEQ = 2048
NT = 16
NPAGE = 128
NPHYS = 5120
PROJ = 2340
DFF = 2816
NFF = 22
ALPHA = 2 ** 0.25
EPS = 1e-5
SCALE = 0.125
N_CORES = 8
STAGE = 9
DO_PROMPT = 1
SUB = 9
DBG = 0


class T:
    def __init__(self, handle, name, space):
        self.h = handle
        self.name = name
        self.space = space
        self.w = None
        self.r = {}
        self.dsem = None
        self.dcount = 0

    def __getitem__(self, idx):
        return self.h[idx]


class Gen:
    ENG = ("pe", "act", "dve", "pool", "sp")

    def __init__(self, nc, stack):
        self.nc = nc
        self.stack = stack
        self.ops = {e: [] for e in self.ENG}
        self.cnt = {e: 0 for e in self.ENG}
        self.seen = {e: {} for e in self.ENG}
        self.sems = {}
        self.cur = {}
        for e in self.ENG[:4]:
            self.sems[e] = self.stack.enter_context(self.nc.semaphore("c_" + e))
        self.out_marks = {}

    def sb(self, name, shape, dtype=F32, stack=None):
        h = (stack or self.stack).enter_context(self.nc.sbuf_tensor("s_" + name, list(shape), dtype))
        return T(h, name, "sbuf")

    def ps(self, name, shape, dtype=F32):
        h = self.stack.enter_context(self.nc.psum_tensor("p_" + name, list(shape), dtype))
        return T(h, name, "psum")

    def _wait(self, e, k, v):
        if e == "pe" and k == "pe":
            return
        if self.seen[e].get(k, 0) < v:
            self.seen[e][k] = v
            sem = self.sems[k]
            self.ops[e].append(lambda eng, sem=sem, v=v: eng.wait_ge(sem, v))

    def _deps(self, e, reads, writes):
        deps = {}
        for t in reads:
            if t.w is not None:
                k, v = t.w
                deps[k] = max(deps.get(k, 0), v)
        for t in writes:
            if t.w is not None:
                k, v = t.w
                deps[k] = max(deps.get(k, 0), v)
            for k, v in t.r.items():
                deps[k] = max(deps.get(k, 0), v)
        for k, v in deps.items():
            self._wait(e, k, v)

    def _mark(self, key, val, reads, writes):
        self.cur[key] = max(self.cur.get(key, 0), val)
        for t in reads:
            t.r[key] = max(t.r.get(key, 0), val)
        for t in writes:
            t.w = (key, val)
            t.r = {}

    def emit(self, e, fn, reads=(), writes=(), signal=True):
        self._deps(e, reads, writes)
        if signal:
            self.cnt[e] += 1
            sem = self.sems[e]
            self.ops[e].append(lambda eng, fn=fn, sem=sem: fn(eng).then_inc(sem, 1))
            val = self.cnt[e]
        else:
            self.ops[e].append(lambda eng, fn=fn: fn(eng))
            val = self.cnt[e] + 1
        self._mark(e, val, reads, writes)

    def dma(self, q, fn, reads=(), writes=(), track=None, is_output=False):
        if track is None:
            for t in list(writes) + list(reads):
                if t.space == "sbuf":
                    track = t
                    break
            if track is None:
                track = (list(writes) + list(reads))[0]
        if track.dsem is None:
            key = "d_" + track.name
            assert key not in self.sems, key
            self.sems[key] = self.stack.enter_context(self.nc.semaphore(key))
            track.dsem = key
        key = track.dsem
        self._deps(q, reads, writes)
        track.dcount += 1
        val = 16 * track.dcount
        sem = self.sems[key]
        self.ops[q].append(lambda eng, fn=fn, sem=sem: fn(eng).then_inc(sem, 16))
        self._mark(key, val, reads, writes)
        if is_output:
            self.out_marks[key] = max(self.out_marks.get(key, 0), val)

    def barrier(self):
        for e in self.ENG:
            for k, v in list(self.cur.items()):
                self._wait(e, k, v)

    def mm(self, out, lhsT, rhs, start, stop, reads, writes, signal=None):
        if signal is None:
            signal = stop
        self.emit("pe", lambda e: e.matmul(out, lhsT, rhs, start=start, stop=stop),
                  reads=reads, writes=writes, signal=signal)

    def tr(self, out, in_, ident, reads, writes):
        self.emit("pe", lambda e: e.transpose(out, in_, ident), reads=reads, writes=writes)

    def act(self, out, in_, func, reads, writes, bias=None, scale=None, accum_out=None):
        kw = {}
        if bias is not None:
            kw["bias"] = bias
        if scale is not None:
            kw["scale"] = scale
        if accum_out is not None:
            kw["accum_out"] = accum_out
        self.emit("act", lambda e: e.activation(out=out, in_=in_, func=func, **kw), reads=reads, writes=writes)

    def tt(self, out, in0, in1, op, reads, writes, eng="dve"):
        self.emit(eng, lambda e: e.tensor_tensor(out=out, in0=in0, in1=in1, op=op), reads=reads, writes=writes)

    def ts(self, out, in0, s1, s2, op0, op1, reads, writes, eng="dve"):
        if op1 is None:
            self.emit(eng, lambda e: e.tensor_scalar(out=out, in0=in0, scalar1=s1, scalar2=None, op0=op0),
                      reads=reads, writes=writes)
        else:
            self.emit(eng, lambda e: e.tensor_scalar(out=out, in0=in0, scalar1=s1, scalar2=s2, op0=op0, op1=op1),
                      reads=reads, writes=writes)

    def stt(self, out, in0, scalar, in1, op0, op1, reads, writes, eng="dve"):
        self.emit(eng, lambda e: e.scalar_tensor_tensor(out=out, in0=in0, scalar=scalar, in1=in1, op0=op0, op1=op1),
                  reads=reads, writes=writes)

    def cp(self, out, in_, reads, writes, eng="dve"):
        if eng == "act":
            self.act(out, in_, AF.Copy, reads, writes)
        else:
            self.emit(eng, lambda e: e.tensor_copy(out=out, in_=in_), reads=reads, writes=writes)

    def rsqrt(self, ap, tiles):
        self.act(ap, ap, AF.Sqrt, tiles, tiles)
        self.emit("dve", lambda e: e.reciprocal(out=ap, in_=ap), reads=tiles, writes=tiles)

    def memset(self, ap, val, writes, eng="dve"):
        self.emit(eng, lambda e: e.memset(ap, val), writes=writes)

    def ld(self, q, out, in_, reads, writes, nc_ok=False, is_output=False, track=None):
        if nc_ok:
            self.dma(q, lambda e: e.dma_start(out=out, in_=in_, allow_slow_non_contiguous=True),
                     reads=reads, writes=writes, is_output=is_output, track=track)
        else:
            self.dma(q, lambda e: e.dma_start(out=out, in_=in_), reads=reads, writes=writes,
                     is_output=is_output, track=track)

    def finish(self):
        for k, v in self.out_marks.items():
            self._wait("sp", k, v)
        for e in ("pe", "act", "dve"):
            self._wait("sp", e, self.cnt[e])
        with self.nc.Block() as block:
            @block.tensor
            def _(eng):
                for f in self.ops["pe"]:
                    f(eng)

            @block.scalar
            def _(eng):
                for f in self.ops["act"]:
                    f(eng)

            @block.vector
            def _(eng):
                for f in self.ops["dve"]:
                    f(eng)

            @block.gpsimd
            def _(eng):
                for f in self.ops["pool"]:
                    f(eng)

            @block.sync
            def _(eng):
                for f in self.ops["sp"]:
                    f(eng)


def host_constants():
    c = {}
    c["ident"] = np.eye(128, dtype=np.float32)
    s = np.arange(128)[:, None]
    q = np.arange(128)[None, :]
    c["tri_le"] = (s <= q).astype(np.float32)
    c["tri_gt"] = (s > q).astype(np.float32)
    i = np.arange(128)[:, None]
    qq = np.arange(SEQ)[None, :]
    cm = ((16 * i + 31) <= qq).astype(np.float32)
    cm[127, :] = 0.0
    c["cmpmask"] = cm
    qpos = (np.arange(NT)[None, :, None] * 128 + np.arange(128)[:, None, None])
    j = np.arange(32)[None, None, :]
    cur = qpos // 64
    forced = (j == 0) | (j == cur) | (j == cur - 1)
    c["force"] = np.where(forced, 1e9 * (1.0 + j / 64.0), 0.0).astype(np.float32)
    c["validneg"] = np.where(j * 64 <= qpos, 0.0, -1e30).astype(np.float32)
    L = np.zeros((32, NT, 128), np.float32)
    for kt in range(NT):
        L[2 * kt, kt, :64] = 1.0
        L[2 * kt + 1, kt, 64:] = 1.0
    c["lsel"] = L
    A = np.zeros((128, 32), np.float32)
    for jj in range(32):
        for ii in range(4 * jj - 1, 4 * jj + 4):
            if 0 <= ii <= 126:
                A[ii, jj] = 1.0
    c["acmp"] = A
    As = np.zeros((128, 8, 257), np.float32)
    for jj in range(257):
        for ii in range(4 * jj - 1, 4 * jj + 4):
            if 0 <= ii <= 1022:
                As[ii % 128, ii // 128, jj] = 1.0
    c["as_"] = As
    fs = np.zeros((8, 257), np.float32)
    for jj in (0, 255, 256):
        fs[:, jj] = 1e9 * (1.0 + jj / 1024.0)
    c["force_s"] = fs
    qi = np.arange(48)
    tq = qi % 8
    c["gsum"] = (tq[:, None] == np.arange(8)[None, :]).astype(np.float32)
    c["gt"] = (np.arange(8)[:, None] == tq[None, :]).astype(np.float32)
    c["causal_new"] = (np.arange(8)[:, None] <= tq[None, :]).astype(np.float32)
    c["mw0"] = (np.arange(128)[:, None] > tq[None, :]).astype(np.float32)
    return c


CONST_SHAPES = {"as_": [128, 8, 257], "force_s": [8, 257], "gsum": [48, 8], "gt": [8, 48], "causal_new": [8, 48],
                "mw0": [128, 48], "ident": [128, 128], "tri_le": [128, 128], "tri_gt": [128, 128], "cmpmask": [128, SEQ],
                "force": [128, NT, 32], "validneg": [128, NT, 32], "lsel": [32, NT, 128], "acmp": [128, 32]}

IN_SHAPES = {
    "xp": ([SEQ, D], F32), "xs": ([32, D], F32),
    "cmp_pool": ([NPHYS * 8, 4096], F32), "slc_pool": ([NPHYS * 8, 4096], F32),
    "win": ([4, 512, 256], F32), "sconvT": ([128, 2, 4, 2], F32), "ptT": ([128, 4], I32),
    "cvecT": ([128, 8, 5], F32),
    "w_ada": ([D, 6 * D], F32), "b_ada": ([6 * D], F32), "b_adaT": ([128, 48], F32), "w_in": ([D, PROJ], F32),
    "conv_wT": ([128, 2, 3], F32), "peT": ([128, 64], F32), "w1rep": ([128, 2 * 32 * 128], F32),
    "b1T": ([128, 2], F32), "w2k": ([128, 128], F32), "w2v": ([128, 64], F32), "g_convT": ([128, 2], F32), "g_nsaT": ([128, 6], F32),
    "w_o": ([D, D], F32), "ln1_g": ([D], F32), "ln1_b": ([D], F32), "ln2_g": ([D], F32), "ln2_b": ([D], F32),
    "w_g": ([D, DFF], F32), "w_u": ([D, DFF], F32), "w_d": ([DFF, D], F32),
}
OUT_SHAPES = {
    "y_p": [SEQ, D], "y_s": [32, D], "cmp_p": [SEQ, 256], "slc_p": [SEQ, 256], "win_p": [512, 256],
    "conv_p": [128, 2, 2], "cmp_s": [32, 256], "slc_s": [32, 256], "win_s": [4, 512, 256], "conv_s": [128, 2, 4, 2],
}


def build_nc(do_attn=True, do_post=True):
    nc = bass.Bass("TRN2", target_bir_lowering=False)
    I = {}
    for k, (shp, dt) in IN_SHAPES.items():
        I[k] = nc.dram_tensor(k, shp, dt, kind="ExternalInput").ap()
    for k, shp in CONST_SHAPES.items():
        I[k] = nc.dram_tensor("k_" + k, shp, F32, kind="ExternalInput").ap()
    O = {}
    for k, shp in OUT_SHAPES.items():
        O[k] = nc.dram_tensor(k, shp, F32, kind="ExternalOutput").ap()

    if DBG:
        for k, shp in {"dbg_o": [32, 768], "dbg_oc": [4, 48, 130], "dbg_os": [4, 48, 130], "dbg_ow": [4, 48, 130],
                       "dbg_sc": [4, 2, 8, 257]}.items():
            O[k] = nc.dram_tensor(k, shp, F32, kind="ExternalOutput").ap()
    wscr = {}
    for nm in ("wg_bf", "wu_bf", "wd_bf"):
        wscr[nm] = nc.dram_tensor(nm, [NFF, 128, 1024], BF16, kind="Internal").ap()
    with ExitStack() as st:
        g = Gen(nc, st)
        WSC = {nm: [T(None, "%s_t%d" % (nm, i), "dram") for i in range(NFF)] for nm in wscr}
        odram = T(None, "odram", "dram")

        PS = [g.ps("ps%d" % i, [128, 512], F32) for i in range(8)]


        modT = g.sb("modT", [128, 48, 5], F32)
        gate_p = g.sb("gate_p", [128, 2, D], F32)
        ops1 = g.sb("ops1", [128, 8, 5], F32)
        ops2 = g.sb("ops2", [128, 8, 5], F32)
        qT = g.sb("qT", [128, 6, SEQ], BF16)
        ksT = g.sb("ksT", [128, SEQ], BF16)
        kwT = g.sb("kwT", [128, SEQ], BF16)
        ycT = g.sb("ycT", [128, 2, SEQ], BF16)
        v_aug = g.sb("v_aug", [128, NT, 2, 2, 65], BF16)
        sig = g.sb("sig", [128, NT, 36], F32)
        rc = g.sb("rc", [128, NT], F32)

        ident = g.sb("ident", [128, 128], F32)
        g.ld("sp", ident[:], I["ident"], [], [ident])
        tri_le = g.sb("tri_le", [128, 128], BF16)
        tri_gt = g.sb("tri_gt", [128, 128], BF16)
        g.ld("pool", tri_le[:], I["tri_le"], [], [tri_le])
        g.ld("pool", tri_gt[:], I["tri_gt"], [], [tri_gt])
        cmpmask = g.sb("cmpmask", [128, SEQ], BF16)
        g.ld("pool", cmpmask[:], I["cmpmask"], [], [cmpmask])
        force = g.sb("force", [128, NT, 32], F32)
        validneg = g.sb("validneg", [128, NT, 32], F32)
        g.ld("sp", force[:], I["force"], [], [force])
        g.ld("sp", validneg[:], I["validneg"], [], [validneg])
        lsel = g.sb("lsel", [32, NT, 128], BF16)
        g.ld("pool", lsel[:], I["lsel"], [], [lsel])
        ones_f = g.sb("ones_f", [128, 1], F32)
        g.memset(ones_f[:], 1.0, [ones_f])

        b_adaT = g.sb("b_adaT", [128, 48], F32)
        g.ld("sp", b_adaT[:], I["b_adaT"], [], [b_adaT])
        conv_wT = g.sb("conv_wT", [128, 2, 3], F32)
        g.ld("sp", conv_wT[:], I["conv_wT"], [], [conv_wT])
        g_convT = g.sb("g_convT", [128, 2], F32)
        g.ld("sp", g_convT[:], I["g_convT"], [], [g_convT])
        g_nsaT = g.sb("g_nsaT", [128, 6], F32)
        g.ld("sp", g_nsaT[:], I["g_nsaT"], [], [g_nsaT])
        b1T = g.sb("b1T", [128, 2], F32)
        g.ld("sp", b1T[:], I["b1T"], [], [b1T])
        stC = ExitStack()
        kcT = g.sb("kcT", [128, 2, SEQ], BF16, stack=stC)
        stS = ExitStack()
        ycTs = g.sb("ycTs", [128, 2, 32], BF16, stack=stS)
        ysqs = g.sb("ysqs", [128, 2, 32], F32, stack=stS)
        gate_s = g.sb("gate_s", [32, 2, D], F32, stack=stS)
        Ps = g.sb("Ps", [32, PROJ], F32, stack=stS)
        sigS = g.sb("sigS", [32, 36], F32, stack=stS)
        qTs = g.sb("qTs", [128, 4, 6, 8], BF16, stack=stS)
        ksTs = g.sb("ksTs", [128, 32], BF16, stack=stS)
        kwTs = g.sb("kwTs", [128, 32], BF16, stack=stS)
        Pv = g.sb("Pv", [32, 2, 128], BF16, stack=stS)
        vnew = g.sb("vnew", [8, 4, 2, 2, 65], BF16, stack=stS)
        gq = g.sb("gq", [48, 4, 2, 3], F32, stack=stS)
        rc_s = g.sb("rc_s", [32, 1], F32, stack=stS)
        o_tok = g.sb("o_tok", [32, 768], F32, stack=stS)
        stA = ExitStack()
        bgate = g.sb("bgate", [128, 2, D], F32, stack=stA)
        for i, m in enumerate((2, 5)):
            g.ld("sp", bgate[:, i, :], I["b_ada"][m * D:(m + 1) * D].rearrange("(o d) -> o d", o=1).to_broadcast([128, D]),
                 [], [bgate])

        cT = g.sb("cT", [128, 8, 5], F32, stack=stA)
        g.ld("sp", cT[:], I["cvecT"], [], [cT])
        scT = g.sb("scT", [128, 8, 5], BF16, stack=stA)
        g.act(scT[:], cT[:], AF.Silu, [cT], [scT])
        rep_p = g.sb("rep_p", [128, 8, 128], BF16, stack=stA)
        g.cp(rep_p[:], scT[:, :, 0:1].to_broadcast([128, 8, 128]), [scT], [rep_p])
        rep_s = g.sb("rep_s", [128, 8, 32], BF16, stack=stA)
        for b in range(4):
            g.cp(rep_s[:, :, b * 8:(b + 1) * 8], scT[:, :, 1 + b:2 + b].to_broadcast([128, 8, 8]), [scT], [rep_s])
        wa = [g.sb("wa%d" % i, [128, 8, 512], BF16, stack=stA) for i in range(2)]
        w_ada_v = I["w_ada"].rearrange("(k p) n -> p k n", p=128)
        for nb in range(12):
            w = wa[nb % 2]
            g.ld("pool", w[:, :, :], w_ada_v[:, :, nb * 512:(nb + 1) * 512], [], [w])
            pm = PS[nb % 2]
            for sub in range(4):
                for kc in range(8):
                    g.mm(pm[:, sub * 5:(sub + 1) * 5], w[:, kc, sub * 128:(sub + 1) * 128], scT[:, kc, :],
                         kc == 0, kc == 7, [w, scT], [pm])
            g.tt(modT[:, nb * 4:(nb + 1) * 4, :], pm[:, 0:20].rearrange("p (s m) -> p s m", m=5),
                 b_adaT[:, nb * 4:(nb + 1) * 4].rearrange("p (s o) -> p s o", o=1).to_broadcast([128, 4, 5]), ALU.add,
                 [pm, b_adaT], [modT])
            if nb in (4, 5, 10, 11):
                gi = 0 if nb < 6 else 1
                half = nb % 2
                pg = PS[2 + nb % 2]
                for kc in range(8):
                    g.mm(pg[:, :], rep_p[:, kc, :], w[:, kc, :], kc == 0, kc == 7, [rep_p, w], [pg])
                g.tt(gate_p[:, gi, half * 512:(half + 1) * 512], pg[:, :], bgate[:, gi, half * 512:(half + 1) * 512],
                     ALU.add, [pg, bgate], [gate_p])
                pg2 = PS[4 + nb % 2]
                for kc in range(8):
                    g.mm(pg2[0:32, :], rep_s[:, kc, :], w[:, kc, :], kc == 0, kc == 7, [rep_s, w], [pg2])
                g.tt(gate_s[:, gi, half * 512:(half + 1) * 512], pg2[0:32, :], bgate[0:32, gi, half * 512:(half + 1) * 512],
                     ALU.add, [pg2, bgate], [gate_s])
        g.ts(ops1[:], modT[:, 8:16, :], 1.0, None, ALU.add, None, [modT], [ops1])
        g.ts(ops2[:], modT[:, 32:40, :], 1.0, None, ALU.add, None, [modT], [ops2])

        g.barrier()
        stA.close()
        stB = ExitStack()
        w_in_sb = g.sb("w_in_sb", [128, 8, PROJ], BF16, stack=stB)
        w_in_v = I["w_in"].rearrange("(k p) n -> p k n", p=128)
        w_in_k = [T(None, "w_in_k%d" % kc, "sbuf") for kc in range(8)]
        for kc in range(8):
            g.ld("pool", w_in_sb[:, kc, :], w_in_v[:, kc, :], [], [w_in_k[kc]])
        hT = g.sb("hT", [128, 8, SEQ], BF16, stack=stB)
        hT_t = [T(None, "hT_t%d" % i, "sbuf") for i in range(NT)]
        xin = [g.sb("xin%d" % i, [128, D], F32, stack=stB) for i in range(1)]
        for tt in range(NT):
            xt = xin[0]
            g.ld("sp", xt[:], I["xp"][tt * 128:(tt + 1) * 128, :], [], [xt])
            for hf in range(2):
                pb = PS[hf]
                for k4 in range(4):
                    kc = hf * 4 + k4
                    g.tr(pb[:, k4 * 128:(k4 + 1) * 128], xt[:, kc * 128:(kc + 1) * 128], ident[:], [xt, ident], [pb])
                for k4 in range(4):
                    kc = hf * 4 + k4
                    g.act(hT[:, kc, tt * 128:(tt + 1) * 128], pb[:, k4 * 128:(k4 + 1) * 128], AF.Identity,
                          [pb, ops1, modT], [hT_t[tt]], bias=modT[:, kc, 0:1], scale=ops1[:, kc, 0:1])

        hTs = g.sb("hTs", [128, 8, 32], BF16, stack=stB)
        xs_sb = g.sb("xs_sb", [32, D], F32, stack=stB)
        g.ld("sp", xs_sb[:], I["xs"], [], [xs_sb])
        for hf in range(2):
            pb = PS[2 + hf]
            for k4 in range(4):
                kc = hf * 4 + k4
                g.tr(pb[:, k4 * 32:(k4 + 1) * 32], xs_sb[:, kc * 128:(kc + 1) * 128], ident[0:32, 0:32], [xs_sb, ident], [pb])
            for k4 in range(4):
                kc = hf * 4 + k4
                for b in range(4):
                    g.act(hTs[:, kc, b * 8:(b + 1) * 8], pb[:, k4 * 32 + b * 8:k4 * 32 + (b + 1) * 8], AF.Identity,
                          [pb, ops1, modT], [hTs], bias=modT[:, kc, 1 + b:2 + b], scale=ops1[:, kc, 1 + b:2 + b])

        g.memset(v_aug[:, :, :, :, 64:65], 1.0, [v_aug])

        def fm_cols(base, r=None):
            return None

        uT = g.sb("uT", [128, 2, 514], F32, stack=stB)
        g.memset(uT[:], 0.0, [uT])
        hc_sb = g.sb("hc_sb", [128, 512], F32, stack=stB)
        acc = g.sb("acc", [128, 512], F32, stack=stB)
        ycf = g.sb("ycf", [128, 512], F32, stack=stB)
        ysq = g.sb("ysq", [128, 2, 512], F32, stack=stB)
        psi = [0]

        def next_ps():
            psi[0] = (psi[0] + 1) % 8
            return PS[psi[0]]

        def fm_proj(col_ap_fn, tc):
            p = next_ps()
            for kc in range(8):
                g.mm(p[:, :], col_ap_fn(kc), hT[:, kc, tc * 512:(tc + 1) * 512], kc == 0, kc == 7,
                     [w_in_k[kc]] + hT_t[tc * 4:(tc + 1) * 4], [p])
            return p

        for tc in range(4):
            tsl = slice(tc * 512, (tc + 1) * 512)
            for blk in range(2):
                c0 = blk * 128
                p_hc = fm_proj(lambda kc, c0=c0: w_in_sb[:, kc, c0:c0 + 128], tc)
                g.cp(hc_sb[:], p_hc[:, :], [p_hc], [hc_sb], eng="act")
                p_gc = fm_proj(lambda kc, c0=c0: w_in_sb[:, kc, 512 + c0:512 + c0 + 128], tc)
                g.tt(uT[:, blk, 2:514], p_gc[:, :], hc_sb[:], ALU.mult, [p_gc, hc_sb], [uT])
                g.ts(acc[:], uT[:, blk, 2:514], conv_wT[:, blk, 2:3], None, ALU.mult, None, [uT, conv_wT], [acc])
                g.stt(acc[:], uT[:, blk, 1:513], conv_wT[:, blk, 1:2], acc[:], ALU.mult, ALU.add, [uT, conv_wT, acc], [acc])
                g.stt(acc[:], uT[:, blk, 0:512], conv_wT[:, blk, 0:1], acc[:], ALU.mult, ALU.add, [uT, conv_wT, acc], [acc])
                p_gb = fm_proj(lambda kc, c0=c0: w_in_sb[:, kc, 256 + c0:256 + c0 + 128], tc)
                g.tt(ycf[:], p_gb[:, :], acc[:], ALU.mult, [p_gb, acc], [ycf])
                g.act(ysq[:, blk, :], ycf[:], AF.Square, [ycf], [ysq])
                g.act(ycT[:, blk, tsl], ycf[:], AF.Copy, [ycf, g_convT], [ycT], scale=g_convT[:, blk:blk + 1])
                if tc == 3:
                    g.ld("sp", O["conv_p"][:, blk, :], uT[:, blk, 512:514], [uT], [], is_output=True)
                else:
                    g.cp(uT[:, blk, 0:2], uT[:, blk, 512:514], [uT], [uT])
            for t4 in range(4):
                tt = tc * 4 + t4
                p = next_ps()
                for blk in range(2):
                    g.mm(p[:, 0:1], ysq[:, blk, t4 * 128:(t4 + 1) * 128], ones_f[:, 0:1], blk == 0, blk == 1, [ysq, ones_f], [p])
                g.ts(rc[:, tt:tt + 1], p[:, 0:1], 1.0 / 256.0, EPS, ALU.mult, ALU.add, [p], [rc])
            for r in range(6):
                p = fm_proj(lambda kc, r=r: w_in_sb[:, kc, 768 + r * 128:768 + (r + 1) * 128], tc)
                g.cp(qT[:, r, tsl], p[:, :], [p], [qT], eng=("act" if r % 2 else "dve"))
            for e in range(2):
                p = fm_proj(lambda kc, e=e: w_in_sb[:, kc, 1536 + e * 128:1536 + (e + 1) * 128], tc)
                g.cp(kcT[:, e, tsl], p[:, :], [p], [kcT], eng=("act" if e else "dve"))
            p = fm_proj(lambda kc: w_in_sb[:, kc, 1792:1920], tc)
            g.cp(ksT[:, tsl], p[:, :], [p], [ksT], eng="act")
            p = fm_proj(lambda kc: w_in_sb[:, kc, 2048:2176], tc)
            g.cp(kwT[:, tsl], p[:, :], [p], [kwT], eng="dve")
        g.rsqrt(rc[:], [rc])

        kvraw = [g.sb("kvraw%d" % i, [128, 768], F32, stack=stB) for i in range(1)]
        for tt in range(NT):
            pa = next_ps()
            pbk = next_ps()
            for kc in range(8):
                g.mm(pa[:, :], hT[:, kc, tt * 128:(tt + 1) * 128], w_in_sb[:, kc, 1536:2048], kc == 0, kc == 7,
                     [hT_t[tt], w_in_k[kc]], [pa])
            for kc in range(8):
                g.mm(pbk[:, 0:292], hT[:, kc, tt * 128:(tt + 1) * 128], w_in_sb[:, kc, 2048:2340], kc == 0, kc == 7,
                     [hT_t[tt], w_in_k[kc]], [pbk])
            kv = kvraw[0]
            g.cp(kv[:, 0:512], pa[:, :], [pa], [kv], eng="dve")
            g.cp(kv[:, 512:768], pbk[:, 0:256], [pbk], [kv], eng="act")
            g.act(sig[:, tt, :], pbk[:, 256:292], AF.Sigmoid, [pbk], [sig])
            g.cp(v_aug[:, tt, 0, :, 0:64], kv[:, 384:512].rearrange("p (g d) -> p g d", g=2), [kv], [v_aug], eng="dve")
            g.cp(v_aug[:, tt, 1, :, 0:64], kv[:, 640:768].rearrange("p (g d) -> p g d", g=2), [kv], [v_aug], eng="dve")
            g.ld("sp", O["cmp_p"][tt * 128:(tt + 1) * 128, :], kv[:, 0:256], [kv], [], is_output=True)
            g.ld("sp", O["slc_p"][tt * 128:(tt + 1) * 128, :], kv[:, 256:512], [kv], [], is_output=True)
            if tt >= 12:
                g.ld("sp", O["win_p"][(tt - 12) * 128:(tt - 11) * 128, :], kv[:, 512:768], [kv], [], is_output=True)

        for cb in range(5):
            c0 = cb * 512
            c1 = min(PROJ, c0 + 512)
            p = next_ps()
            for kc in range(8):
                g.mm(p[0:32, 0:c1 - c0], hTs[:, kc, :], w_in_sb[:, kc, c0:c1], kc == 0, kc == 7, [hTs, w_in_k[kc]], [p])
            g.cp(Ps[:, c0:c1], p[0:32, 0:c1 - c0], [p], [Ps], eng=("act" if cb % 2 else "dve"))
        g.ld("sp", O["cmp_s"], Ps[:, 1536:1792], [Ps], [], is_output=True)
        g.ld("sp", O["slc_s"], Ps[:, 1792:2048], [Ps], [], is_output=True)
        for b in range(4):
            g.ld("sp", O["win_s"][b, 504:512, :], Ps[b * 8:(b + 1) * 8, 2048:2304], [Ps], [], is_output=True)
            g.ld("sp", O["win_s"][b, 0:504, :], I["win"][b, 8:512, :], [], [], is_output=True, track=odram)
        us = g.sb("us", [128, 2, 4, 10], F32, stack=stB)
        g.ld("sp", us[:, :, :, 0:2], I["sconvT"], [], [us])
        ycs = g.sb("ycs", [128, 2, 32], F32, stack=stB)
        hcs = g.sb("hcs", [128, 32], F32, stack=stB)
        accs = g.sb("accs", [128, 32], F32, stack=stB)
        for blk in range(2):
            c0 = blk * 128

            def fm_s(cofs):
                p = next_ps()
                for kc in range(8):
                    g.mm(p[:, 0:32], w_in_sb[:, kc, cofs:cofs + 128], hTs[:, kc, :], kc == 0, kc == 7, [w_in_k[kc], hTs], [p])
                return p
            p_hc = fm_s(c0)
            g.cp(hcs[:], p_hc[:, 0:32], [p_hc], [hcs], eng="act")
            p_gc = fm_s(512 + c0)
            g.tt(us[:, blk, :, 2:10], p_gc[:, 0:32].rearrange("p (b t) -> p b t", b=4),
                 hcs[:].rearrange("p (b t) -> p b t", b=4), ALU.mult, [p_gc, hcs], [us])
            a3 = accs[:].rearrange("p (b t) -> p b t", b=4)
            g.ts(a3, us[:, blk, :, 2:10], conv_wT[:, blk, 2:3], None, ALU.mult, None, [us, conv_wT], [accs])
            g.stt(a3, us[:, blk, :, 1:9], conv_wT[:, blk, 1:2], a3, ALU.mult, ALU.add, [us, conv_wT, accs], [accs])
            g.stt(a3, us[:, blk, :, 0:8], conv_wT[:, blk, 0:1], a3, ALU.mult, ALU.add, [us, conv_wT, accs], [accs])
            p_gb = fm_s(256 + c0)
            g.tt(ycs[:, blk, :], p_gb[:, 0:32], accs[:], ALU.mult, [p_gb, accs], [ycs])
            g.act(ysqs[:, blk, :], ycs[:, blk, :], AF.Square, [ycs], [ysqs])
            g.act(ycTs[:, blk, :], ycs[:, blk, :], AF.Copy, [ycs, g_convT], [ycTs], scale=g_convT[:, blk:blk + 1])
            g.ld("sp", O["conv_s"][:, blk, :, :], us[:, blk, :, 8:10], [us], [], is_output=True)

        def fm_s2(cofs):
            p = next_ps()
            for kc in range(8):
                g.mm(p[:, 0:32], w_in_sb[:, kc, cofs:cofs + 128], hTs[:, kc, :], kc == 0, kc == 7, [w_in_k[kc], hTs], [p])
            return p
        for r in range(6):
            p = fm_s2(768 + r * 128)
            g.cp(qTs[:, :, r, :], p[:, 0:32].rearrange("p (b t) -> p b t", b=4), [p], [qTs])
        p = fm_s2(1792)
        g.cp(ksTs[:], p[:, 0:32], [p], [ksTs])
        p = fm_s2(2048)
        g.cp(kwTs[:], p[:, 0:32], [p], [kwTs])
        g.act(sigS[:], Ps[:, 2304:2340], AF.Sigmoid, [Ps], [sigS])
        g.cp(Pv[:, 0, :], Ps[:, 1920:2048], [Ps], [Pv])
        g.cp(Pv[:, 1, :], Ps[:, 2176:2304], [Ps], [Pv])
        g.memset(vnew[:], 1.0, [vnew])
        for b in range(4):
            for br in range(2):
                g.ld("sp", vnew[0:8, b, br, :, 0:64], Pv[b * 8:(b + 1) * 8, br, :].rearrange("p (g d) -> p g d", g=2), [Pv], [vnew])
            for r in range(6):
                for gg in range(2):
                    g.ld("sp", gq[r * 8:(r + 1) * 8, b, gg, :], sigS[b * 8:(b + 1) * 8, gg * 6 + r:36:12], [sigS], [gq], nc_ok=True)
        p = next_ps()
        for blk in range(2):
            g.mm(p[0:32, 0:1], ysqs[:, blk, :], ones_f[:, 0:1], blk == 0, blk == 1, [ysqs, ones_f], [p])
        g.ts(rc_s[:], p[0:32, 0:1], 1.0 / 256.0, EPS, ALU.mult, ALU.add, [p], [rc_s])
        g.rsqrt(rc_s[:], [rc_s])
        g.barrier()
        stB.close()

        def load_cmp_w(stk, tag):
            w1rep = g.sb("w1rep" + tag, [128, 2, 32, 128], BF16, stack=stk)
            g.ld("pool", w1rep[:].rearrange("p e j h -> p (e j h)"), I["w1rep"], [], [w1rep])
            peT = g.sb("peT" + tag, [128, 2, 32], BF16, stack=stk)
            g.ld("pool", peT[:].rearrange("p e j -> p (e j)"), I["peT"], [], [peT])
            w2k = g.sb("w2k" + tag, [128, 128], BF16, stack=stk)
            g.ld("pool", w2k[:], I["w2k"], [], [w2k])
            w2v = g.sb("w2v" + tag, [128, 64], BF16, stack=stk)
            g.ld("pool", w2v[:], I["w2v"], [], [w2v])
            biasT = g.sb("biasT" + tag, [128, 2], F32, stack=stk)
            pbias = next_ps()
            for e in range(2):
                for j in range(32):
                    g.mm(pbias[:, e:e + 1], w1rep[0:64, e, j, :], peT[0:64, e, j:j + 1], j == 0, j == 31, [w1rep, peT], [pbias])
            g.tt(biasT[:], pbias[:, 0:2], b1T[:], ALU.add, [pbias, b1T], [biasT])
            return w1rep, w2k, w2v, biasT

        def gelu_tanh(hx_ap, hy_ap, out_ap, hx_t, hy_t, out_t):
            g.tt(hy_ap, hx_ap, hx_ap, ALU.mult, [hx_t], [hy_t])
            g.ts(hy_ap, hy_ap, 0.044715, 1.0, ALU.mult, ALU.add, [hy_t], [hy_t])
            g.tt(hy_ap, hy_ap, hx_ap, ALU.mult, [hy_t, hx_t], [hy_t])
            g.act(hy_ap, hy_ap, AF.Tanh, [hy_t], [hy_t], scale=0.7978845608028654)
            g.ts(hy_ap, hy_ap, 0.5, 0.5, ALU.mult, ALU.add, [hy_t], [hy_t])
            g.tt(out_ap, hy_ap, hx_ap, ALU.mult, [hy_t, hx_t], [out_t])

        def layernorm(src, gi, dst, stats, mv, lnbc, n=128):
            src_ap, src_t = src
            dst_ap, dst_t = dst
            for c2 in range(2):
                g.emit("dve", lambda e, c2=c2: e.bn_stats(out=stats[0:n, c2, :], in_=src_ap[:, c2 * 512:(c2 + 1) * 512]),
                       reads=[src_t], writes=[stats])
            g.emit("dve", lambda e: e.bn_aggr(out=mv[0:n, :], in_=stats[0:n, :, :]), reads=[stats], writes=[mv])
            g.ts(mv[0:n, 1:2], mv[0:n, 1:2], EPS, None, ALU.add, None, [mv], [mv])
            g.rsqrt(mv[0:n, 1:2], [mv])
            g.ts(dst_ap, src_ap, mv[0:n, 0:1], mv[0:n, 1:2], ALU.subtract, ALU.mult, [src_t, mv], [dst_t])
            g.tt(dst_ap, dst_ap, lnbc[0:n, gi, :], ALU.mult, [dst_t, lnbc], [dst_t])
            g.tt(dst_ap, dst_ap, lnbc[0:n, gi + 1, :], ALU.add, [dst_t, lnbc], [dst_t])

        w_o_v = I["w_o"].rearrange("(k p) n -> p k n", p=128)
        w_g_v = I["w_g"].rearrange("(k p) n -> p k n", p=128)
        w_u_v = I["w_u"].rearrange("(k p) n -> p k n", p=128)

        stS2 = ExitStack()
        w1rep_s, w2k_s, w2v_s, biasT_s = load_cmp_w(stS2, "_s")
        ident_bf = g.sb("ident_bf", [128, 128], BF16, stack=stS2)
        g.ld("pool", ident_bf[:], I["ident"], [], [ident_bf])
        ones_bf = g.sb("ones_bf", [128, 1], BF16, stack=stS2)
        g.memset(ones_bf[:], 1.0, [ones_bf])
        force_s = g.sb("force_s", [8, 257], F32, stack=stS2)
        g.ld("sp", force_s[:], I["force_s"], [], [force_s])
        gsum = g.sb("gsum", [48, 8], F32, stack=stS2)
        g.ld("sp", gsum[:], I["gsum"], [], [gsum])
        gtb = g.sb("gtb", [8, 48], BF16, stack=stS2)
        g.ld("pool", gtb[:], I["gt"], [], [gtb])
        cnew = g.sb("cnew", [8, 48], BF16, stack=stS2)
        g.ld("pool", cnew[:], I["causal_new"], [], [cnew])
        mw0 = g.sb("mw0", [128, 48], BF16, stack=stS2)
        g.ld("pool", mw0[:], I["mw0"], [], [mw0])
        ptT_sb = g.sb("ptT_sb", [128, 4], I32, stack=stS2)
        g.ld("sp", ptT_sb[:], I["ptT"], [], [ptT_sb])
        idx8 = g.sb("idx8", [128, 4, 8], I32, stack=stS2)
        for b in range(4):
            for k in range(8):
                g.ts(idx8[:, b, k:k + 1], ptT_sb[:, b:b + 1], 8, k, ALU.mult, ALU.add, [ptT_sb], [idx8])
        vaug_s = g.sb("vaug_s", [128, 8, 2, 322], BF16, stack=stS2)
        g.memset(vaug_s[:], 1.0, [vaug_s])
        for gg in range(2):
            g.ld("pool", vaug_s[:, :, gg, 65:322], I["as_"], [], [vaug_s])
        Xg = [g.sb("Xg%d" % i, [128, 16, 256], BF16, stack=stS2) for i in range(1)]
        XT = g.sb("XT", [128, 2, 16, 128], BF16, stack=stS2)
        TB = g.sb("TB", [128, 2, 4, 1024], F32, stack=stS2)
        hidTs = g.sb("hidTs", [128, 4, 1024], BF16, stack=stS2)
        g.memset(hidTs[:], 0.0, [hidTs])
        kcmpTs = g.sb("kcmpTs", [128, 1024], BF16, stack=stS2)
        ecs = g.sb("ecs", [128, 8, 48], BF16, stack=stS2)
        pss = g.sb("pss", [128, 8, 48], BF16, stack=stS2)
        rs_s = g.sb("rs_s", [48, 3], F32, stack=stS2)
        scn = g.sb("scn", [48, 257], F32, stack=stS2)
        sc2s = g.sb("sc2s", [8, 257], F32, stack=stS2)
        scw = g.sb("scw", [8, 257], F32, stack=stS2)
        m8s = g.sb("m8s", [8, 8], F32, stack=stS2)
        m8t = g.sb("m8t", [8, 8], F32, stack=stS2)
        sel_s = g.sb("sel_s", [8, 258], BF16, stack=stS2)
        maskTs = g.sb("maskTs", [128, 2, 2, 48], BF16, stack=stS2)
        oc_sb = g.sb("oc_sb", [48, 2, 65], F32, stack=stS2)
        pnew = g.sb("pnew", [8, 48], BF16, stack=stS2)
        Wb = g.sb("Wb", [128, 4, 256], BF16, stack=stS2)
        vx = g.sb("vx", [128, 16, 2, 65], BF16, stack=stS2)
        g.memset(vx[:], 1.0, [vx])
        Wv = g.sb("Wv", [128, 4, 2, 65], BF16, stack=stS2)
        g.memset(Wv[:], 1.0, [Wv])
        KTw = g.sb("KTw", [128, 4, 128], BF16, stack=stS2)
        cfs = g.sb("cfs", [48, 3], F32, stack=stS2)
        og_sb = g.sb("og_sb", [48, 2, 64], F32, stack=stS2)
        psb = [PS[0].h.bitcast(BF16), PS[1].h.bitcast(BF16)]
        tcount = [0]

        def transposes8(srcs, src_t, dst_ap, dst_t):
            i = tcount[0] % 2
            tcount[0] += 1
            for k, sap in enumerate(srcs):
                g.tr(psb[i][:, k * 128:(k + 1) * 128], sap, ident_bf[:], [src_t, ident_bf], [PS[i]])
            n = len(srcs)
            g.cp(dst_ap, psb[i][:, 0:n * 128].rearrange("p (k q) -> p k q", q=128), [PS[i]], [dst_t],
                 eng=("act" if tcount[0] % 2 else "dve"))

        def gather(pool_name, b, k, dst):
            g.dma("pool", lambda e: e.indirect_dma_start(
                out=dst[:].rearrange("p r c -> p (r c)"), out_offset=None, in_=I[pool_name],
                in_offset=bass.IndirectOffsetOnAxis(ap=idx8[:, b, k:k + 1], axis=0)), reads=[idx8], writes=[dst])

        dbt_all = g.sb("dbt_all", [48, 3, 130], F32, stack=stS2) if DBG else None
        gcount = [0]
        for b in range(4 if STAGE >= 2 else 0):
            qb = [qTs[0:64, b, :, :], qTs[64:128, b, :, :]]
            for cl in range(8):
                X = Xg[0]
                gcount[0] += 1
                gather("cmp_pool", b, cl, X)
                if SUB < 1:
                    continue
                for e in range(2):
                    for jh in range(2):
                        transposes8([X[:, jh * 8 + j8, e * 128:(e + 1) * 128] for j8 in range(8)], X,
                                    XT[:, e, jh * 8:(jh + 1) * 8, :], XT)
                if SUB < 2:
                    continue
                for tb in range(2):
                    for gg in range(2):
                        pb = PS[2 + gg] if tb == 0 else PS[4 + gg]
                        gs = slice(gg * 64, (gg + 1) * 64)
                        for e in range(2):
                            col = e * 128
                            for j in range(16):
                                g.mm(pb[:, col:col + 128], w1rep_s[gs, e, tb * 16 + j, :], XT[gs, e, j, :], j == 0, j == 15,
                                     [w1rep_s, XT], [pb])
                        g.cp(TB[:, tb, gg:4:2, cl:1024:8], pb[:, 0:256].rearrange("p (a q) -> p a q", q=128), [pb], [TB],
                             eng=("act" if (tb + gg) % 2 else "dve"))
            if SUB < 3:
                continue
            hx_ap = TB[:, 0, :, 0:1023]
            hy_ap = TB[:, 1, :, 0:1023]
            g.tt(hx_ap, TB[:, 0, :, 0:1023], TB[:, 1, :, 1:1024], ALU.add, [TB], [TB])
            for e in range(2):
                g.ts(TB[:, 0, e * 2:(e + 1) * 2, 0:1023], TB[:, 0, e * 2:(e + 1) * 2, 0:1023], biasT_s[:, e:e + 1], None, ALU.add, None,
                     [TB, biasT_s], [TB])
            gelu_tanh(hx_ap, hy_ap, hidTs[:, :, 0:1023], TB, TB, hidTs)
            if SUB < 4:
                continue
            for gg in range(2):
                gs = slice(gg * 64, (gg + 1) * 64)
                for hf in range(2):
                    po = PS[2 + hf]
                    g.mm(po[:, :], w2k_s[:], hidTs[:, gg, hf * 512:(hf + 1) * 512], True, True, [w2k_s, hidTs], [po])
                    g.cp(kcmpTs[gs, hf * 512:(hf + 1) * 512], po[gs, :], [po], [kcmpTs], eng="act")
                po = PS[4]
                for it in range(8):
                    g.mm(po[:, it * 64:(it + 1) * 64], hidTs[:, 2 + gg, it * 128:(it + 1) * 128], w2v_s[:], True, True,
                         [hidTs, w2v_s], [po])
                g.cp(vaug_s[:, :, gg, 0:64], po[:, :].rearrange("p (i d) -> p i d", d=64), [po], [vaug_s], eng="dve")
            if STAGE < 3:
                continue
            for gg in range(2):
                gs = slice(gg * 64, (gg + 1) * 64)
                pS = PS[2]
                for it in range(8):
                    g.mm(pS[:, it * 48:(it + 1) * 48], kcmpTs[gs, it * 128:(it + 1) * 128], qb[gg], True, True, [kcmpTs, qTs], [pS])
                g.act(ecs[:], pS[:, 0:384].rearrange("p (i q) -> p i q", q=48), AF.Exp, [pS], [ecs], scale=SCALE)
                pO = PS[3]
                for it in range(8):
                    n = 127 if it == 7 else 128
                    g.mm(pO[0:48, 0:322], ecs[0:n, it, :], vaug_s[0:n, it, gg, :], it == 0, it == 7, [ecs, vaug_s], [pO], signal=True)
                g.cp(oc_sb[:, gg, :], pO[0:48, 0:65], [pO], [oc_sb], eng="act")
                g.ts(rs_s[:, 0:1], pO[0:48, 64:65], 1e-30, None, ALU.max, None, [pO], [rs_s])
                g.emit("dve", lambda e: e.reciprocal(out=rs_s[:, 0:1], in_=rs_s[:, 0:1]), reads=[rs_s], writes=[rs_s])
                g.ts(scn[:], pO[0:48, 65:322], rs_s[:, 0:1], None, ALU.mult, None, [pO, rs_s], [scn])
                pI = PS[4]
                g.mm(pI[0:8, 0:257], gsum[:], scn[:], True, True, [gsum, scn], [pI])
                g.tt(sc2s[:], pI[0:8, 0:257], force_s[:], ALU.max, [pI, force_s], [sc2s])
                if DBG:
                    g.ld("sp", O["dbg_sc"][b, gg], sc2s[:], [sc2s], [], is_output=True)
                g.emit("dve", lambda e: e.max(out=m8s[:], in_=sc2s[:]), reads=[sc2s], writes=[m8s])
                g.emit("dve", lambda e: e.match_replace(out=scw[:], in_to_replace=m8s[:], in_values=sc2s[:], imm_value=-3e38),
                       reads=[m8s, sc2s], writes=[scw])
                g.emit("dve", lambda e: e.max(out=m8t[:], in_=scw[:]), reads=[scw], writes=[m8t])
                g.ts(sel_s[:, 0:257], sc2s[:], m8t[:, 7:8], None, ALU.is_ge, None, [sc2s, m8t], [sel_s])
                for par in range(2):
                    pM = PS[4]
                    g.mm(pM[:, 0:48], sel_s[:, par:256:2], gtb[:], True, True, [sel_s, gtb], [pM])
                    g.cp(maskTs[:, gg, par, :], pM[:, 0:48], [pM], [maskTs], eng="act")
            if STAGE < 4:
                continue
            pOs2 = [PS[4], PS[5]]
            pOw2 = [PS[6], PS[7]]
            for rg in range(8):
                X = Xg[0]
                gcount[0] += 1
                gather("slc_pool", b, rg, X)
                for rh in range(2):
                    transposes8([X[:, rh * 8 + r8, 0:128] for r8 in range(8)], X, XT[:, 0, rh * 8:(rh + 1) * 8, :], XT)
                par = rg // 4
                g.cp(vx[:, :, :, 0:64], X[:, :, 128:256].rearrange("p r (g d) -> p r g d", g=2), [X], [vx], eng="dve")
                for gg in range(2):
                    gs = slice(gg * 64, (gg + 1) * 64)
                    for rh in range(2):
                        pS = PS[2 + rh]
                        for r8 in range(8):
                            g.mm(pS[:, r8 * 48:(r8 + 1) * 48], XT[gs, 0, rh * 8 + r8, :], qb[gg], True, True, [XT, qTs], [pS])
                        g.act(ecs[:], pS[:, 0:384].rearrange("p (i q) -> p i q", q=48), AF.Exp, [pS], [ecs], scale=SCALE)
                        g.tt(pss[:], ecs[:], maskTs[:, gg, par, :].rearrange("p (o q) -> p o q", o=1).to_broadcast([128, 8, 48]),
                             ALU.mult, [ecs, maskTs], [pss])
                        for r8 in range(8):
                            rr = rh * 8 + r8
                            first = (rg == 0 and rr == 0)
                            g.mm(pOs2[gg][0:48, 0:65], pss[:, r8, :], vx[:, rr, gg, :], first, False,
                                 [pss, vx], [pOs2[gg]], signal=True)
            for gg in range(2):
                gs = slice(gg * 64, (gg + 1) * 64)
                for br, (kTn, pOx) in enumerate(((ksTs, pOs2[gg]), (kwTs, pOw2[gg]))):
                    if br == 1:
                        continue
                    pS = PS[2]
                    g.mm(pS[0:8, 0:48], kTn[gs, b * 8:(b + 1) * 8], qb[gg], True, True, [kTn, qTs], [pS])
                    g.act(pnew[:], pS[0:8, 0:48], AF.Exp, [pS], [pnew], scale=SCALE)
                    g.tt(pnew[:], pnew[:], cnew[:], ALU.mult, [pnew, cnew], [pnew])
                    g.mm(pOx[0:48, 0:65], pnew[:], vnew[0:8, b, br, gg, :], False, True, [pnew, vnew], [pOx], signal=True)
            if STAGE < 5:
                continue
            g.ld("pool", Wb[:], I["win"][b].rearrange("(w p) c -> p w c", p=128), [], [Wb])
            transposes8([Wb[:, w, 0:128] for w in range(4)], Wb, KTw[:, :, :], KTw)
            g.cp(Wv[:, :, :, 0:64], Wb[:, :, 128:256].rearrange("p w (g d) -> p w g d", g=2), [Wb], [Wv], eng="dve")
            for gg in range(2):
                gs = slice(gg * 64, (gg + 1) * 64)
                pS = PS[2]
                for w in range(4):
                    g.mm(pS[:, w * 48:(w + 1) * 48], KTw[gs, w, :], qb[gg], True, True, [KTw, qTs], [pS])
                g.act(ecs[:, 0:4, :], pS[:, 0:192].rearrange("p (i q) -> p i q", q=48), AF.Exp, [pS], [ecs], scale=SCALE)
                g.tt(ecs[:, 0, :], ecs[:, 0, :], mw0[:], ALU.mult, [ecs, mw0], [ecs])
                for w in range(4):
                    g.mm(pOw2[gg][0:48, 0:65], ecs[:, w, :], Wv[:, w, gg, :], w == 0, False, [ecs, Wv], [pOw2[gg]], signal=True)
                pS = PS[3]
                g.mm(pS[0:8, 0:48], kwTs[gs, b * 8:(b + 1) * 8], qb[gg], True, True, [kwTs, qTs], [pS])
                g.act(pnew[:], pS[0:8, 0:48], AF.Exp, [pS], [pnew], scale=SCALE)
                g.tt(pnew[:], pnew[:], cnew[:], ALU.mult, [pnew, cnew], [pnew])
                g.mm(pOw2[gg][0:48, 0:65], pnew[:], vnew[0:8, b, 1, gg, :], False, True, [pnew, vnew], [pOw2[gg]], signal=True)
            if STAGE < 6:
                continue
            if DBG:
                dbt = dbt_all
                g.cp(dbt[:, 0, :], oc_sb[:].rearrange("p g c -> p (g c)"), [oc_sb], [dbt])
                for gg_ in range(2):
                    g.cp(dbt[:, 1, gg_ * 65:(gg_ + 1) * 65], pOs2[gg_][0:48, 0:65], [pOs2[gg_]], [dbt])
                    g.cp(dbt[:, 2, gg_ * 65:(gg_ + 1) * 65], pOw2[gg_][0:48, 0:65], [pOw2[gg_]], [dbt])
                g.ld("sp", O["dbg_oc"][b], dbt[:, 0, :], [dbt], [], is_output=True)
                g.ld("sp", O["dbg_os"][b], dbt[:, 1, :], [dbt], [], is_output=True)
                g.ld("sp", O["dbg_ow"][b], dbt[:, 2, :], [dbt], [], is_output=True)
            for gg in range(2):
                g.ts(rs_s[:, 0:1], oc_sb[:, gg, 64:65], 1e-30, None, ALU.max, None, [oc_sb], [rs_s])
                pOs = pOs2[gg]
                pOw = pOw2[gg]
                g.ts(rs_s[:, 1:2], pOs[0:48, 64:65], 1e-30, None, ALU.max, None, [pOs], [rs_s])
                g.ts(rs_s[:, 2:3], pOw[0:48, 64:65], 1e-30, None, ALU.max, None, [pOw], [rs_s])
                g.emit("dve", lambda e: e.reciprocal(out=rs_s[:], in_=rs_s[:]), reads=[rs_s], writes=[rs_s])
                g.tt(cfs[:], rs_s[:], gq[:, b, gg, :], ALU.mult, [rs_s, gq], [cfs])
                g.ts(og_sb[:, gg, :], oc_sb[:, gg, 0:64], cfs[:, 0:1], None, ALU.mult, None, [oc_sb, cfs], [og_sb])
                g.stt(og_sb[:, gg, :], pOs[0:48, 0:64], cfs[:, 1:2], og_sb[:, gg, :], ALU.mult, ALU.add,
                      [pOs, cfs, og_sb], [og_sb])
                g.stt(og_sb[:, gg, :], pOw[0:48, 0:64], cfs[:, 2:3], og_sb[:, gg, :], ALU.mult, ALU.add,
                      [pOw, cfs, og_sb], [og_sb])
                for r in range(6):
                    g.ld("sp", o_tok[b * 8:(b + 1) * 8, gg * 384 + r * 64:gg * 384 + (r + 1) * 64], og_sb[r * 8:(r + 1) * 8, gg, :],
                         [og_sb], [o_tok])

        if DBG:
            g.ld("sp", O["dbg_o"], o_tok[:], [o_tok], [], is_output=True)
        if STAGE >= 7:
            g.barrier()
            stS2.close()
            stS2 = ExitStack()
            w_o_s = g.sb("w_o_s", [128, 8, D], BF16, stack=stS2)
            for kc in range(8):
                g.ld("pool", w_o_s[:, kc, :], w_o_v[:, kc, :], [], [w_o_s])
            lnbc_s = g.sb("lnbc_s", [32, 4, D], F32, stack=stS2)
            for i, nm in enumerate(("ln1_g", "ln1_b", "ln2_g", "ln2_b")):
                g.ld("sp", lnbc_s[:, i, :], I[nm].rearrange("(o d) -> o d", o=1).to_broadcast([32, D]), [], [lnbc_s])
            stats_s = g.sb("stats_s", [32, 2, 6], F32, stack=stS2)
            mv_s = g.sb("mv_s", [32, 2], F32, stack=stS2)
            osq_s = g.sb("osq_s", [32, 768], F32, stack=stS2)
            rn_s = g.sb("rn_s", [32, 1], F32, stack=stS2)
            oTs = g.sb("oTs", [128, 6, 32], BF16, stack=stS2)
            t1s = g.sb("t1s", [32, D], F32, stack=stS2)
            xs2 = g.sb("xs2", [32, D], F32, stack=stS2)
            x1s = g.sb("x1s", [32, D], F32, stack=stS2)
            h2Ts = g.sb("h2Ts", [128, 8, 32], BF16, stack=stS2)
            aTs = g.sb("aTs", [128, NFF, 32], BF16, stack=stS2)
            sgs = g.sb("sgs", [128, 32], F32, stack=stS2)
            wgs = [g.sb("wgs%d" % i, [128, 8, 128], BF16, stack=stS2) for i in range(2)]
            wus = [g.sb("wus%d" % i, [128, 8, 128], BF16, stack=stS2) for i in range(2)]
            wds = [g.sb("wds%d" % i, [128, D], BF16, stack=stS2) for i in range(2)]
            g.tt(osq_s[:], o_tok[:], o_tok[:], ALU.mult, [o_tok], [osq_s])
            g.emit("dve", lambda e: e.reduce_sum(out=rn_s[:], in_=osq_s[:], axis=AX.X), reads=[osq_s], writes=[rn_s])
            g.ts(rn_s[:], rn_s[:], 1.0 / 768.0, EPS, ALU.mult, ALU.add, [rn_s], [rn_s])
            g.rsqrt(rn_s[:], [rn_s])
            pb = PS[7]
            for blk in range(6):
                g.tr(pb[:, blk * 32:(blk + 1) * 32], o_tok[:, blk * 128:(blk + 1) * 128], ident[0:32, 0:32], [o_tok, ident], [pb])
            for blk in range(6):
                g.act(oTs[:, blk, :], pb[:, blk * 32:(blk + 1) * 32], AF.Copy, [pb, g_nsaT], [oTs], scale=g_nsaT[:, blk:blk + 1])
            for hf in range(2):
                cs_ = slice(hf * 512, (hf + 1) * 512)
                pc = PS[0 + hf]
                pn = PS[2 + hf]
                for blk in range(2):
                    g.mm(pc[0:32, :], ycTs[:, blk, :], w_o_s[:, blk, cs_], blk == 0, blk == 1, [ycTs, w_o_s], [pc])
                for blk in range(6):
                    g.mm(pn[0:32, :], oTs[:, blk, :], w_o_s[:, 2 + blk, cs_], blk == 0, blk == 5, [oTs, w_o_s], [pn])
                g.ts(t1s[:, cs_], pc[0:32, :], rc_s[:, 0:1], None, ALU.mult, None, [pc, rc_s], [t1s])
                g.stt(t1s[:, cs_], pn[0:32, :], rn_s[:, 0:1], t1s[:, cs_], ALU.mult, ALU.add, [pn, rn_s, t1s], [t1s])
            g.tt(t1s[:], t1s[:], gate_s[:, 0, :], ALU.mult, [t1s, gate_s], [t1s])
            g.ld("sp", xs2[:], I["xs"], [], [xs2])
            g.stt(t1s[:], xs2[:], ALPHA, t1s[:], ALU.mult, ALU.add, [xs2, t1s], [t1s])
            layernorm((t1s[:], t1s), 0, (x1s[:], x1s), stats_s, mv_s, lnbc_s, n=32)
            for hf in range(2):
                pb = PS[6 + hf]
                for k4 in range(4):
                    kc = hf * 4 + k4
                    g.tr(pb[:, k4 * 32:(k4 + 1) * 32], x1s[:, kc * 128:(kc + 1) * 128], ident[0:32, 0:32], [x1s, ident], [pb])
                for k4 in range(4):
                    kc = hf * 4 + k4
                    for b in range(4):
                        g.act(h2Ts[:, kc, b * 8:(b + 1) * 8], pb[:, k4 * 32 + b * 8:k4 * 32 + (b + 1) * 8], AF.Identity,
                              [pb, ops2, modT], [h2Ts], bias=modT[:, 24 + kc, 1 + b:2 + b], scale=ops2[:, kc, 1 + b:2 + b])
            for ffc in range(NFF):
                wg_t = wgs[ffc % 2]
                wu_t = wus[ffc % 2]
                fs = slice(ffc * 128, (ffc + 1) * 128)
                g.ld("pool", wg_t[:], w_g_v[:, :, fs], [], [wg_t])
                g.ld("pool", wu_t[:], w_u_v[:, :, fs], [], [wu_t])
                g.ld("sp", wscr["wg_bf"][ffc], wg_t[:].rearrange("p k n -> p (k n)"), [wg_t], [WSC["wg_bf"][ffc]])
                g.ld("sp", wscr["wu_bf"][ffc], wu_t[:].rearrange("p k n -> p (k n)"), [wu_t], [WSC["wu_bf"][ffc]])
                pG = PS[(2 * ffc) % 4]
                pU = PS[(2 * ffc + 1) % 4]
                for kc in range(8):
                    g.mm(pG[:, 0:32], wg_t[:, kc, :], h2Ts[:, kc, :], kc == 0, kc == 7, [wg_t, h2Ts], [pG])
                for kc in range(8):
                    g.mm(pU[:, 0:32], wu_t[:, kc, :], h2Ts[:, kc, :], kc == 0, kc == 7, [wu_t, h2Ts], [pU])
                g.act(sgs[:], pG[:, 0:32], AF.Silu, [pG], [sgs])
                g.tt(aTs[:, ffc, :], sgs[:], pU[:, 0:32], ALU.mult, [sgs, pU], [aTs])
            for ffc in range(NFF):
                wd_t = wds[ffc % 2]
                g.ld("pool", wd_t[:], I["w_d"][ffc * 128:(ffc + 1) * 128, :], [], [wd_t])
                g.ld("sp", wscr["wd_bf"][ffc], wd_t[:], [wd_t], [WSC["wd_bf"][ffc]])
                for hf in range(2):
                    g.mm(PS[4 + hf][0:32, :], aTs[:, ffc, :], wd_t[:, hf * 512:(hf + 1) * 512], ffc == 0, ffc == NFF - 1,
                         [aTs, wd_t], [PS[4 + hf]], signal=True)
            for hf in range(2):
                cs_ = slice(hf * 512, (hf + 1) * 512)
                g.tt(t1s[:, cs_], PS[4 + hf][0:32, :], gate_s[:, 1, cs_], ALU.mult, [PS[4 + hf], gate_s], [t1s])
            g.stt(t1s[:], x1s[:], ALPHA, t1s[:], ALU.mult, ALU.add, [x1s, t1s], [t1s])
            layernorm((t1s[:], t1s), 2, (xs2[:], xs2), stats_s, mv_s, lnbc_s, n=32)
            g.ld("sp", O["y_s"], xs2[:], [xs2], [], is_output=True)
        g.barrier()
        stS2.close()
        stS.close()


        if DO_PROMPT:
            kcmpT = g.sb("kcmpT", [128, 128], BF16, stack=stC)
            vcmp = g.sb("vcmp", [128, 2, 97], BF16, stack=stC)
            stC2 = ExitStack()
            w1rep, w2k, w2v, biasT = load_cmp_w(stC2, "_p")
            acmp_f = g.sb("acmp_f", [128, 32], F32, stack=stC2)
            g.ld("sp", acmp_f[:], I["acmp"], [], [acmp_f])
            g.memset(vcmp[:], 0.0, [vcmp])
            g.memset(vcmp[:, :, 64:65], 1.0, [vcmp])
            for gg in range(2):
                g.cp(vcmp[:, gg, 65:97], acmp_f[:], [acmp_f, vcmp], [vcmp])
            bot_sb = g.sb("bot_sb", [128, 128], F32, stack=stC2)
            hx = g.sb("hx", [128, 127], F32, stack=stC2)
            hy = g.sb("hy", [128, 127], F32, stack=stC2)
            hidT = g.sb("hidT", [128, 127], BF16, stack=stC2)
            for e in range(2):
                for gg in range(2):
                    gs = slice(gg * 64, (gg + 1) * 64)
                    ptop = next_ps()
                    pbot = next_ps()
                    for j in range(16):
                        g.mm(ptop[:, 0:128], w1rep[gs, e, j, :], kcT[gs, e, j:SEQ:16], j == 0, j == 15, [w1rep, kcT], [ptop])
                    for j in range(16):
                        g.mm(pbot[:, 0:128], w1rep[gs, e, 16 + j, :], kcT[gs, e, j:SEQ:16], j == 0, j == 15, [w1rep, kcT], [pbot])
                    g.cp(bot_sb[:], pbot[:, 0:128], [pbot], [bot_sb], eng="act")
                    g.tt(hx[:], ptop[:, 0:127], bot_sb[:, 1:128], ALU.add, [ptop, bot_sb], [hx])
                    g.ts(hx[:], hx[:], biasT[:, e:e + 1], None, ALU.add, None, [hx, biasT], [hx])
                    g.tt(hy[:], hx[:], hx[:], ALU.mult, [hx], [hy])
                    g.ts(hy[:], hy[:], 0.044715, 1.0, ALU.mult, ALU.add, [hy], [hy])
                    g.tt(hy[:], hy[:], hx[:], ALU.mult, [hy, hx], [hy])
                    g.act(hy[:], hy[:], AF.Tanh, [hy], [hy], scale=0.7978845608028654)
                    g.ts(hy[:], hy[:], 0.5, 0.5, ALU.mult, ALU.add, [hy], [hy])
                    g.tt(hidT[:], hy[:], hx[:], ALU.mult, [hy, hx], [hidT])
                    po = next_ps()
                    if e == 0:
                        g.mm(po[:, 0:127], w2k[:], hidT[:], True, True, [w2k, hidT], [po])
                        g.cp(kcmpT[gs, 0:127], po[gs, 0:127], [po], [kcmpT], eng="dve")
                    else:
                        g.mm(po[0:127, 0:64], hidT[:], w2v[:], True, True, [w2v, hidT], [po])
                        g.cp(vcmp[0:127, gg, 0:64], po[0:127, 0:64], [po], [vcmp], eng="dve")
            g.barrier()
            stC2.close()

            stD = ExitStack()
            w_o_sb = g.sb("w_o_sb", [128, 8, D], BF16, stack=stD)
            for kc in range(8):
                g.ld("pool", w_o_sb[:, kc, :], w_o_v[:, kc, :], [], [w_o_sb])
            lnbc = g.sb("lnbc", [128, 4, D], F32, stack=stD)
            for i, nm in enumerate(("ln1_g", "ln1_b", "ln2_g", "ln2_b")):
                g.ld("sp", lnbc[:, i, :], I[nm].rearrange("(o d) -> o d", o=1).to_broadcast([128, D]), [], [lnbc])
            ec = g.sb("ec", [128, 6, 128], BF16, stack=stD)
            es = [g.sb("es%d" % i, [128, 4, 128], BF16, stack=stD) for i in range(3)]
            pTt = [g.sb("pTt%d" % i, [128, 4, 128], BF16, stack=stD) for i in range(3)]
            maskT = g.sb("maskT", [128, NT, 128], BF16, stack=stD)
            rsc = g.sb("rsc", [128, 6], F32, stack=stD)
            score = g.sb("score", [128, 32], F32, stack=stD)
            sc2 = g.sb("sc2", [128, 32], F32, stack=stD)
            m8a = g.sb("m8a", [128, 8], F32, stack=stD)
            m8b = g.sb("m8b", [128, 8], F32, stack=stD)
            sel = g.sb("sel", [128, 32], F32, stack=stD)
            selT = g.sb("selT", [32, 128], BF16, stack=stD)
            rs2 = g.sb("rs2", [128, 2], F32, stack=stD)
            cf = g.sb("cf", [128, 3], F32, stack=stD)
            o_sb = g.sb("o_sb", [128, 768], F32, stack=stD)
            osq = g.sb("osq", [128, 768], F32, stack=stD)
            rn = g.sb("rn", [128, 1], F32, stack=stD)
            oT = g.sb("oT", [128, 6, 128], BF16, stack=stD)
            t1 = g.sb("t1", [128, D], F32, stack=stD)
            xt2 = g.sb("xt2", [128, D], F32, stack=stD)
            stats = g.sb("stats", [128, 2, 6], F32, stack=stD)
            mv = g.sb("mv", [128, 2], F32, stack=stD)
            x1g = g.sb("x1g", [128, 4, D], F32, stack=stD)
            h2T = g.sb("h2T", [128, 8, 512], BF16, stack=stD)
            aT = g.sb("aT", [128, NFF, 512], BF16, stack=stD)
            sg_sb = g.sb("sg_sb", [128, 512], F32, stack=stD)
            wgc = [g.sb("wgc%d" % i, [128, 8, 128], BF16, stack=stD) for i in range(2)]
            wuc = [g.sb("wuc%d" % i, [128, 8, 128], BF16, stack=stD) for i in range(2)]
            wdc = [g.sb("wdc%d" % i, [128, D], BF16, stack=stD) for i in range(2)]
            w_g_v = I["w_g"].rearrange("(k p) n -> p k n", p=128)
            w_u_v = I["w_u"].rearrange("(k p) n -> p k n", p=128)

            for grp in range(NT // 4):
                for t4 in range(4):
                    qt = grp * 4 + t4
                    qs = slice(qt * 128, (qt + 1) * 128)
                    for gg in range(2):
                        gs = slice(gg * 64, (gg + 1) * 64)
                        for r in range(6):
                            pS = PS[2 + (r // 4)]
                            g.mm(pS[0:127, (r % 4) * 128:(r % 4 + 1) * 128], kcmpT[gs, 0:127], qT[gs, r, qs], True, True,
                                 [kcmpT, qT], [pS])
                        g.act(ec[0:127, 0:4, :], PS[2][0:127, :].rearrange("p (r q) -> p r q", r=4), AF.Exp, [PS[2]], [ec], scale=SCALE)
                        g.act(ec[0:127, 4:6, :], PS[3][0:127, 0:256].rearrange("p (r q) -> p r q", r=2), AF.Exp, [PS[3]], [ec], scale=SCALE)
                        g.tt(ec[0:127, :, :], ec[0:127, :, :],
                             cmpmask[0:127, qs].rearrange("p (o q) -> p o q", o=1).to_broadcast([127, 6, 128]), ALU.mult,
                             [ec, cmpmask], [ec])
                        for r in range(6):
                            pO = PS[r // 3]
                            g.mm(pO[:, (r % 3) * 97:(r % 3 + 1) * 97], ec[0:127, r, :], vcmp[0:127, gg, :], True, True, [ec, vcmp], [pO])
                        for hh in range(2):
                            g.ts(rsc[:, hh * 3:(hh + 1) * 3], PS[hh][:, 0:291].rearrange("p (r c) -> p r c", c=97)[:, :, 64],
                                 1e-30, None, ALU.max, None, [PS[hh]], [rsc])
                        g.emit("dve", lambda e: e.reciprocal(out=rsc[:], in_=rsc[:]), reads=[rsc], writes=[rsc])
                        for r in range(6):
                            src = PS[r // 3][:, (r % 3) * 97 + 65:(r % 3) * 97 + 97]
                            if r == 0:
                                g.ts(score[:], src, rsc[:, 0:1], None, ALU.mult, None, [PS[0], rsc], [score])
                            else:
                                g.stt(score[:], src, rsc[:, r:r + 1], score[:], ALU.mult, ALU.add, [PS[r // 3], rsc, score], [score])
                        g.tt(sc2[:], score[:], force[:, qt, :], ALU.max, [score, force], [sc2])
                        g.tt(sc2[:], sc2[:], validneg[:, qt, :], ALU.add, [sc2, validneg], [sc2])
                        g.emit("dve", lambda e: e.max(out=m8a[:], in_=sc2[:]), reads=[sc2], writes=[m8a])
                        g.emit("dve", lambda e: e.match_replace(out=score[:], in_to_replace=m8a[:], in_values=sc2[:], imm_value=-3e38),
                               reads=[m8a, sc2], writes=[score])
                        g.emit("dve", lambda e: e.max(out=m8b[:], in_=score[:]), reads=[score], writes=[m8b])
                        g.ts(sel[:], sc2[:], m8b[:, 7:8], None, ALU.is_ge, None, [sc2, m8b], [sel])
                        g.tr(PS[4][0:32, 0:128], sel[:], ident[:], [sel, ident], [PS[4]])
                        g.cp(selT[:], PS[4][0:32, 0:128], [PS[4]], [selT], eng="act")
                        for k0 in range(0, qt + 1, 4):
                            nk = min(4, qt + 1 - k0)
                            for k4 in range(nk):
                                g.mm(PS[4][:, k4 * 128:(k4 + 1) * 128], lsel[:, k0 + k4, :], selT[:], True, True, [lsel, selT], [PS[4]])
                            g.cp(maskT[:, k0:k0 + nk, :], PS[4][:, 0:nk * 128].rearrange("p (k q) -> p k q", q=128), [PS[4]], [maskT],
                                 eng="act")
                        g.tt(maskT[:, qt, :], maskT[:, qt, :], tri_le[:], ALU.mult, [maskT, tri_le], [maskT])
                        rounds = []
                        wks = list(range(max(0, qt - 4), qt + 1))
                        for r in range(6):
                            for k0 in range(0, qt + 1, 4):
                                rounds.append(("slc", r, list(range(k0, min(k0 + 4, qt + 1))), False))
                            for c0 in range(0, len(wks), 4):
                                rounds.append(("win", r, wks[c0:c0 + 4], c0 + 4 >= len(wks)))
                        SBK = [PS[2], PS[3], PS[7]]
                        LOOK = 2

                        def emit_scores(i):
                            kind, r, kts, _ = rounds[i]
                            pS = SBK[i % 3]
                            kT = ksT if kind == "slc" else kwT
                            for k4, kt in enumerate(kts):
                                g.mm(pS[:, k4 * 128:(k4 + 1) * 128], kT[gs, kt * 128:(kt + 1) * 128], qT[gs, r, qs], True, True,
                                     [kT, qT], [pS])

                        def emit_rest(i):
                            kind, r, kts, last = rounds[i]
                            pS = SBK[i % 3]
                            e_t = es[i % 3]
                            p_t = pTt[i % 3]
                            nk = len(kts)
                            pOW = PS[5 + (r % 2)]
                            g.act(e_t[:, 0:nk, :], pS[:, 0:nk * 128].rearrange("p (k q) -> p k q", q=128), AF.Exp, [pS], [e_t], scale=SCALE)
                            if kind == "slc":
                                g.tt(p_t[:, 0:nk, :], e_t[:, 0:nk, :], maskT[:, kts[0]:kts[0] + nk, :], ALU.mult, [e_t, maskT], [p_t])
                                for k4, kt in enumerate(kts):
                                    g.mm(pOW[:, 0:65], p_t[:, k4, :], v_aug[:, kt, 0, gg, :], kt == 0, kt == qt, [p_t, v_aug], [pOW],
                                         signal=True)
                            else:
                                for k4, kt in enumerate(kts):
                                    if kt == qt:
                                        g.tt(e_t[:, k4, :], e_t[:, k4, :], tri_le[:], ALU.mult, [e_t, tri_le], [e_t])
                                    elif kt == qt - 4:
                                        g.tt(e_t[:, k4, :], e_t[:, k4, :], tri_gt[:], ALU.mult, [e_t, tri_gt], [e_t])
                                for k4, kt in enumerate(kts):
                                    g.mm(pOW[:, 65:130], e_t[:, k4, :], v_aug[:, kt, 1, gg, :], kt == wks[0], kt == qt, [e_t, v_aug], [pOW],
                                         signal=True)
                            if last:
                                pending.append([r, pOW, 2])

                        def emit_combine(r, pOW):
                            if True:
                                g.ts(rs2[:], pOW[:, 0:130].rearrange("p (b c) -> p b c", c=65)[:, :, 64], 1e-30, None, ALU.max, None,
                                     [pOW], [rs2])
                                g.emit("dve", lambda e: e.reciprocal(out=rs2[:], in_=rs2[:]), reads=[rs2], writes=[rs2])
                                gi0 = gg * 6 + r
                                g.tt(cf[:, 0:1], rsc[:, r:r + 1], sig[:, qt, gi0:gi0 + 1], ALU.mult, [rsc, sig], [cf])
                                g.tt(cf[:, 1:2], rs2[:, 0:1], sig[:, qt, 12 + gi0:13 + gi0], ALU.mult, [rs2, sig], [cf])
                                g.tt(cf[:, 2:3], rs2[:, 1:2], sig[:, qt, 24 + gi0:25 + gi0], ALU.mult, [rs2, sig], [cf])
                                oc = o_sb[:, gg * 384 + r * 64:gg * 384 + (r + 1) * 64]
                                g.ts(oc, PS[r // 3][:, (r % 3) * 97:(r % 3) * 97 + 64], cf[:, 0:1], None, ALU.mult, None,
                                     [PS[r // 3], cf], [o_sb])
                                g.stt(oc, pOW[:, 0:64], cf[:, 1:2], oc, ALU.mult, ALU.add, [pOW, cf, o_sb], [o_sb])
                                g.stt(oc, pOW[:, 65:129], cf[:, 2:3], oc, ALU.mult, ALU.add, [pOW, cf, o_sb], [o_sb])

                        nr = len(rounds)
                        pending = []
                        for i in range(min(LOOK, nr)):
                            emit_scores(i)
                        for i in range(nr):
                            if i + LOOK < nr:
                                emit_scores(i + LOOK)
                            emit_rest(i)
                            for pd in list(pending):
                                pd[2] -= 1
                                if pd[2] < 0:
                                    emit_combine(pd[0], pd[1])
                                    pending.remove(pd)
                        for pd in pending:
                            emit_combine(pd[0], pd[1])
                        pending = []
                    g.tt(osq[:], o_sb[:], o_sb[:], ALU.mult, [o_sb], [osq])
                    g.emit("dve", lambda e: e.reduce_sum(out=rn[:], in_=osq[:], axis=AX.X), reads=[osq], writes=[rn])
                    g.ts(rn[:], rn[:], 1.0 / 768.0, EPS, ALU.mult, ALU.add, [rn], [rn])
                    g.rsqrt(rn[:], [rn])
                    for hf in range(2):
                        pb = PS[6 + hf]
                        nb_ = 4 if hf == 0 else 2
                        for k4 in range(nb_):
                            blk = hf * 4 + k4
                            g.tr(pb[:, k4 * 128:(k4 + 1) * 128], o_sb[:, blk * 128:(blk + 1) * 128], ident[:], [o_sb, ident], [pb])
                        for k4 in range(nb_):
                            blk = hf * 4 + k4
                            g.act(oT[:, blk, :], pb[:, k4 * 128:(k4 + 1) * 128], AF.Copy, [pb, g_nsaT], [oT], scale=g_nsaT[:, blk:blk + 1])
                    for hf in range(2):
                        cs_ = slice(hf * 512, (hf + 1) * 512)
                        pc = PS[0 + hf]
                        pn = PS[2 + hf]
                        for blk in range(2):
                            g.mm(pc[:, :], ycT[:, blk, qs], w_o_sb[:, blk, cs_], blk == 0, blk == 1, [ycT, w_o_sb], [pc])
                        for blk in range(6):
                            g.mm(pn[:, :], oT[:, blk, :], w_o_sb[:, 2 + blk, cs_], blk == 0, blk == 5, [oT, w_o_sb], [pn])
                        g.ts(t1[:, cs_], pc[:, :], rc[:, qt:qt + 1], None, ALU.mult, None, [pc, rc], [t1])
                        g.stt(t1[:, cs_], pn[:, :], rn[:, 0:1], t1[:, cs_], ALU.mult, ALU.add, [pn, rn, t1], [t1])
                    g.tt(t1[:], t1[:], gate_p[:, 0, :], ALU.mult, [t1, gate_p], [t1])
                    g.ld("sp", xt2[:], I["xp"][qs, :], [], [xt2])
                    g.stt(t1[:], xt2[:], ALPHA, t1[:], ALU.mult, ALU.add, [xt2, t1], [t1])
                    layernorm((t1[:], t1), 0, (x1g[:, t4, :], x1g), stats, mv, lnbc)
                    for hf in range(2):
                        pb = PS[6 + hf]
                        for k4 in range(4):
                            kc = hf * 4 + k4
                            g.tr(pb[:, k4 * 128:(k4 + 1) * 128], x1g[:, t4, kc * 128:(kc + 1) * 128], ident[:], [x1g, ident], [pb])
                        for k4 in range(4):
                            kc = hf * 4 + k4
                            g.act(h2T[:, kc, t4 * 128:(t4 + 1) * 128], pb[:, k4 * 128:(k4 + 1) * 128], AF.Identity,
                                  [pb, ops2, modT], [h2T], bias=modT[:, 24 + kc, 0:1], scale=ops2[:, kc, 0:1])
                for ffc in range(NFF):
                    wg_t = wgc[ffc % 2]
                    wu_t = wuc[ffc % 2]
                    fs = slice(ffc * 128, (ffc + 1) * 128)
                    g.ld("sp", wg_t[:].rearrange("p k n -> p (k n)"), wscr["wg_bf"][ffc], [WSC["wg_bf"][ffc]], [wg_t])
                    g.ld("pool", wu_t[:].rearrange("p k n -> p (k n)"), wscr["wu_bf"][ffc], [WSC["wu_bf"][ffc]], [wu_t])
                    pG = PS[(2 * ffc) % 8]
                    pU = PS[(2 * ffc + 1) % 8]
                    for kc in range(8):
                        g.mm(pG[:, :], wg_t[:, kc, :], h2T[:, kc, :], kc == 0, kc == 7, [wg_t, h2T], [pG])
                    for kc in range(8):
                        g.mm(pU[:, :], wu_t[:, kc, :], h2T[:, kc, :], kc == 0, kc == 7, [wu_t, h2T], [pU])
                    g.act(sg_sb[:], pG[:, :], AF.Silu, [pG], [sg_sb])
                    g.tt(aT[:, ffc, :], sg_sb[:], pU[:, :], ALU.mult, [sg_sb, pU], [aT])
                for ffc in range(NFF):
                    wd_t = wdc[ffc % 2]
                    g.ld("sp" if ffc % 2 else "pool", wd_t[:], wscr["wd_bf"][ffc], [WSC["wd_bf"][ffc]], [wd_t])
                    for t4 in range(4):
                        for hf in range(2):
                            g.mm(PS[t4 * 2 + hf][:, :], aT[:, ffc, t4 * 128:(t4 + 1) * 128], wd_t[:, hf * 512:(hf + 1) * 512],
                                 ffc == 0, ffc == NFF - 1, [aT, wd_t], [PS[t4 * 2 + hf]], signal=True)
                for t4 in range(4):
                    qt = grp * 4 + t4
                    for hf in range(2):
                        cs_ = slice(hf * 512, (hf + 1) * 512)
                        g.tt(t1[:, cs_], PS[t4 * 2 + hf][:, :], gate_p[:, 1, cs_], ALU.mult, [PS[t4 * 2 + hf], gate_p], [t1])
                    g.stt(t1[:], x1g[:, t4, :], ALPHA, t1[:], ALU.mult, ALU.add, [x1g, t1], [t1])
                    layernorm((t1[:], t1), 2, (xt2[:], xt2), stats, mv, lnbc)
                    g.ld("sp", O["y_p"][qt * 128:(qt + 1) * 128, :], xt2[:], [xt2], [], is_output=True)
            g.barrier()
            stD.close()

        g.finish()
        stC.close()
    return nc


_NC_CACHE = {}


def _perm_w_in(w):
    w = w.copy()
    q = w[:, 768:1536].reshape(D, 2, 6, 64).transpose(0, 2, 1, 3).reshape(D, 768)
    w[:, 768:1536] = q
    return w


def kernel(x_prompt, x_sample, cache_cmp_kv, cache_slc_kv, cache_win_kv, state_conv, page_table,
           c_prompt, c_sample, w_ada, b_ada, w_in, conv_w, cmp_pe, cmp_w1, cmp_b1, cmp_w2,
           g_conv_out, g_nsa_out, w_o, ln1_g, ln1_b, ln2_g, ln2_b, w_ffn_gate, w_ffn_up, w_ffn_down,
           _cores=None):
    f = lambda a: np.ascontiguousarray(np.asarray(a, dtype=np.float32))
    if "nc" not in _NC_CACHE:
        _NC_CACHE["nc"] = build_nc()
    nc = _NC_CACHE["nc"]
    consts = host_constants()
    cores = list(range(N_CORES)) if _cores is None else _cores
    shared = {
        "cmp_pool": f(cache_cmp_kv).reshape(NPHYS * 8, 4096), "slc_pool": f(cache_slc_kv).reshape(NPHYS * 8, 4096),
        "w_ada": f(w_ada)[0], "b_ada": f(b_ada)[0], "w_in": _perm_w_in(f(w_in)[0]), "conv_wT": np.ascontiguousarray(f(conv_w)[0].reshape(3, 2, 128).transpose(2, 1, 0)),
        "b_adaT": np.ascontiguousarray(f(b_ada)[0].reshape(48, 128).T),
        "peT": np.ascontiguousarray(np.tile(f(cmp_pe)[0].transpose(2, 0, 1).reshape(64, 64), (2, 1))),
        "w1rep": np.ascontiguousarray(np.tile(f(cmp_w1)[0].transpose(2, 0, 1, 3).reshape(64, 2 * 32 * 128), (2, 1))), "b1T": np.ascontiguousarray(f(cmp_b1)[0].T), "w2k": np.ascontiguousarray(np.tile(f(cmp_w2)[0][0], (1, 2))), "w2v": f(cmp_w2)[0][1],
        "g_convT": np.ascontiguousarray(f(g_conv_out)[0].reshape(2, 128).T), "g_nsaT": np.ascontiguousarray(f(g_nsa_out)[0].reshape(6, 128).T), "w_o": f(w_o)[0],
        "ln1_g": f(ln1_g)[0], "ln1_b": f(ln1_b)[0], "ln2_g": f(ln2_g)[0], "ln2_b": f(ln2_b)[0],
        "w_g": f(w_ffn_gate)[0], "w_u": f(w_ffn_up)[0], "w_d": f(w_ffn_down)[0],
    }
    for k, v in consts.items():
        shared["k_" + k] = v
    xp = f(x_prompt)
    xs = f(x_sample)
    win = f(cache_win_kv)[0].reshape(32, 512, 256)
    sconv = f(state_conv)[0]
    pt = np.ascontiguousarray(np.asarray(page_table, dtype=np.int32))
    cp = f(c_prompt)
    cs = f(c_sample)
    in_maps = []
    for c in cores:
        m = dict(shared)
        m["xp"] = xp[c]
        m["xs"] = xs[4 * c:4 * c + 4].reshape(32, D)
        m["win"] = win[4 * c:4 * c + 4]
        m["sconvT"] = np.ascontiguousarray(sconv[4 * c:4 * c + 4].reshape(4, 2, 2, 128).transpose(3, 2, 0, 1))
        m["ptT"] = np.ascontiguousarray(pt[4 * c:4 * c + 4].T)
        m["cvecT"] = np.ascontiguousarray(np.concatenate([cp[c:c + 1], cs[4 * c:4 * c + 4]], axis=0).reshape(5, 8, 128).transpose(2, 1, 0))
        in_maps.append(m)
    res = run_bass_kernel_spmd(nc, in_maps, core_ids=list(range(len(cores))))
    R = res.results
    n = len(cores)
    y_p = np.stack([R[i]["y_p"] for i in range(n)])
    y_s = np.concatenate([R[i]["y_s"].reshape(4, 8, D) for i in range(n)])
    cmp_p = np.stack([R[i]["cmp_p"].reshape(SEQ, 2, 2, 64) for i in range(n)])[None]
    slc_p = np.stack([R[i]["slc_p"].reshape(SEQ, 2, 2, 64) for i in range(n)])[None]
    win_p = np.stack([R[i]["win_p"].reshape(512, 2, 2, 64) for i in range(n)])[None]
    conv_p = np.stack([R[i]["conv_p"].transpose(2, 1, 0).reshape(2, 256) for i in range(n)])[None]
    cmp_s = np.concatenate([R[i]["cmp_s"].reshape(4, 8, 2, 2, 64) for i in range(n)])[None]
    slc_s = np.concatenate([R[i]["slc_s"].reshape(4, 8, 2, 2, 64) for i in range(n)])[None]
    win_s = np.concatenate([R[i]["win_s"].reshape(4, 512, 2, 2, 64) for i in range(n)])[None]
    conv_s = np.concatenate([R[i]["conv_s"].transpose(2, 3, 1, 0).reshape(4, 2, 256) for i in range(n)])[None]
    return (y_p, y_s, cmp_p, slc_p, win_p, conv_p, cmp_s, slc_s, win_s, conv_s)
```

```python
import numpy as np
from contextlib import ExitStack
import concourse.bass as bass
import concourse.mybir as mybir
from concourse.bass_utils import run_bass_kernel_spmd

F32 = mybir.dt.float32
BF16 = mybir.dt.bfloat16
I32 = mybir.dt.int32
AF = mybir.ActivationFunctionType
ALU = mybir.AluOpType
AX = mybir.AxisListType

D = 1024
SEQ = 2048
NT = 16
NPAGE = 128
NPHYS = 5120
PROJ = 2340
DFF = 2816
NFF = 22
ALPHA = 2 ** 0.25
EPS = 1e-5
SCALE = 0.125
N_CORES = 8
STAGE = 9
DO_PROMPT = 1
SUB = 9
DBG = 0


class T:
    def __init__(self, handle, name, space):
        self.h = handle
        self.name = name
        self.space = space
        self.w = None
        self.r = {}
        self.dsem = None
        self.dcount = 0

    def __getitem__(self, idx):
        return self.h[idx]


class Gen:
    ENG = ("pe", "act", "dve", "pool", "sp")

    def __init__(self, nc, stack):
        self.nc = nc
        self.stack = stack
        self.ops = {e: [] for e in self.ENG}
        self.cnt = {e: 0 for e in self.ENG}
        self.seen = {e: {} for e in self.ENG}
        self.sems = {}
        self.cur = {}
        for e in self.ENG[:4]:
            self.sems[e] = self.stack.enter_context(self.nc.semaphore("c_" + e))
        self.out_marks = {}

    def sb(self, name, shape, dtype=F32, stack=None):
        h = (stack or self.stack).enter_context(self.nc.sbuf_tensor("s_" + name, list(shape), dtype))
        return T(h, name, "sbuf")

    def ps(self, name, shape, dtype=F32):
        h = self.stack.enter_context(self.nc.psum_tensor("p_" + name, list(shape), dtype))
        return T(h, name, "psum")

    def _wait(self, e, k, v):
        if e == "pe" and k == "pe":
            return
        if self.seen[e].get(k, 0) < v:
            self.seen[e][k] = v
            sem = self.sems[k]
            self.ops[e].append(lambda eng, sem=sem, v=v: eng.wait_ge(sem, v))

    def _deps(self, e, reads, writes):
        deps = {}
        for t in reads:
            if t.w is not None:
                k, v = t.w
                deps[k] = max(deps.get(k, 0), v)
        for t in writes:
            if t.w is not None:
                k, v = t.w
                deps[k] = max(deps.get(k, 0), v)
            for k, v in t.r.items():
                deps[k] = max(deps.get(k, 0), v)
        for k, v in deps.items():
            self._wait(e, k, v)

    def _mark(self, key, val, reads, writes):
        self.cur[key] = max(self.cur.get(key, 0), val)
        for t in reads:
            t.r[key] = max(t.r.get(key, 0), val)
        for t in writes:
            t.w = (key, val)
            t.r = {}

    def emit(self, e, fn, reads=(), writes=(), signal=True):
        self._deps(e, reads, writes)
        if signal:
            self.cnt[e] += 1
            sem = self.sems[e]
            self.ops[e].append(lambda eng, fn=fn, sem=sem: fn(eng).then_inc(sem, 1))
            val = self.cnt[e]
        else:
            self.ops[e].append(lambda eng, fn=fn: fn(eng))
            val = self.cnt[e] + 1
        self._mark(e, val, reads, writes)

    def dma(self, q, fn, reads=(), writes=(), track=None, is_output=False):
        if track is None:
            for t in list(writes) + list(reads):
                if t.space == "sbuf":
                    track = t
                    break
            if track is None:
                track = (list(writes) + list(reads))[0]
        if track.dsem is None:
            key = "d_" + track.name
            assert key not in self.sems, key
            self.sems[key] = self.stack.enter_context(self.nc.semaphore(key))
            track.dsem = key
        key = track.dsem
        self._deps(q, reads, writes)
        track.dcount += 1
        val = 16 * track.dcount
        sem = self.sems[key]
        self.ops[q].append(lambda eng, fn=fn, sem=sem: fn(eng).then_inc(sem, 16))
        self._mark(key, val, reads, writes)
        if is_output:
            self.out_marks[key] = max(self.out_marks.get(key, 0), val)

    def barrier(self):
        for e in self.ENG:
            for k, v in list(self.cur.items()):
                self._wait(e, k, v)

    def mm(self, out, lhsT, rhs, start, stop, reads, writes, signal=None):
        if signal is None:
            signal = stop
        self.emit("pe", lambda e: e.matmul(out, lhsT, rhs, start=start, stop=stop),
                  reads=reads, writes=writes, signal=signal)

    def tr(self, out, in_, ident, reads, writes):
        self.emit("pe", lambda e: e.transpose(out, in_, ident), reads=reads, writes=writes)

    def act(self, out, in_, func, reads, writes, bias=None, scale=None, accum_out=None):
        kw = {}
        if bias is not None:
            kw["bias"] = bias
        if scale is not None:
            kw["scale"] = scale
        if accum_out is not None:
            kw["accum_out"] = accum_out
        self.emit("act", lambda e: e.activation(out=out, in_=in_, func=func, **kw), reads=reads, writes=writes)

    def tt(self, out, in0, in1, op, reads, writes, eng="dve"):
        self.emit(eng, lambda e: e.tensor_tensor(out=out, in0=in0, in1=in1, op=op), reads=reads, writes=writes)

    def ts(self, out, in0, s1, s2, op0, op1, reads, writes, eng="dve"):
        if op1 is None:
            self.emit(eng, lambda e: e.tensor_scalar(out=out, in0=in0, scalar1=s1, scalar2=None, op0=op0),
                      reads=reads, writes=writes)
        else:
            self.emit(eng, lambda e: e.tensor_scalar(out=out, in0=in0, scalar1=s1, scalar2=s2, op0=op0, op1=op1),
                      reads=reads, writes=writes)

    def stt(self, out, in0, scalar, in1, op0, op1, reads, writes, eng="dve"):
        self.emit(eng, lambda e: e.scalar_tensor_tensor(out=out, in0=in0, scalar=scalar, in1=in1, op0=op0, op1=op1),
                  reads=reads, writes=writes)

    def cp(self, out, in_, reads, writes, eng="dve"):
        if eng == "act":
            self.act(out, in_, AF.Copy, reads, writes)
        else:
            self.emit(eng, lambda e: e.tensor_copy(out=out, in_=in_), reads=reads, writes=writes)

    def rsqrt(self, ap, tiles):
        self.act(ap, ap, AF.Sqrt, tiles, tiles)
        self.emit("dve", lambda e: e.reciprocal(out=ap, in_=ap), reads=tiles, writes=tiles)

    def memset(self, ap, val, writes, eng="dve"):
        self.emit(eng, lambda e: e.memset(ap, val), writes=writes)

    def ld(self, q, out, in_, reads, writes, nc_ok=False, is_output=False, track=None):
        if nc_ok:
            self.dma(q, lambda e: e.dma_start(out=out, in_=in_, allow_slow_non_contiguous=True),
                     reads=reads, writes=writes, is_output=is_output, track=track)
        else:
            self.dma(q, lambda e: e.dma_start(out=out, in_=in_), reads=reads, writes=writes,
                     is_output=is_output, track=track)

    def finish(self):
        for k, v in self.out_marks.items():
            self._wait("sp", k, v)
        for e in ("pe", "act", "dve"):
            self._wait("sp", e, self.cnt[e])
        with self.nc.Block() as block:
            @block.tensor
            def _(eng):
                for f in self.ops["pe"]:
                    f(eng)

            @block.scalar
            def _(eng):
                for f in self.ops["act"]:
                    f(eng)

            @block.vector
            def _(eng):
                for f in self.ops["dve"]:
                    f(eng)

            @block.gpsimd
            def _(eng):
                for f in self.ops["pool"]:
                    f(eng)

            @block.sync
            def _(eng):
                for f in self.ops["sp"]:
                    f(eng)


def host_constants():
    c = {}
    c["ident"] = np.eye(128, dtype=np.float32)
    s = np.arange(128)[:, None]
    q = np.arange(128)[None, :]
    c["tri_le"] = (s <= q).astype(np.float32)
    c["tri_gt"] = (s > q).astype(np.float32)
    i = np.arange(128)[:, None]
    qq = np.arange(SEQ)[None, :]
    cm = ((16 * i + 31) <= qq).astype(np.float32)
    cm[127, :] = 0.0
    c["cmpmask"] = cm
    qpos = (np.arange(NT)[None, :, None] * 128 + np.arange(128)[:, None, None])
    j = np.arange(32)[None, None, :]
    cur = qpos // 64
    forced = (j == 0) | (j == cur) | (j == cur - 1)
    c["force"] = np.where(forced, 1e9 * (1.0 + j / 64.0), 0.0).astype(np.float32)
    c["validneg"] = np.where(j * 64 <= qpos, 0.0, -1e30).astype(np.float32)
    L = np.zeros((32, NT, 128), np.float32)
    for kt in range(NT):
        L[2 * kt, kt, :64] = 1.0
        L[2 * kt + 1, kt, 64:] = 1.0
    c["lsel"] = L
    A = np.zeros((128, 32), np.float32)
    for jj in range(32):
        for ii in range(4 * jj - 1, 4 * jj + 4):
            if 0 <= ii <= 126:
                A[ii, jj] = 1.0
    c["acmp"] = A
    As = np.zeros((128, 8, 257), np.float32)
    for jj in range(257):
        for ii in range(4 * jj - 1, 4 * jj + 4):
            if 0 <= ii <= 1022:
                As[ii % 128, ii // 128, jj] = 1.0
    c["as_"] = As
    fs = np.zeros((8, 257), np.float32)
    for jj in (0, 255, 256):
        fs[:, jj] = 1e9 * (1.0 + jj / 1024.0)
    c["force_s"] = fs
    qi = np.arange(48)
    tq = qi % 8
    c["gsum"] = (tq[:, None] == np.arange(8)[None, :]).astype(np.float32)
    c["gt"] = (np.arange(8)[:, None] == tq[None, :]).astype(np.float32)
    c["causal_new"] = (np.arange(8)[:, None] <= tq[None, :]).astype(np.float32)
    c["mw0"] = (np.arange(128)[:, None] > tq[None, :]).astype(np.float32)
    return c


CONST_SHAPES = {"as_": [128, 8, 257], "force_s": [8, 257], "gsum": [48, 8], "gt": [8, 48], "causal_new": [8, 48],
                "mw0": [128, 48], "ident": [128, 128], "tri_le": [128, 128], "tri_gt": [128, 128], "cmpmask": [128, SEQ],
                "force": [128, NT, 32], "validneg": [128, NT, 32], "lsel": [32, NT, 128], "acmp": [128, 32]}

IN_SHAPES = {
    "xp": ([SEQ, D], F32), "xs": ([32, D], F32),
    "cmp_pool": ([NPHYS * 8, 4096], F32), "slc_pool": ([NPHYS * 8, 4096], F32),
    "win": ([4, 512, 256], F32), "sconvT": ([128, 2, 4, 2], F32), "ptT": ([128, 4], I32),
    "cvecT": ([128, 8, 5], F32),
    "w_ada": ([D, 6 * D], F32), "b_ada": ([6 * D], F32), "b_adaT": ([128, 48], F32), "w_in": ([D, PROJ], F32),
    "conv_wT": ([128, 2, 3], F32), "peT": ([128, 64], F32), "w1rep": ([128, 2 * 32 * 128], F32),
    "b1T": ([128, 2], F32), "w2k": ([128, 128], F32), "w2v": ([128, 64], F32), "g_convT": ([128, 2], F32), "g_nsaT": ([128, 6], F32),
    "w_o": ([D, D], F32), "ln1_g": ([D], F32), "ln1_b": ([D], F32), "ln2_g": ([D], F32), "ln2_b": ([D], F32),
    "w_g": ([D, DFF], F32), "w_u": ([D, DFF], F32), "w_d": ([DFF, D], F32),
}
OUT_SHAPES = {
    "y_p": [SEQ, D], "y_s": [32, D], "cmp_p": [SEQ, 256], "slc_p": [SEQ, 256], "win_p": [512, 256],
    "conv_p": [128, 2, 2], "cmp_s": [32, 256], "slc_s": [32, 256], "win_s": [4, 512, 256], "conv_s": [128, 2, 4, 2],
}


def build_nc(do_attn=True, do_post=True):
    nc = bass.Bass("TRN2", target_bir_lowering=False)
    I = {}
    for k, (shp, dt) in IN_SHAPES.items():
        I[k] = nc.dram_tensor(k, shp, dt, kind="ExternalInput").ap()
    for k, shp in CONST_SHAPES.items():
        I[k] = nc.dram_tensor("k_" + k, shp, F32, kind="ExternalInput").ap()
    O = {}
    for k, shp in OUT_SHAPES.items():
        O[k] = nc.dram_tensor(k, shp, F32, kind="ExternalOutput").ap()

    if DBG:
        for k, shp in {"dbg_o": [32, 768], "dbg_oc": [4, 48, 130], "dbg_os": [4, 48, 130], "dbg_ow": [4, 48, 130],
                       "dbg_sc": [4, 2, 8, 257]}.items():
            O[k] = nc.dram_tensor(k, shp, F32, kind="ExternalOutput").ap()
    wscr = {}
    for nm in ("wg_bf", "wu_bf", "wd_bf"):
        wscr[nm] = nc.dram_tensor(nm, [NFF, 128, 1024], BF16, kind="Internal").ap()
    with ExitStack() as st:
        g = Gen(nc, st)
        WSC = {nm: [T(None, "%s_t%d" % (nm, i), "dram") for i in range(NFF)] for nm in wscr}
        odram = T(None, "odram", "dram")

        PS = [g.ps("ps%d" % i, [128, 512], F32) for i in range(8)]


        modT = g.sb("modT", [128, 48, 5], F32)
        gate_p = g.sb("gate_p", [128, 2, D], F32)
        ops1 = g.sb("ops1", [128, 8, 5], F32)
        ops2 = g.sb("ops2", [128, 8, 5], F32)
        qT = g.sb("qT", [128, 6, SEQ], BF16)
        ksT = g.sb("ksT", [128, SEQ], BF16)
        kwT = g.sb("kwT", [128, SEQ], BF16)
        ycT = g.sb("ycT", [128, 2, SEQ], BF16)
        v_aug = g.sb("v_aug", [128, NT, 2, 2, 65], BF16)
        sig = g.sb("sig", [128, NT, 36], F32)
        rc = g.sb("rc", [128, NT], F32)

        ident = g.sb("ident", [128, 128], F32)
        g.ld("sp", ident[:], I["ident"], [], [ident])
        tri_le = g.sb("tri_le", [128, 128], BF16)
        tri_gt = g.sb("tri_gt", [128, 128], BF16)
        g.ld("pool", tri_le[:], I["tri_le"], [], [tri_le])
        g.ld("pool", tri_gt[:], I["tri_gt"], [], [tri_gt])
        cmpmask = g.sb("cmpmask", [128, SEQ], BF16)
        g.ld("pool", cmpmask[:], I["cmpmask"], [], [cmpmask])
        force = g.sb("force", [128, NT, 32], F32)
        validneg = g.sb("validneg", [128, NT, 32], F32)
        g.ld("sp", force[:], I["force"], [], [force])
        g.ld("sp", validneg[:], I["validneg"], [], [validneg])
        lsel = g.sb("lsel", [32, NT, 128], BF16)
        g.ld("pool", lsel[:], I["lsel"], [], [lsel])
        ones_f = g.sb("ones_f", [128, 1], F32)
        g.memset(ones_f[:], 1.0, [ones_f])

        b_adaT = g.sb("b_adaT", [128, 48], F32)
        g.ld("sp", b_adaT[:], I["b_adaT"], [], [b_adaT])
        conv_wT = g.sb("conv_wT", [128, 2, 3], F32)
        g.ld("sp", conv_wT[:], I["conv_wT"], [], [conv_wT])
        g_convT = g.sb("g_convT", [128, 2], F32)
        g.ld("sp", g_convT[:], I["g_convT"], [], [g_convT])
        g_nsaT = g.sb("g_nsaT", [128, 6], F32)
        g.ld("sp", g_nsaT[:], I["g_nsaT"], [], [g_nsaT])
        b1T = g.sb("b1T", [128, 2], F32)
        g.ld("sp", b1T[:], I["b1T"], [], [b1T])
        stC = ExitStack()
        kcT = g.sb("kcT", [128, 2, SEQ], BF16, stack=stC)
        stS = ExitStack()
        ycTs = g.sb("ycTs", [128, 2, 32], BF16, stack=stS)
        ysqs = g.sb("ysqs", [128, 2, 32], F32, stack=stS)
        gate_s = g.sb("gate_s", [32, 2, D], F32, stack=stS)
        Ps = g.sb("Ps", [32, PROJ], F32, stack=stS)
        sigS = g.sb("sigS", [32, 36], F32, stack=stS)
        qTs = g.sb("qTs", [128, 4, 6, 8], BF16, stack=stS)
        ksTs = g.sb("ksTs", [128, 32], BF16, stack=stS)
        kwTs = g.sb("kwTs", [128, 32], BF16, stack=stS)
        Pv = g.sb("Pv", [32, 2, 128], BF16, stack=stS)
        vnew = g.sb("vnew", [8, 4, 2, 2, 65], BF16, stack=stS)
        gq = g.sb("gq", [48, 4, 2, 3], F32, stack=stS)
        rc_s = g.sb("rc_s", [32, 1], F32, stack=stS)
        o_tok = g.sb("o_tok", [32, 768], F32, stack=stS)
        stA = ExitStack()
        bgate = g.sb("bgate", [128, 2, D], F32, stack=stA)
        for i, m in enumerate((2, 5)):
            g.ld("sp", bgate[:, i, :], I["b_ada"][m * D:(m + 1) * D].rearrange("(o d) -> o d", o=1).to_broadcast([128, D]),
                 [], [bgate])

        cT = g.sb("cT", [128, 8, 5], F32, stack=stA)
        g.ld("sp", cT[:], I["cvecT"], [], [cT])
        scT = g.sb("scT", [128, 8, 5], BF16, stack=stA)
        g.act(scT[:], cT[:], AF.Silu, [cT], [scT])
        rep_p = g.sb("rep_p", [128, 8, 128], BF16, stack=stA)
        g.cp(rep_p[:], scT[:, :, 0:1].to_broadcast([128, 8, 128]), [scT], [rep_p])
        rep_s = g.sb("rep_s", [128, 8, 32], BF16, stack=stA)
        for b in range(4):
            g.cp(rep_s[:, :, b * 8:(b + 1) * 8], scT[:, :, 1 + b:2 + b].to_broadcast([128, 8, 8]), [scT], [rep_s])
        wa = [g.sb("wa%d" % i, [128, 8, 512], BF16, stack=stA) for i in range(2)]
        w_ada_v = I["w_ada"].rearrange("(k p) n -> p k n", p=128)
        for nb in range(12):
            w = wa[nb % 2]
            g.ld("pool", w[:, :, :], w_ada_v[:, :, nb * 512:(nb + 1) * 512], [], [w])
            pm = PS[nb % 2]
            for sub in range(4):
                for kc in range(8):
                    g.mm(pm[:, sub * 5:(sub + 1) * 5], w[:, kc, sub * 128:(sub + 1) * 128], scT[:, kc, :],
                         kc == 0, kc == 7, [w, scT], [pm])
            g.tt(modT[:, nb * 4:(nb + 1) * 4, :], pm[:, 0:20].rearrange("p (s m) -> p s m", m=5),
                 b_adaT[:, nb * 4:(nb + 1) * 4].rearrange("p (s o) -> p s o", o=1).to_broadcast([128, 4, 5]), ALU.add,
                 [pm, b_adaT], [modT])
            if nb in (4, 5, 10, 11):
                gi = 0 if nb < 6 else 1
                half = nb % 2
                pg = PS[2 + nb % 2]
                for kc in range(8):
                    g.mm(pg[:, :], rep_p[:, kc, :], w[:, kc, :], kc == 0, kc == 7, [rep_p, w], [pg])
                g.tt(gate_p[:, gi, half * 512:(half + 1) * 512], pg[:, :], bgate[:, gi, half * 512:(half + 1) * 512],
                     ALU.add, [pg, bgate], [gate_p])
                pg2 = PS[4 + nb % 2]
                for kc in range(8):
                    g.mm(pg2[0:32, :], rep_s[:, kc, :], w[:, kc, :], kc == 0, kc == 7, [rep_s, w], [pg2])
                g.tt(gate_s[:, gi, half * 512:(half + 1) * 512], pg2[0:32, :], bgate[0:32, gi, half * 512:(half + 1) * 512],
                     ALU.add, [pg2, bgate], [gate_s])
        g.ts(ops1[:], modT[:, 8:16, :], 1.0, None, ALU.add, None, [modT], [ops1])
        g.ts(ops2[:], modT[:, 32:40, :], 1.0, None, ALU.add, None, [modT], [ops2])

        g.barrier()
        stA.close()
        stB = ExitStack()
        w_in_sb = g.sb("w_in_sb", [128, 8, PROJ], BF16, stack=stB)
        w_in_v = I["w_in"].rearrange("(k p) n -> p k n", p=128)
        w_in_k = [T(None, "w_in_k%d" % kc, "sbuf") for kc in range(8)]
        for kc in range(8):
            g.ld("pool", w_in_sb[:, kc, :], w_in_v[:, kc, :], [], [w_in_k[kc]])
        hT = g.sb("hT", [128, 8, SEQ], BF16, stack=stB)
        hT_t = [T(None, "hT_t%d" % i, "sbuf") for i in range(NT)]
        xin = [g.sb("xin%d" % i, [128, D], F32, stack=stB) for i in range(1)]
        for tt in range(NT):
            xt = xin[0]
            g.ld("sp", xt[:], I["xp"][tt * 128:(tt + 1) * 128, :], [], [xt])
            for hf in range(2):
                pb = PS[hf]
                for k4 in range(4):
                    kc = hf * 4 + k4
                    g.tr(pb[:, k4 * 128:(k4 + 1) * 128], xt[:, kc * 128:(kc + 1) * 128], ident[:], [xt, ident], [pb])
                for k4 in range(4):
                    kc = hf * 4 + k4
                    g.act(hT[:, kc, tt * 128:(tt + 1) * 128], pb[:, k4 * 128:(k4 + 1) * 128], AF.Identity,
                          [pb, ops1, modT], [hT_t[tt]], bias=modT[:, kc, 0:1], scale=ops1[:, kc, 0:1])

        hTs = g.sb("hTs", [128, 8, 32], BF16, stack=stB)
        xs_sb = g.sb("xs_sb", [32, D], F32, stack=stB)
        g.ld("sp", xs_sb[:], I["xs"], [], [xs_sb])
        for hf in range(2):
            pb = PS[2 + hf]
            for k4 in range(4):
                kc = hf * 4 + k4
                g.tr(pb[:, k4 * 32:(k4 + 1) * 32], xs_sb[:, kc * 128:(kc + 1) * 128], ident[0:32, 0:32], [xs_sb, ident], [pb])
            for k4 in range(4):
                kc = hf * 4 + k4
                for b in range(4):
                    g.act(hTs[:, kc, b * 8:(b + 1) * 8], pb[:, k4 * 32 + b * 8:k4 * 32 + (b + 1) * 8], AF.Identity,
                          [pb, ops1, modT], [hTs], bias=modT[:, kc, 1 + b:2 + b], scale=ops1[:, kc, 1 + b:2 + b])

        g.memset(v_aug[:, :, :, :, 64:65], 1.0, [v_aug])

        def fm_cols(base, r=None):
            return None

        uT = g.sb("uT", [128, 2, 514], F32, stack=stB)
        g.memset(uT[:], 0.0, [uT])
        hc_sb = g.sb("hc_sb", [128, 512], F32, stack=stB)
        acc = g.sb("acc", [128, 512], F32, stack=stB)
        ycf = g.sb("ycf", [128, 512], F32, stack=stB)
        ysq = g.sb("ysq", [128, 2, 512], F32, stack=stB)
        psi = [0]

        def next_ps():
            psi[0] = (psi[0] + 1) % 8
            return PS[psi[0]]

        def fm_proj(col_ap_fn, tc):
            p = next_ps()
            for kc in range(8):
                g.mm(p[:, :], col_ap_fn(kc), hT[:, kc, tc * 512:(tc + 1) * 512], kc == 0, kc == 7,
                     [w_in_k[kc]] + hT_t[tc * 4:(tc + 1) * 4], [p])
            return p

        for tc in range(4):
            tsl = slice(tc * 512, (tc + 1) * 512)
            for blk in range(2):
                c0 = blk * 128
                p_hc = fm_proj(lambda kc, c0=c0: w_in_sb[:, kc, c0:c0 + 128], tc)
                g.cp(hc_sb[:], p_hc[:, :], [p_hc], [hc_sb], eng="act")
                p_gc = fm_proj(lambda kc, c0=c0: w_in_sb[:, kc, 512 + c0:512 + c0 + 128], tc)
                g.tt(uT[:, blk, 2:514], p_gc[:, :], hc_sb[:], ALU.mult, [p_gc, hc_sb], [uT])
                g.ts(acc[:], uT[:, blk, 2:514], conv_wT[:, blk, 2:3], None, ALU.mult, None, [uT, conv_wT], [acc])
                g.stt(acc[:], uT[:, blk, 1:513], conv_wT[:, blk, 1:2], acc[:], ALU.mult, ALU.add, [uT, conv_wT, acc], [acc])
                g.stt(acc[:], uT[:, blk, 0:512], conv_wT[:, blk, 0:1], acc[:], ALU.mult, ALU.add, [uT, conv_wT, acc], [acc])
                p_gb = fm_proj(lambda kc, c0=c0: w_in_sb[:, kc, 256 + c0:256 + c0 + 128], tc)
                g.tt(ycf[:], p_gb[:, :], acc[:], ALU.mult, [p_gb, acc], [ycf])
                g.act(ysq[:, blk, :], ycf[:], AF.Square, [ycf], [ysq])
                g.act(ycT[:, blk, tsl], ycf[:], AF.Copy, [ycf, g_convT], [ycT], scale=g_convT[:, blk:blk + 1])
                if tc == 3:
                    g.ld("sp", O["conv_p"][:, blk, :], uT[:, blk, 512:514], [uT], [], is_output=True)
                else:
                    g.cp(uT[:, blk, 0:2], uT[:, blk, 512:514], [uT], [uT])
            for t4 in range(4):
                tt = tc * 4 + t4
                p = next_ps()
                for blk in range(2):
                    g.mm(p[:, 0:1], ysq[:, blk, t4 * 128:(t4 + 1) * 128], ones_f[:, 0:1], blk == 0, blk == 1, [ysq, ones_f], [p])
                g.ts(rc[:, tt:tt + 1], p[:, 0:1], 1.0 / 256.0, EPS, ALU.mult, ALU.add, [p], [rc])
            for r in range(6):
                p = fm_proj(lambda kc, r=r: w_in_sb[:, kc, 768 + r * 128:768 + (r + 1) * 128], tc)
                g.cp(qT[:, r, tsl], p[:, :], [p], [qT], eng=("act" if r % 2 else "dve"))
            for e in range(2):
                p = fm_proj(lambda kc, e=e: w_in_sb[:, kc, 1536 + e * 128:1536 + (e + 1) * 128], tc)
                g.cp(kcT[:, e, tsl], p[:, :], [p], [kcT], eng=("act" if e else "dve"))
            p = fm_proj(lambda kc: w_in_sb[:, kc, 1792:1920], tc)
            g.cp(ksT[:, tsl], p[:, :], [p], [ksT], eng="act")
            p = fm_proj(lambda kc: w_in_sb[:, kc, 2048:2176], tc)
            g.cp(kwT[:, tsl], p[:, :], [p], [kwT], eng="dve")
        g.rsqrt(rc[:], [rc])

        kvraw = [g.sb("kvraw%d" % i, [128, 768], F32, stack=stB) for i in range(1)]
        for tt in range(NT):
            pa = next_ps()
            pbk = next_ps()
            for kc in range(8):
                g.mm(pa[:, :], hT[:, kc, tt * 128:(tt + 1) * 128], w_in_sb[:, kc, 1536:2048], kc == 0, kc == 7,
                     [hT_t[tt], w_in_k[kc]], [pa])
            for kc in range(8):
                g.mm(pbk[:, 0:292], hT[:, kc, tt * 128:(tt + 1) * 128], w_in_sb[:, kc, 2048:2340], kc == 0, kc == 7,
                     [hT_t[tt], w_in_k[kc]], [pbk])
            kv = kvraw[0]
            g.cp(kv[:, 0:512], pa[:, :], [pa], [kv], eng="dve")
            g.cp(kv[:, 512:768], pbk[:, 0:256], [pbk], [kv], eng="act")
            g.act(sig[:, tt, :], pbk[:, 256:292], AF.Sigmoid, [pbk], [sig])
            g.cp(v_aug[:, tt, 0, :, 0:64], kv[:, 384:512].rearrange("p (g d) -> p g d", g=2), [kv], [v_aug], eng="dve")
            g.cp(v_aug[:, tt, 1, :, 0:64], kv[:, 640:768].rearrange("p (g d) -> p g d", g=2), [kv], [v_aug], eng="dve")
            g.ld("sp", O["cmp_p"][tt * 128:(tt + 1) * 128, :], kv[:, 0:256], [kv], [], is_output=True)
            g.ld("sp", O["slc_p"][tt * 128:(tt + 1) * 128, :], kv[:, 256:512], [kv], [], is_output=True)
            if tt >= 12:
                g.ld("sp", O["win_p"][(tt - 12) * 128:(tt - 11) * 128, :], kv[:, 512:768], [kv], [], is_output=True)

        for cb in range(5):
            c0 = cb * 512
            c1 = min(PROJ, c0 + 512)
            p = next_ps()
            for kc in range(8):
                g.mm(p[0:32, 0:c1 - c0], hTs[:, kc, :], w_in_sb[:, kc, c0:c1], kc == 0, kc == 7, [hTs, w_in_k[kc]], [p])
            g.cp(Ps[:, c0:c1], p[0:32, 0:c1 - c0], [p], [Ps], eng=("act" if cb % 2 else "dve"))
        g.ld("sp", O["cmp_s"], Ps[:, 1536:1792], [Ps], [], is_output=True)
        g.ld("sp", O["slc_s"], Ps[:, 1792:2048], [Ps], [], is_output=True)
        for b in range(4):
            g.ld("sp", O["win_s"][b, 504:512, :], Ps[b * 8:(b + 1) * 8, 2048:2304], [Ps], [], is_output=True)
            g.ld("sp", O["win_s"][b, 0:504, :], I["win"][b, 8:512, :], [], [], is_output=True, track=odram)
        us = g.sb("us", [128, 2, 4, 10], F32, stack=stB)
        g.ld("sp", us[:, :, :, 0:2], I["sconvT"], [], [us])
        ycs = g.sb("ycs", [128, 2, 32], F32, stack=stB)
        hcs = g.sb("hcs", [128, 32], F32, stack=stB)
        accs = g.sb("accs", [128, 32], F32, stack=stB)
        for blk in range(2):
            c0 = blk * 128

            def fm_s(cofs):
                p = next_ps()
                for kc in range(8):
                    g.mm(p[:, 0:32], w_in_sb[:, kc, cofs:cofs + 128], hTs[:, kc, :], kc == 0, kc == 7, [w_in_k[kc], hTs], [p])
                return p
            p_hc = fm_s(c0)
            g.cp(hcs[:], p_hc[:, 0:32], [p_hc], [hcs], eng="act")
            p_gc = fm_s(512 + c0)
            g.tt(us[:, blk, :, 2:10], p_gc[:, 0:32].rearrange("p (b t) -> p b t", b=4),
                 hcs[:].rearrange("p (b t) -> p b t", b=4), ALU.mult, [p_gc, hcs], [us])
            a3 = accs[:].rearrange("p (b t) -> p b t", b=4)
            g.ts(a3, us[:, blk, :, 2:10], conv_wT[:, blk, 2:3], None, ALU.mult, None, [us, conv_wT], [accs])
            g.stt(a3, us[:, blk, :, 1:9], conv_wT[:, blk, 1:2], a3, ALU.mult, ALU.add, [us, conv_wT, accs], [accs])
            g.stt(a3, us[:, blk, :, 0:8], conv_wT[:, blk, 0:1], a3, ALU.mult, ALU.add, [us, conv_wT, accs], [accs])
            p_gb = fm_s(256 + c0)
            g.tt(ycs[:, blk, :], p_gb[:, 0:32], accs[:], ALU.mult, [p_gb, accs], [ycs])
            g.act(ysqs[:, blk, :], ycs[:, blk, :], AF.Square, [ycs], [ysqs])
            g.act(ycTs[:, blk, :], ycs[:, blk, :], AF.Copy, [ycs, g_convT], [ycTs], scale=g_convT[:, blk:blk + 1])
            g.ld("sp", O["conv_s"][:, blk, :, :], us[:, blk, :, 8:10], [us], [], is_output=True)

        def fm_s2(cofs):
            p = next_ps()
            for kc in range(8):
                g.mm(p[:, 0:32], w_in_sb[:, kc, cofs:cofs + 128], hTs[:, kc, :], kc == 0, kc == 7, [w_in_k[kc], hTs], [p])
            return p
        for r in range(6):
            p = fm_s2(768 + r * 128)
            g.cp(qTs[:, :, r, :], p[:, 0:32].rearrange("p (b t) -> p b t", b=4), [p], [qTs])
        p = fm_s2(1792)
        g.cp(ksTs[:], p[:, 0:32], [p], [ksTs])
        p = fm_s2(2048)
        g.cp(kwTs[:], p[:, 0:32], [p], [kwTs])
        g.act(sigS[:], Ps[:, 2304:2340], AF.Sigmoid, [Ps], [sigS])
        g.cp(Pv[:, 0, :], Ps[:, 1920:2048], [Ps], [Pv])
        g.cp(Pv[:, 1, :], Ps[:, 2176:2304], [Ps], [Pv])
        g.memset(vnew[:], 1.0, [vnew])
        for b in range(4):
            for br in range(2):
                g.ld("sp", vnew[0:8, b, br, :, 0:64], Pv[b * 8:(b + 1) * 8, br, :].rearrange("p (g d) -> p g d", g=2), [Pv], [vnew])
            for r in range(6):
                for gg in range(2):
                    g.ld("sp", gq[r * 8:(r + 1) * 8, b, gg, :], sigS[b * 8:(b + 1) * 8, gg * 6 + r:36:12], [sigS], [gq], nc_ok=True)
        p = next_ps()
        for blk in range(2):
            g.mm(p[0:32, 0:1], ysqs[:, blk, :], ones_f[:, 0:1], blk == 0, blk == 1, [ysqs, ones_f], [p])
        g.ts(rc_s[:], p[0:32, 0:1], 1.0 / 256.0, EPS, ALU.mult, ALU.add, [p], [rc_s])
        g.rsqrt(rc_s[:], [rc_s])
        g.barrier()
        stB.close()

        def load_cmp_w(stk, tag):
            w1rep = g.sb("w1rep" + tag, [128, 2, 32, 128], BF16, stack=stk)
            g.ld("pool", w1rep[:].rearrange("p e j h -> p (e j h)"), I["w1rep"], [], [w1rep])
            peT = g.sb("peT" + tag, [128, 2, 32], BF16, stack=stk)
            g.ld("pool", peT[:].rearrange("p e j -> p (e j)"), I["peT"], [], [peT])
            w2k = g.sb("w2k" + tag, [128, 128], BF16, stack=stk)
            g.ld("pool", w2k[:], I["w2k"], [], [w2k])
            w2v = g.sb("w2v" + tag, [128, 64], BF16, stack=stk)
            g.ld("pool", w2v[:], I["w2v"], [], [w2v])
            biasT = g.sb("biasT" + tag, [128, 2], F32, stack=stk)
            pbias = next_ps()
            for e in range(2):
                for j in range(32):
                    g.mm(pbias[:, e:e + 1], w1rep[0:64, e, j, :], peT[0:64, e, j:j + 1], j == 0, j == 31, [w1rep, peT], [pbias])
            g.tt(biasT[:], pbias[:, 0:2], b1T[:], ALU.add, [pbias, b1T], [biasT])
            return w1rep, w2k, w2v, biasT

        def gelu_tanh(hx_ap, hy_ap, out_ap, hx_t, hy_t, out_t):
            g.tt(hy_ap, hx_ap, hx_ap, ALU.mult, [hx_t], [hy_t])
            g.ts(hy_ap, hy_ap, 0.044715, 1.0, ALU.mult, ALU.add, [hy_t], [hy_t])
            g.tt(hy_ap, hy_ap, hx_ap, ALU.mult, [hy_t, hx_t], [hy_t])
            g.act(hy_ap, hy_ap, AF.Tanh, [hy_t], [hy_t], scale=0.7978845608028654)
            g.ts(hy_ap, hy_ap, 0.5, 0.5, ALU.mult, ALU.add, [hy_t], [hy_t])
            g.tt(out_ap, hy_ap, hx_ap, ALU.mult, [hy_t, hx_t], [out_t])

        def layernorm(src, gi, dst, stats, mv, lnbc, n=128):
            src_ap, src_t = src
            dst_ap, dst_t = dst
            for c2 in range(2):
                g.emit("dve", lambda e, c2=c2: e.bn_stats(out=stats[0:n, c2, :], in_=src_ap[:, c2 * 512:(c2 + 1) * 512]),
                       reads=[src_t], writes=[stats])
            g.emit("dve", lambda e: e.bn_aggr(out=mv[0:n, :], in_=stats[0:n, :, :]), reads=[stats], writes=[mv])
            g.ts(mv[0:n, 1:2], mv[0:n, 1:2], EPS, None, ALU.add, None, [mv], [mv])
            g.rsqrt(mv[0:n, 1:2], [mv])
            g.ts(dst_ap, src_ap, mv[0:n, 0:1], mv[0:n, 1:2], ALU.subtract, ALU.mult, [src_t, mv], [dst_t])
            g.tt(dst_ap, dst_ap, lnbc[0:n, gi, :], ALU.mult, [dst_t, lnbc], [dst_t])
            g.tt(dst_ap, dst_ap, lnbc[0:n, gi + 1, :], ALU.add, [dst_t, lnbc], [dst_t])

        w_o_v = I["w_o"].rearrange("(k p) n -> p k n", p=128)
        w_g_v = I["w_g"].rearrange("(k p) n -> p k n", p=128)
        w_u_v = I["w_u"].rearrange("(k p) n -> p k n", p=128)

        stS2 = ExitStack()
        w1rep_s, w2k_s, w2v_s, biasT_s = load_cmp_w(stS2, "_s")
        ident_bf = g.sb("ident_bf", [128, 128], BF16, stack=stS2)
        g.ld("pool", ident_bf[:], I["ident"], [], [ident_bf])
        ones_bf = g.sb("ones_bf", [128, 1], BF16, stack=stS2)
        g.memset(ones_bf[:], 1.0, [ones_bf])
        force_s = g.sb("force_s", [8, 257], F32, stack=stS2)
        g.ld("sp", force_s[:], I["force_s"], [], [force_s])
        gsum = g.sb("gsum", [48, 8], F32, stack=stS2)
        g.ld("sp", gsum[:], I["gsum"], [], [gsum])
        gtb = g.sb("gtb", [8, 48], BF16, stack=stS2)
        g.ld("pool", gtb[:], I["gt"], [], [gtb])
        cnew = g.sb("cnew", [8, 48], BF16, stack=stS2)
        g.ld("pool", cnew[:], I["causal_new"], [], [cnew])
        mw0 = g.sb("mw0", [128, 48], BF16, stack=stS2)
        g.ld("pool", mw0[:], I["mw0"], [], [mw0])
        ptT_sb = g.sb("ptT_sb", [128, 4], I32, stack=stS2)
        g.ld("sp", ptT_sb[:], I["ptT"], [], [ptT_sb])
        idx8 = g.sb("idx8", [128, 4, 8], I32, stack=stS2)
        for b in range(4):
            for k in range(8):
                g.ts(idx8[:, b, k:k + 1], ptT_sb[:, b:b + 1], 8, k, ALU.mult, ALU.add, [ptT_sb], [idx8])
        vaug_s = g.sb("vaug_s", [128, 8, 2, 322], BF16, stack=stS2)
        g.memset(vaug_s[:], 1.0, [vaug_s])
        for gg in range(2):
            g.ld("pool", vaug_s[:, :, gg, 65:322], I["as_"], [], [vaug_s])
        Xg = [g.sb("Xg%d" % i, [128, 16, 256], BF16, stack=stS2) for i in range(1)]
        XT = g.sb("XT", [128, 2, 16, 128], BF16, stack=stS2)
        TB = g.sb("TB", [128, 2, 4, 1024], F32, stack=stS2)
        hidTs = g.sb("hidTs", [128, 4, 1024], BF16, stack=stS2)
        g.memset(hidTs[:], 0.0, [hidTs])
        kcmpTs = g.sb("kcmpTs", [128, 1024], BF16, stack=stS2)
        ecs = g.sb("ecs", [128, 8, 48], BF16, stack=stS2)
        pss = g.sb("pss", [128, 8, 48], BF16, stack=stS2)
        rs_s = g.sb("rs_s", [48, 3], F32, stack=stS2)
        scn = g.sb("scn", [48, 257], F32, stack=stS2)
        sc2s = g.sb("sc2s", [8, 257], F32, stack=stS2)
        scw = g.sb("scw", [8, 257], F32, stack=stS2)
        m8s = g.sb("m8s", [8, 8], F32, stack=stS2)
        m8t = g.sb("m8t", [8, 8], F32, stack=stS2)
        sel_s = g.sb("sel_s", [8, 258], BF16, stack=stS2)
        maskTs = g.sb("maskTs", [128, 2, 2, 48], BF16, stack=stS2)
        oc_sb = g.sb("oc_sb", [48, 2, 65], F32, stack=stS2)
        pnew = g.sb("pnew", [8, 48], BF16, stack=stS2)
        Wb = g.sb("Wb", [128, 4, 256], BF16, stack=stS2)
        vx = g.sb("vx", [128, 16, 2, 65], BF16, stack=stS2)
        g.memset(vx[:], 1.0, [vx])
        Wv = g.sb("Wv", [128, 4, 2, 65], BF16, stack=stS2)
        g.memset(Wv[:], 1.0, [Wv])
        KTw = g.sb("KTw", [128, 4, 128], BF16, stack=stS2)
        cfs = g.sb("cfs", [48, 3], F32, stack=stS2)
        og_sb = g.sb("og_sb", [48, 2, 64], F32, stack=stS2)
        psb = [PS[0].h.bitcast(BF16), PS[1].h.bitcast(BF16)]
        tcount = [0]

        def transposes8(srcs, src_t, dst_ap, dst_t):
            i = tcount[0] % 2
            tcount[0] += 1
            for k, sap in enumerate(srcs):
                g.tr(psb[i][:, k * 128:(k + 1) * 128], sap, ident_bf[:], [src_t, ident_bf], [PS[i]])
            n = len(srcs)
            g.cp(dst_ap, psb[i][:, 0:n * 128].rearrange("p (k q) -> p k q", q=128), [PS[i]], [dst_t],
                 eng=("act" if tcount[0] % 2 else "dve"))

        def gather(pool_name, b, k, dst):
            g.dma("pool", lambda e: e.indirect_dma_start(
                out=dst[:].rearrange("p r c -> p (r c)"), out_offset=None, in_=I[pool_name],
                in_offset=bass.IndirectOffsetOnAxis(ap=idx8[:, b, k:k + 1], axis=0)), reads=[idx8], writes=[dst])

        dbt_all = g.sb("dbt_all", [48, 3, 130], F32, stack=stS2) if DBG else None
        gcount = [0]
        for b in range(4 if STAGE >= 2 else 0):
            qb = [qTs[0:64, b, :, :], qTs[64:128, b, :, :]]
            for cl in range(8):
                X = Xg[0]
                gcount[0] += 1
                gather("cmp_pool", b, cl, X)
                if SUB < 1:
                    continue
                for e in range(2):
                    for jh in range(2):
                        transposes8([X[:, jh * 8 + j8, e * 128:(e + 1) * 128] for j8 in range(8)], X,
                                    XT[:, e, jh * 8:(jh + 1) * 8, :], XT)
                if SUB < 2:
                    continue
                for tb in range(2):
                    for gg in range(2):
                        pb = PS[2 + gg] if tb == 0 else PS[4 + gg]
                        gs = slice(gg * 64, (gg + 1) * 64)
                        for e in range(2):
                            col = e * 128
                            for j in range(16):
                                g.mm(pb[:, col:col + 128], w1rep_s[gs, e, tb * 16 + j, :], XT[gs, e, j, :], j == 0, j == 15,
                                     [w1rep_s, XT], [pb])
                        g.cp(TB[:, tb, gg:4:2, cl:1024:8], pb[:, 0:256].rearrange("p (a q) -> p a q", q=128), [pb], [TB],
                             eng=("act" if (tb + gg) % 2 else "dve"))
            if SUB < 3:
                continue
            hx_ap = TB[:, 0, :, 0:1023]
            hy_ap = TB[:, 1, :, 0:1023]
            g.tt(hx_ap, TB[:, 0, :, 0:1023], TB[:, 1, :, 1:1024], ALU.add, [TB], [TB])
            for e in range(2):
                g.ts(TB[:, 0, e * 2:(e + 1) * 2, 0:1023], TB[:, 0, e * 2:(e + 1) * 2, 0:1023], biasT_s[:, e:e + 1], None, ALU.add, None,
                     [TB, biasT_s], [TB])
            gelu_tanh(hx_ap, hy_ap, hidTs[:, :, 0:1023], TB, TB, hidTs)
            if SUB < 4:
                continue
            for gg in range(2):
                gs = slice(gg * 64, (gg + 1) * 64)
                for hf in range(2):
                    po = PS[2 + hf]
                    g.mm(po[:, :], w2k_s[:], hidTs[:, gg, hf * 512:(hf + 1) * 512], True, True, [w2k_s, hidTs], [po])
                    g.cp(kcmpTs[gs, hf * 512:(hf + 1) * 512], po[gs, :], [po], [kcmpTs], eng="act")
                po = PS[4]
                for it in range(8):
                    g.mm(po[:, it * 64:(it + 1) * 64], hidTs[:, 2 + gg, it * 128:(it + 1) * 128], w2v_s[:], True, True,
                         [hidTs, w2v_s], [po])
                g.cp(vaug_s[:, :, gg, 0:64], po[:, :].rearrange("p (i d) -> p i d", d=64), [po], [vaug_s], eng="dve")
            if STAGE < 3:
                continue
            for gg in range(2):
                gs = slice(gg * 64, (gg + 1) * 64)
                pS = PS[2]
                for it in range(8):
                    g.mm(pS[:, it * 48:(it + 1) * 48], kcmpTs[gs, it * 128:(it + 1) * 128], qb[gg], True, True, [kcmpTs, qTs], [pS])
                g.act(ecs[:], pS[:, 0:384].rearrange("p (i q) -> p i q", q=48), AF.Exp, [pS], [ecs], scale=SCALE)
                pO = PS[3]
                for it in range(8):
                    n = 127 if it == 7 else 128
                    g.mm(pO[0:48, 0:322], ecs[0:n, it, :], vaug_s[0:n, it, gg, :], it == 0, it == 7, [ecs, vaug_s], [pO], signal=True)
                g.cp(oc_sb[:, gg, :], pO[0:48, 0:65], [pO], [oc_sb], eng="act")
                g.ts(rs_s[:, 0:1], pO[0:48, 64:65], 1e-30, None, ALU.max, None, [pO], [rs_s])
                g.emit("dve", lambda e: e.reciprocal(out=rs_s[:, 0:1], in_=rs_s[:, 0:1]), reads=[rs_s], writes=[rs_s])
                g.ts(scn[:], pO[0:48, 65:322], rs_s[:, 0:1], None, ALU.mult, None, [pO, rs_s], [scn])
                pI = PS[4]
                g.mm(pI[0:8, 0:257], gsum[:], scn[:], True, True, [gsum, scn], [pI])
                g.tt(sc2s[:], pI[0:8, 0:257], force_s[:], ALU.max, [pI, force_s], [sc2s])
                if DBG:
                    g.ld("sp", O["dbg_sc"][b, gg], sc2s[:], [sc2s], [], is_output=True)
                g.emit("dve", lambda e: e.max(out=m8s[:], in_=sc2s[:]), reads=[sc2s], writes=[m8s])
                g.emit("dve", lambda e: e.match_replace(out=scw[:], in_to_replace=m8s[:], in_values=sc2s[:], imm_value=-3e38),
                       reads=[m8s, sc2s], writes=[scw])
                g.emit("dve", lambda e: e.max(out=m8t[:], in_=scw[:]), reads=[scw], writes=[m8t])
                g.ts(sel_s[:, 0:257], sc2s[:], m8t[:, 7:8], None, ALU.is_ge, None, [sc2s, m8t], [sel_s])
                for par in range(2):
                    pM = PS[4]
                    g.mm(pM[:, 0:48], sel_s[:, par:256:2], gtb[:], True, True, [sel_s, gtb], [pM])
                    g.cp(maskTs[:, gg, par, :], pM[:, 0:48], [pM], [maskTs], eng="act")
            if STAGE < 4:
                continue
            pOs2 = [PS[4], PS[5]]
            pOw2 = [PS[6], PS[7]]
            for rg in range(8):
                X = Xg[0]
                gcount[0] += 1
                gather("slc_pool", b, rg, X)
                for rh in range(2):
                    transposes8([X[:, rh * 8 + r8, 0:128] for r8 in range(8)], X, XT[:, 0, rh * 8:(rh + 1) * 8, :], XT)
                par = rg // 4
                g.cp(vx[:, :, :, 0:64], X[:, :, 128:256].rearrange("p r (g d) -> p r g d", g=2), [X], [vx], eng="dve")
                for gg in range(2):
                    gs = slice(gg * 64, (gg + 1) * 64)
                    for rh in range(2):
                        pS = PS[2 + rh]
                        for r8 in range(8):
                            g.mm(pS[:, r8 * 48:(r8 + 1) * 48], XT[gs, 0, rh * 8 + r8, :], qb[gg], True, True, [XT, qTs], [pS])
                        g.act(ecs[:], pS[:, 0:384].rearrange("p (i q) -> p i q", q=48), AF.Exp, [pS], [ecs], scale=SCALE)
                        g.tt(pss[:], ecs[:], maskTs[:, gg, par, :].rearrange("p (o q) -> p o q", o=1).to_broadcast([128, 8, 48]),
                             ALU.mult, [ecs, maskTs], [pss])
                        for r8 in range(8):
                            rr = rh * 8 + r8
                            first = (rg == 0 and rr == 0)
                            g.mm(pOs2[gg][0:48, 0:65], pss[:, r8, :], vx[:, rr, gg, :], first, False,
                                 [pss, vx], [pOs2[gg]], signal=True)
            for gg in range(2):
                gs = slice(gg * 64, (gg + 1) * 64)
                for br, (kTn, pOx) in enumerate(((ksTs, pOs2[gg]), (kwTs, pOw2[gg]))):
                    if br == 1:
                        continue
                    pS = PS[2]
                    g.mm(pS[0:8, 0:48], kTn[gs, b * 8:(b + 1) * 8], qb[gg], True, True, [kTn, qTs], [pS])
                    g.act(pnew[:], pS[0:8, 0:48], AF.Exp, [pS], [pnew], scale=SCALE)
                    g.tt(pnew[:], pnew[:], cnew[:], ALU.mult, [pnew, cnew], [pnew])
                    g.mm(pOx[0:48, 0:65], pnew[:], vnew[0:8, b, br, gg, :], False, True, [pnew, vnew], [pOx], signal=True)
            if STAGE < 5:
                continue
            g.ld("pool", Wb[:], I["win"][b].rearrange("(w p) c -> p w c", p=128), [], [Wb])
            transposes8([Wb[:, w, 0:128] for w in range(4)], Wb, KTw[:, :, :], KTw)
            g.cp(Wv[:, :, :, 0:64], Wb[:, :, 128:256].rearrange("p w (g d) -> p w g d", g=2), [Wb], [Wv], eng="dve")
            for gg in range(2):
                gs = slice(gg * 64, (gg + 1) * 64)
                pS = PS[2]
                for w in range(4):
                    g.mm(pS[:, w * 48:(w + 1) * 48], KTw[gs, w, :], qb[gg], True, True, [KTw, qTs], [pS])
                g.act(ecs[:, 0:4, :], pS[:, 0:192].rearrange("p (i q) -> p i q", q=48), AF.Exp, [pS], [ecs], scale=SCALE)
                g.tt(ecs[:, 0, :], ecs[:, 0, :], mw0[:], ALU.mult, [ecs, mw0], [ecs])
                for w in range(4):
                    g.mm(pOw2[gg][0:48, 0:65], ecs[:, w, :], Wv[:, w, gg, :], w == 0, False, [ecs, Wv], [pOw2[gg]], signal=True)
                pS = PS[3]
                g.mm(pS[0:8, 0:48], kwTs[gs, b * 8:(b + 1) * 8], qb[gg], True, True, [kwTs, qTs], [pS])
                g.act(pnew[:], pS[0:8, 0:48], AF.Exp, [pS], [pnew], scale=SCALE)
                g.tt(pnew[:], pnew[:], cnew[:], ALU.mult, [pnew, cnew], [pnew])
                g.mm(pOw2[gg][0:48, 0:65], pnew[:], vnew[0:8, b, 1, gg, :], False, True, [pnew, vnew], [pOw2[gg]], signal=True)
            if STAGE < 6:
                continue
            if DBG:
                dbt = dbt_all
                g.cp(dbt[:, 0, :], oc_sb[:].rearrange("p g c -> p (g c)"), [oc_sb], [dbt])
                for gg_ in range(2):
                    g.cp(dbt[:, 1, gg_ * 65:(gg_ + 1) * 65], pOs2[gg_][0:48, 0:65], [pOs2[gg_]], [dbt])
                    g.cp(dbt[:, 2, gg_ * 65:(gg_ + 1) * 65], pOw2[gg_][0:48, 0:65], [pOw2[gg_]], [dbt])
                g.ld("sp", O["dbg_oc"][b], dbt[:, 0, :], [dbt], [], is_output=True)
                g.ld("sp", O["dbg_os"][b], dbt[:, 1, :], [dbt], [], is_output=True)
                g.ld("sp", O["dbg_ow"][b], dbt[:, 2, :], [dbt], [], is_output=True)
            for gg in range(2):
                g.ts(rs_s[:, 0:1], oc_sb[:, gg, 64:65], 1e-30, None, ALU.max, None, [oc_sb], [rs_s])
                pOs = pOs2[gg]
                pOw = pOw2[gg]
                g.ts(rs_s[:, 1:2], pOs[0:48, 64:65], 1e-30, None, ALU.max, None, [pOs], [rs_s])
                g.ts(rs_s[:, 2:3], pOw[0:48, 64:65], 1e-30, None, ALU.max, None, [pOw], [rs_s])
                g.emit("dve", lambda e: e.reciprocal(out=rs_s[:], in_=rs_s[:]), reads=[rs_s], writes=[rs_s])
                g.tt(cfs[:], rs_s[:], gq[:, b, gg, :], ALU.mult, [rs_s, gq], [cfs])
                g.ts(og_sb[:, gg, :], oc_sb[:, gg, 0:64], cfs[:, 0:1], None, ALU.mult, None, [oc_sb, cfs], [og_sb])
                g.stt(og_sb[:, gg, :], pOs[0:48, 0:64], cfs[:, 1:2], og_sb[:, gg, :], ALU.mult, ALU.add,
                      [pOs, cfs, og_sb], [og_sb])
                g.stt(og_sb[:, gg, :], pOw[0:48, 0:64], cfs[:, 2:3], og_sb[:, gg, :], ALU.mult, ALU.add,
                      [pOw, cfs, og_sb], [og_sb])
                for r in range(6):
                    g.ld("sp", o_tok[b * 8:(b + 1) * 8, gg * 384 + r * 64:gg * 384 + (r + 1) * 64], og_sb[r * 8:(r + 1) * 8, gg, :],
                         [og_sb], [o_tok])

        if DBG:
            g.ld("sp", O["dbg_o"], o_tok[:], [o_tok], [], is_output=True)
        if STAGE >= 7:
            g.barrier()
            stS2.close()
            stS2 = ExitStack()
            w_o_s = g.sb("w_o_s", [128, 8, D], BF16, stack=stS2)
            for kc in range(8):
                g.ld("pool", w_o_s[:, kc, :], w_o_v[:, kc, :], [], [w_o_s])
            lnbc_s = g.sb("lnbc_s", [32, 4, D], F32, stack=stS2)
            for i, nm in enumerate(("ln1_g", "ln1_b", "ln2_g", "ln2_b")):
                g.ld("sp", lnbc_s[:, i, :], I[nm].rearrange("(o d) -> o d", o=1).to_broadcast([32, D]), [], [lnbc_s])
            stats_s = g.sb("stats_s", [32, 2, 6], F32, stack=stS2)
            mv_s = g.sb("mv_s", [32, 2], F32, stack=stS2)
            osq_s = g.sb("osq_s", [32, 768], F32, stack=stS2)
            rn_s = g.sb("rn_s", [32, 1], F32, stack=stS2)
            oTs = g.sb("oTs", [128, 6, 32], BF16, stack=stS2)
            t1s = g.sb("t1s", [32, D], F32, stack=stS2)
            xs2 = g.sb("xs2", [32, D], F32, stack=stS2)
            x1s = g.sb("x1s", [32, D], F32, stack=stS2)
            h2Ts = g.sb("h2Ts", [128, 8, 32], BF16, stack=stS2)
            aTs = g.sb("aTs", [128, NFF, 32], BF16, stack=stS2)
            sgs = g.sb("sgs", [128, 32], F32, stack=stS2)
            wgs = [g.sb("wgs%d" % i, [128, 8, 128], BF16, stack=stS2) for i in range(2)]
            wus = [g.sb("wus%d" % i, [128, 8, 128], BF16, stack=stS2) for i in range(2)]
            wds = [g.sb("wds%d" % i, [128, D], BF16, stack=stS2) for i in range(2)]
            g.tt(osq_s[:], o_tok[:], o_tok[:], ALU.mult, [o_tok], [osq_s])
            g.emit("dve", lambda e: e.reduce_sum(out=rn_s[:], in_=osq_s[:], axis=AX.X), reads=[osq_s], writes=[rn_s])
            g.ts(rn_s[:], rn_s[:], 1.0 / 768.0, EPS, ALU.mult, ALU.add, [rn_s], [rn_s])
            g.rsqrt(rn_s[:], [rn_s])
            pb = PS[7]
            for blk in range(6):
                g.tr(pb[:, blk * 32:(blk + 1) * 32], o_tok[:, blk * 128:(blk + 1) * 128], ident[0:32, 0:32], [o_tok, ident], [pb])
            for blk in range(6):
                g.act(oTs[:, blk, :], pb[:, blk * 32:(blk + 1) * 32], AF.Copy, [pb, g_nsaT], [oTs], scale=g_nsaT[:, blk:blk + 1])
            for hf in range(2):
                cs_ = slice(hf * 512, (hf + 1) * 512)
                pc = PS[0 + hf]
                pn = PS[2 + hf]
                for blk in range(2):
                    g.mm(pc[0:32, :], ycTs[:, blk, :], w_o_s[:, blk, cs_], blk == 0, blk == 1, [ycTs, w_o_s], [pc])
                for blk in range(6):
                    g.mm(pn[0:32, :], oTs[:, blk, :], w_o_s[:, 2 + blk, cs_], blk == 0, blk == 5, [oTs, w_o_s], [pn])
                g.ts(t1s[:, cs_], pc[0:32, :], rc_s[:, 0:1], None, ALU.mult, None, [pc, rc_s], [t1s])
                g.stt(t1s[:, cs_], pn[0:32, :], rn_s[:, 0:1], t1s[:, cs_], ALU.mult, ALU.add, [pn, rn_s, t1s], [t1s])
            g.tt(t1s[:], t1s[:], gate_s[:, 0, :], ALU.mult, [t1s, gate_s], [t1s])
            g.ld("sp", xs2[:], I["xs"], [], [xs2])
            g.stt(t1s[:], xs2[:], ALPHA, t1s[:], ALU.mult, ALU.add, [xs2, t1s], [t1s])
            layernorm((t1s[:], t1s), 0, (x1s[:], x1s), stats_s, mv_s, lnbc_s, n=32)
            for hf in range(2):
                pb = PS[6 + hf]
                for k4 in range(4):
                    kc = hf * 4 + k4
                    g.tr(pb[:, k4 * 32:(k4 + 1) * 32], x1s[:, kc * 128:(kc + 1) * 128], ident[0:32, 0:32], [x1s, ident], [pb])
                for k4 in range(4):
                    kc = hf * 4 + k4
                    for b in range(4):
                        g.act(h2Ts[:, kc, b * 8:(b + 1) * 8], pb[:, k4 * 32 + b * 8:k4 * 32 + (b + 1) * 8], AF.Identity,
                              [pb, ops2, modT], [h2Ts], bias=modT[:, 24 + kc, 1 + b:2 + b], scale=ops2[:, kc, 1 + b:2 + b])
            for ffc in range(NFF):
                wg_t = wgs[ffc % 2]
                wu_t = wus[ffc % 2]
                fs = slice(ffc * 128, (ffc + 1) * 128)
                g.ld("pool", wg_t[:], w_g_v[:, :, fs], [], [wg_t])
                g.ld("pool", wu_t[:], w_u_v[:, :, fs], [], [wu_t])
                g.ld("sp", wscr["wg_bf"][ffc], wg_t[:].rearrange("p k n -> p (k n)"), [wg_t], [WSC["wg_bf"][ffc]])
                g.ld("sp", wscr["wu_bf"][ffc], wu_t[:].rearrange("p k n -> p (k n)"), [wu_t], [WSC["wu_bf"][ffc]])
                pG = PS[(2 * ffc) % 4]
                pU = PS[(2 * ffc + 1) % 4]
                for kc in range(8):
                    g.mm(pG[:, 0:32], wg_t[:, kc, :], h2Ts[:, kc, :], kc == 0, kc == 7, [wg_t, h2Ts], [pG])
                for kc in range(8):
                    g.mm(pU[:, 0:32], wu_t[:, kc, :], h2Ts[:, kc, :], kc == 0, kc == 7, [wu_t, h2Ts], [pU])
                g.act(sgs[:], pG[:, 0:32], AF.Silu, [pG], [sgs])
                g.tt(aTs[:, ffc, :], sgs[:], pU[:, 0:32], ALU.mult, [sgs, pU], [aTs])
            for ffc in range(NFF):
                wd_t = wds[ffc % 2]
                g.ld("pool", wd_t[:], I["w_d"][ffc * 128:(ffc + 1) * 128, :], [], [wd_t])
                g.ld("sp", wscr["wd_bf"][ffc], wd_t[:], [wd_t], [WSC["wd_bf"][ffc]])
                for hf in range(2):
                    g.mm(PS[4 + hf][0:32, :], aTs[:, ffc, :], wd_t[:, hf * 512:(hf + 1) * 512], ffc == 0, ffc == NFF - 1,
                         [aTs, wd_t], [PS[4 + hf]], signal=True)
            for hf in range(2):
                cs_ = slice(hf * 512, (hf + 1) * 512)
                g.tt(t1s[:, cs_], PS[4 + hf][0:32, :], gate_s[:, 1, cs_], ALU.mult, [PS[4 + hf], gate_s], [t1s])
            g.stt(t1s[:], x1s[:], ALPHA, t1s[:], ALU.mult, ALU.add, [x1s, t1s], [t1s])
            layernorm((t1s[:], t1s), 2, (xs2[:], xs2), stats_s, mv_s, lnbc_s, n=32)
            g.ld("sp", O["y_s"], xs2[:], [xs2], [], is_output=True)
        g.barrier()
        stS2.close()
        stS.close()


        if DO_PROMPT:
            kcmpT = g.sb("kcmpT", [128, 128], BF16, stack=stC)
            vcmp = g.sb("vcmp", [128, 2, 97], BF16, stack=stC)
            stC2 = ExitStack()
            w1rep, w2k, w2v, biasT = load_cmp_w(stC2, "_p")
            acmp_f = g.sb("acmp_f", [128, 32], F32, stack=stC2)
            g.ld("sp", acmp_f[:], I["acmp"], [], [acmp_f])
            g.memset(vcmp[:], 0.0, [vcmp])
            g.memset(vcmp[:, :, 64:65], 1.0, [vcmp])
            for gg in range(2):
                g.cp(vcmp[:, gg, 65:97], acmp_f[:], [acmp_f, vcmp], [vcmp])
            bot_sb = g.sb("bot_sb", [128, 128], F32, stack=stC2)
            hx = g.sb("hx", [128, 127], F32, stack=stC2)
            hy = g.sb("hy", [128, 127], F32, stack=stC2)
            hidT = g.sb("hidT", [128, 127], BF16, stack=stC2)
            for e in range(2):
                for gg in range(2):
                    gs = slice(gg * 64, (gg + 1) * 64)
                    ptop = next_ps()
                    pbot = next_ps()
                    for j in range(16):
                        g.mm(ptop[:, 0:128], w1rep[gs, e, j, :], kcT[gs, e, j:SEQ:16], j == 0, j == 15, [w1rep, kcT], [ptop])
                    for j in range(16):
                        g.mm(pbot[:, 0:128], w1rep[gs, e, 16 + j, :], kcT[gs, e, j:SEQ:16], j == 0, j == 15, [w1rep, kcT], [pbot])
                    g.cp(bot_sb[:], pbot[:, 0:128], [pbot], [bot_sb], eng="act")
                    g.tt(hx[:], ptop[:, 0:127], bot_sb[:, 1:128], ALU.add, [ptop, bot_sb], [hx])
                    g.ts(hx[:], hx[:], biasT[:, e:e + 1], None, ALU.add, None, [hx, biasT], [hx])
                    g.tt(hy[:], hx[:], hx[:], ALU.mult, [hx], [hy])
                    g.ts(hy[:], hy[:], 0.044715, 1.0, ALU.mult, ALU.add, [hy], [hy])
                    g.tt(hy[:], hy[:], hx[:], ALU.mult, [hy, hx], [hy])
                    g.act(hy[:], hy[:], AF.Tanh, [hy], [hy], scale=0.7978845608028654)
                    g.ts(hy[:], hy[:], 0.5, 0.5, ALU.mult, ALU.add, [hy], [hy])
                    g.tt(hidT[:], hy[:], hx[:], ALU.mult, [hy, hx], [hidT])
                    po = next_ps()
                    if e == 0:
                        g.mm(po[:, 0:127], w2k[:], hidT[:], True, True, [w2k, hidT], [po])
                        g.cp(kcmpT[gs, 0:127], po[gs, 0:127], [po], [kcmpT], eng="dve")
                    else:
                        g.mm(po[0:127, 0:64], hidT[:], w2v[:], True, True, [w2v, hidT], [po])
                        g.cp(vcmp[0:127, gg, 0:64], po[0:127, 0:64], [po], [vcmp], eng="dve")
            g.barrier()
            stC2.close()

            stD = ExitStack()
            w_o_sb = g.sb("w_o_sb", [128, 8, D], BF16, stack=stD)
            for kc in range(8):
                g.ld("pool", w_o_sb[:, kc, :], w_o_v[:, kc, :], [], [w_o_sb])
            lnbc = g.sb("lnbc", [128, 4, D], F32, stack=stD)
            for i, nm in enumerate(("ln1_g", "ln1_b", "ln2_g", "ln2_b")):
                g.ld("sp", lnbc[:, i, :], I[nm].rearrange("(o d) -> o d", o=1).to_broadcast([128, D]), [], [lnbc])
            ec = g.sb("ec", [128, 6, 128], BF16, stack=stD)
            es = [g.sb("es%d" % i, [128, 4, 128], BF16, stack=stD) for i in range(3)]
            pTt = [g.sb("pTt%d" % i, [128, 4, 128], BF16, stack=stD) for i in range(3)]
            maskT = g.sb("maskT", [128, NT, 128], BF16, stack=stD)
            rsc = g.sb("rsc", [128, 6], F32, stack=stD)
            score = g.sb("score", [128, 32], F32, stack=stD)
            sc2 = g.sb("sc2", [128, 32], F32, stack=stD)
            m8a = g.sb("m8a", [128, 8], F32, stack=stD)
            m8b = g.sb("m8b", [128, 8], F32, stack=stD)
            sel = g.sb("sel", [128, 32], F32, stack=stD)
            selT = g.sb("selT", [32, 128], BF16, stack=stD)
            rs2 = g.sb("rs2", [128, 2], F32, stack=stD)
            cf = g.sb("cf", [128, 3], F32, stack=stD)
            o_sb = g.sb("o_sb", [128, 768], F32, stack=stD)
            osq = g.sb("osq", [128, 768], F32, stack=stD)
            rn = g.sb("rn", [128, 1], F32, stack=stD)
            oT = g.sb("oT", [128, 6, 128], BF16, stack=stD)
            t1 = g.sb("t1", [128, D], F32, stack=stD)
            xt2 = g.sb("xt2", [128, D], F32, stack=stD)
            stats = g.sb("stats", [128, 2, 6], F32, stack=stD)
            mv = g.sb("mv", [128, 2], F32, stack=stD)
            x1g = g.sb("x1g", [128, 4, D], F32, stack=stD)
            h2T = g.sb("h2T", [128, 8, 512], BF16, stack=stD)
            aT = g.sb("aT", [128, NFF, 512], BF16, stack=stD)
            sg_sb = g.sb("sg_sb", [128, 512], F32, stack=stD)
            wgc = [g.sb("wgc%d" % i, [128, 8, 128], BF16, stack=stD) for i in range(2)]
            wuc = [g.sb("wuc%d" % i, [128, 8, 128], BF16, stack=stD) for i in range(2)]
            wdc = [g.sb("wdc%d" % i, [128, D], BF16, stack=stD) for i in range(2)]
            w_g_v = I["w_g"].rearrange("(k p) n -> p k n", p=128)
            w_u_v = I["w_u"].rearrange("(k p) n -> p k n", p=128)

            for grp in range(NT // 4):
                for t4 in range(4):
                    qt = grp * 4 + t4
                    qs = slice(qt * 128, (qt + 1) * 128)
                    g.ld("sp", xt2[:], I["xp"][qs, :], [], [xt2])
                    for gg in range(2):
                        gs = slice(gg * 64, (gg + 1) * 64)
                        for r in range(6):
                            pS = PS[2 + (r // 4)]
                            g.mm(pS[0:127, (r % 4) * 128:(r % 4 + 1) * 128], kcmpT[gs, 0:127], qT[gs, r, qs], True, True,
                                 [kcmpT, qT], [pS])
                        g.act(ec[0:127, 0:4, :], PS[2][0:127, :].rearrange("p (r q) -> p r q", r=4), AF.Exp, [PS[2]], [ec], scale=SCALE)
                        g.act(ec[0:127, 4:6, :], PS[3][0:127, 0:256].rearrange("p (r q) -> p r q", r=2), AF.Exp, [PS[3]], [ec], scale=SCALE)
                        g.tt(ec[0:127, :, :], ec[0:127, :, :],
                             cmpmask[0:127, qs].rearrange("p (o q) -> p o q", o=1).to_broadcast([127, 6, 128]), ALU.mult,
                             [ec, cmpmask], [ec])
                        for r in range(6):
                            pO = PS[r // 3]
                            g.mm(pO[:, (r % 3) * 97:(r % 3 + 1) * 97], ec[0:127, r, :], vcmp[0:127, gg, :], True, True, [ec, vcmp], [pO])
                        for hh in range(2):
                            g.ts(rsc[:, hh * 3:(hh + 1) * 3], PS[hh][:, 0:291].rearrange("p (r c) -> p r c", c=97)[:, :, 64],
                                 1e-30, None, ALU.max, None, [PS[hh]], [rsc])
                        g.emit("dve", lambda e: e.reciprocal(out=rsc[:], in_=rsc[:]), reads=[rsc], writes=[rsc])
                        for r in range(6):
                            src = PS[r // 3][:, (r % 3) * 97 + 65:(r % 3) * 97 + 97]
                            if r == 0:
                                g.ts(score[:], src, rsc[:, 0:1], None, ALU.mult, None, [PS[0], rsc], [score])
                            else:
                                g.stt(score[:], src, rsc[:, r:r + 1], score[:], ALU.mult, ALU.add, [PS[r // 3], rsc, score], [score])
                        g.tt(sc2[:], score[:], force[:, qt, :], ALU.max, [score, force], [sc2])
                        g.tt(sc2[:], sc2[:], validneg[:, qt, :], ALU.add, [sc2, validneg], [sc2])
                        g.emit("dve", lambda e: e.max(out=m8a[:], in_=sc2[:]), reads=[sc2], writes=[m8a])
                        g.emit("dve", lambda e: e.match_replace(out=score[:], in_to_replace=m8a[:], in_values=sc2[:], imm_value=-3e38),
                               reads=[m8a, sc2], writes=[score])
                        g.emit("dve", lambda e: e.max(out=m8b[:], in_=score[:]), reads=[score], writes=[m8b])
                        g.ts(sel[:], sc2[:], m8b[:, 7:8], None, ALU.is_ge, None, [sc2, m8b], [sel])
                        g.tr(PS[4][0:32, 0:128], sel[:], ident[:], [sel, ident], [PS[4]])
                        g.cp(selT[:], PS[4][0:32, 0:128], [PS[4]], [selT], eng="act")
                        for k0 in range(0, qt + 1, 4):
                            nk = min(4, qt + 1 - k0)
                            for k4 in range(nk):
                                g.mm(PS[4][:, k4 * 128:(k4 + 1) * 128], lsel[:, k0 + k4, :], selT[:], True, True, [lsel, selT], [PS[4]])
                            g.cp(maskT[:, k0:k0 + nk, :], PS[4][:, 0:nk * 128].rearrange("p (k q) -> p k q", q=128), [PS[4]], [maskT],
                                 eng="act")
                        g.tt(maskT[:, qt, :], maskT[:, qt, :], tri_le[:], ALU.mult, [maskT, tri_le], [maskT])
                        rounds = []
                        wks = list(range(max(0, qt - 4), qt + 1))
                        for r in range(6):
                            for k0 in range(0, qt + 1, 4):
                                rounds.append(("slc", r, list(range(k0, min(k0 + 4, qt + 1))), False))
                            for c0 in range(0, len(wks), 4):
                                rounds.append(("win", r, wks[c0:c0 + 4], c0 + 4 >= len(wks)))
                        SBK = [PS[2], PS[3], PS[7]]
                        LOOK = 2

                        def emit_scores(i):
                            kind, r, kts, _ = rounds[i]
                            pS = SBK[i % 3]
                            kT = ksT if kind == "slc" else kwT
                            for k4, kt in enumerate(kts):
                                g.mm(pS[:, k4 * 128:(k4 + 1) * 128], kT[gs, kt * 128:(kt + 1) * 128], qT[gs, r, qs], True, True,
                                     [kT, qT], [pS])

                        def emit_rest(i):
                            kind, r, kts, last = rounds[i]
                            pS = SBK[i % 3]
                            e_t = es[i % 3]
                            p_t = pTt[i % 3]
                            nk = len(kts)
                            pOW = PS[5 + (r % 2)]
                            g.act(e_t[:, 0:nk, :], pS[:, 0:nk * 128].rearrange("p (k q) -> p k q", q=128), AF.Exp, [pS], [e_t], scale=SCALE)
                            if kind == "slc":
                                g.tt(p_t[:, 0:nk, :], e_t[:, 0:nk, :], maskT[:, kts[0]:kts[0] + nk, :], ALU.mult, [e_t, maskT], [p_t])
                                for k4, kt in enumerate(kts):
                                    g.mm(pOW[:, 0:65], p_t[:, k4, :], v_aug[:, kt, 0, gg, :], kt == 0, kt == qt, [p_t, v_aug], [pOW],
                                         signal=True)
                            else:
                                for k4, kt in enumerate(kts):
                                    if kt == qt:
                                        g.tt(e_t[:, k4, :], e_t[:, k4, :], tri_le[:], ALU.mult, [e_t, tri_le], [e_t])
                                    elif kt == qt - 4:
                                        g.tt(e_t[:, k4, :], e_t[:, k4, :], tri_gt[:], ALU.mult, [e_t, tri_gt], [e_t])
                                for k4, kt in enumerate(kts):
                                    g.mm(pOW[:, 65:130], e_t[:, k4, :], v_aug[:, kt, 1, gg, :], kt == wks[0], kt == qt, [e_t, v_aug], [pOW],
                                         signal=True)
                            if last:
                                pending.append([r, pOW, 2])

                        def emit_combine(r, pOW):
                            if True:
                                g.ts(rs2[:], pOW[:, 0:130].rearrange("p (b c) -> p b c", c=65)[:, :, 64], 1e-30, None, ALU.max, None,
                                     [pOW], [rs2])
                                g.emit("dve", lambda e: e.reciprocal(out=rs2[:], in_=rs2[:]), reads=[rs2], writes=[rs2])
                                gi0 = gg * 6 + r
                                g.tt(cf[:, 0:1], rsc[:, r:r + 1], sig[:, qt, gi0:gi0 + 1], ALU.mult, [rsc, sig], [cf])
                                g.tt(cf[:, 1:2], rs2[:, 0:1], sig[:, qt, 12 + gi0:13 + gi0], ALU.mult, [rs2, sig], [cf])
                                g.tt(cf[:, 2:3], rs2[:, 1:2], sig[:, qt, 24 + gi0:25 + gi0], ALU.mult, [rs2, sig], [cf])
                                oc = o_sb[:, gg * 384 + r * 64:gg * 384 + (r + 1) * 64]
                                g.ts(oc, PS[r // 3][:, (r % 3) * 97:(r % 3) * 97 + 64], cf[:, 0:1], None, ALU.mult, None,
                                     [PS[r // 3], cf], [o_sb])
                                g.stt(oc, pOW[:, 0:64], cf[:, 1:2], oc, ALU.mult, ALU.add, [pOW, cf, o_sb], [o_sb])
                                g.stt(oc, pOW[:, 65:129], cf[:, 2:3], oc, ALU.mult, ALU.add, [pOW, cf, o_sb], [o_sb])

                        nr = len(rounds)
                        pending = []
                        for i in range(min(LOOK, nr)):
                            emit_scores(i)
                        for i in range(nr):
                            if i + LOOK < nr:
                                emit_scores(i + LOOK)
                            emit_rest(i)
                            for pd in list(pending):
                                pd[2] -= 1
                                if pd[2] < 0:
                                    emit_combine(pd[0], pd[1])
                                    pending.remove(pd)
                        for pd in pending:
                            emit_combine(pd[0], pd[1])
                        pending = []
                    g.tt(osq[:], o_sb[:], o_sb[:], ALU.mult, [o_sb], [osq])
                    g.emit("dve", lambda e: e.reduce_sum(out=rn[:], in_=osq[:], axis=AX.X), reads=[osq], writes=[rn])
                    g.ts(rn[:], rn[:], 1.0 / 768.0, EPS, ALU.mult, ALU.add, [rn], [rn])
                    g.rsqrt(rn[:], [rn])
                    for hf in range(2):
                        pb = PS[6 + hf]
                        nb_ = 4 if hf == 0 else 2
                        for k4 in range(nb_):
                            blk = hf * 4 + k4
                            g.tr(pb[:, k4 * 128:(k4 + 1) * 128], o_sb[:, blk * 128:(blk + 1) * 128], ident[:], [o_sb, ident], [pb])
                        for k4 in range(nb_):
                            blk = hf * 4 + k4
                            g.act(oT[:, blk, :], pb[:, k4 * 128:(k4 + 1) * 128], AF.Copy, [pb, g_nsaT], [oT], scale=g_nsaT[:, blk:blk + 1])
                    for hf in range(2):
                        cs_ = slice(hf * 512, (hf + 1) * 512)
                        pc = PS[0 + hf]
                        pn = PS[2 + hf]
                        for blk in range(2):
                            g.mm(pc[:, :], ycT[:, blk, qs], w_o_sb[:, blk, cs_], blk == 0, blk == 1, [ycT, w_o_sb], [pc])
                        for blk in range(6):
                            g.mm(pn[:, :], oT[:, blk, :], w_o_sb[:, 2 + blk, cs_], blk == 0, blk == 5, [oT, w_o_sb], [pn])
                        g.ts(t1[:, cs_], pc[:, :], rc[:, qt:qt + 1], None, ALU.mult, None, [pc, rc], [t1])
                        g.stt(t1[:, cs_], pn[:, :], rn[:, 0:1], t1[:, cs_], ALU.mult, ALU.add, [pn, rn, t1], [t1])
                    g.tt(t1[:], t1[:], gate_p[:, 0, :], ALU.mult, [t1, gate_p], [t1])
                    g.stt(t1[:], xt2[:], ALPHA, t1[:], ALU.mult, ALU.add, [xt2, t1], [t1])
                    layernorm((t1[:], t1), 0, (x1g[:, t4, :], x1g), stats, mv, lnbc)
                    for hf in range(2):
                        pb = PS[6 + hf]
                        for k4 in range(4):
                            kc = hf * 4 + k4
                            g.tr(pb[:, k4 * 128:(k4 + 1) * 128], x1g[:, t4, kc * 128:(kc + 1) * 128], ident[:], [x1g, ident], [pb])
                        for k4 in range(4):
                            kc = hf * 4 + k4
                            g.act(h2T[:, kc, t4 * 128:(t4 + 1) * 128], pb[:, k4 * 128:(k4 + 1) * 128], AF.Identity,
                                  [pb, ops2, modT], [h2T], bias=modT[:, 24 + kc, 0:1], scale=ops2[:, kc, 0:1])
                for ffc in range(NFF):
                    wg_t = wgc[ffc % 2]
                    wu_t = wuc[ffc % 2]
                    fs = slice(ffc * 128, (ffc + 1) * 128)
                    g.ld("sp", wg_t[:].rearrange("p k n -> p (k n)"), wscr["wg_bf"][ffc], [WSC["wg_bf"][ffc]], [wg_t])
                    g.ld("pool", wu_t[:].rearrange("p k n -> p (k n)"), wscr["wu_bf"][ffc], [WSC["wu_bf"][ffc]], [wu_t])
                    pG = PS[(2 * ffc) % 8]
                    pU = PS[(2 * ffc + 1) % 8]
                    for kc in range(8):
                        g.mm(pG[:, :], wg_t[:, kc, :], h2T[:, kc, :], kc == 0, kc == 7, [wg_t, h2T], [pG])
                    for kc in range(8):
                        g.mm(pU[:, :], wu_t[:, kc, :], h2T[:, kc, :], kc == 0, kc == 7, [wu_t, h2T], [pU])
                    g.act(sg_sb[:], pG[:, :], AF.Silu, [pG], [sg_sb])
                    g.tt(aT[:, ffc, :], sg_sb[:], pU[:, :], ALU.mult, [sg_sb, pU], [aT])
                for ffc in range(NFF):
                    wd_t = wdc[ffc % 2]
                    g.ld("sp" if ffc % 2 else "pool", wd_t[:], wscr["wd_bf"][ffc], [WSC["wd_bf"][ffc]], [wd_t])
                    for t4 in range(4):
                        for hf in range(2):
                            g.mm(PS[t4 * 2 + hf][:, :], aT[:, ffc, t4 * 128:(t4 + 1) * 128], wd_t[:, hf * 512:(hf + 1) * 512],
                                 ffc == 0, ffc == NFF - 1, [aT, wd_t], [PS[t4 * 2 + hf]], signal=True)
                for t4 in range(4):
                    qt = grp * 4 + t4
                    for hf in range(2):
                        cs_ = slice(hf * 512, (hf + 1) * 512)
                        g.tt(t1[:, cs_], PS[t4 * 2 + hf][:, :], gate_p[:, 1, cs_], ALU.mult, [PS[t4 * 2 + hf], gate_p], [t1])
                    g.stt(t1[:], x1g[:, t4, :], ALPHA, t1[:], ALU.mult, ALU.add, [x1g, t1], [t1])
                    layernorm((t1[:], t1), 2, (xt2[:], xt2), stats, mv, lnbc)
                    g.ld("sp", O["y_p"][qt * 128:(qt + 1) * 128, :], xt2[:], [xt2], [], is_output=True)
            g.barrier()
            stD.close()

        g.finish()
        stC.close()
    return nc


_NC_CACHE = {}


def _perm_w_in(w):
    w = w.copy()
    q = w[:, 768:1536].reshape(D, 2, 6, 64).transpose(0, 2, 1, 3).reshape(D, 768)
    w[:, 768:1536] = q
    return w


def kernel(x_prompt, x_sample, cache_cmp_kv, cache_slc_kv, cache_win_kv, state_conv, page_table,
           c_prompt, c_sample, w_ada, b_ada, w_in, conv_w, cmp_pe, cmp_w1, cmp_b1, cmp_w2,
           g_conv_out, g_nsa_out, w_o, ln1_g, ln1_b, ln2_g, ln2_b, w_ffn_gate, w_ffn_up, w_ffn_down,
           _cores=None):
    f = lambda a: np.ascontiguousarray(np.asarray(a, dtype=np.float32))
    if "nc" not in _NC_CACHE:
        _NC_CACHE["nc"] = build_nc()
    nc = _NC_CACHE["nc"]
    consts = host_constants()
    cores = list(range(N_CORES)) if _cores is None else _cores
    shared = {
        "cmp_pool": f(cache_cmp_kv).reshape(NPHYS * 8, 4096), "slc_pool": f(cache_slc_kv).reshape(NPHYS * 8, 4096),
        "w_ada": f(w_ada)[0], "b_ada": f(b_ada)[0], "w_in": _perm_w_in(f(w_in)[0]), "conv_wT": np.ascontiguousarray(f(conv_w)[0].reshape(3, 2, 128).transpose(2, 1, 0)),
        "b_adaT": np.ascontiguousarray(f(b_ada)[0].reshape(48, 128).T),
        "peT": np.ascontiguousarray(np.tile(f(cmp_pe)[0].transpose(2, 0, 1).reshape(64, 64), (2, 1))),
        "w1rep": np.ascontiguousarray(np.tile(f(cmp_w1)[0].transpose(2, 0, 1, 3).reshape(64, 2 * 32 * 128), (2, 1))), "b1T": np.ascontiguousarray(f(cmp_b1)[0].T), "w2k": np.ascontiguousarray(np.tile(f(cmp_w2)[0][0], (1, 2))), "w2v": f(cmp_w2)[0][1],
        "g_convT": np.ascontiguousarray(f(g_conv_out)[0].reshape(2, 128).T), "g_nsaT": np.ascontiguousarray(f(g_nsa_out)[0].reshape(6, 128).T), "w_o": f(w_o)[0],
        "ln1_g": f(ln1_g)[0], "ln1_b": f(ln1_b)[0], "ln2_g": f(ln2_g)[0], "ln2_b": f(ln2_b)[0],
        "w_g": f(w_ffn_gate)[0], "w_u": f(w_ffn_up)[0], "w_d": f(w_ffn_down)[0],
    }
    for k, v in consts.items():
        shared["k_" + k] = v
    xp = f(x_prompt)
    xs = f(x_sample)
    win = f(cache_win_kv)[0].reshape(32, 512, 256)
    sconv = f(state_conv)[0]
    pt = np.ascontiguousarray(np.asarray(page_table, dtype=np.int32))
    cp = f(c_prompt)
    cs = f(c_sample)
    in_maps = []
    for c in cores:
        m = dict(shared)
        m["xp"] = xp[c]
        m["xs"] = xs[4 * c:4 * c + 4].reshape(32, D)
        m["win"] = win[4 * c:4 * c + 4]
        m["sconvT"] = np.ascontiguousarray(sconv[4 * c:4 * c + 4].reshape(4, 2, 2, 128).transpose(3, 2, 0, 1))
        m["ptT"] = np.ascontiguousarray(pt[4 * c:4 * c + 4].T)
        m["cvecT"] = np.ascontiguousarray(np.concatenate([cp[c:c + 1], cs[4 * c:4 * c + 4]], axis=0).reshape(5, 8, 128).transpose(2, 1, 0))
        in_maps.append(m)
    res = run_bass_kernel_spmd(nc, in_maps, core_ids=list(range(len(cores))))
    R = res.results
    n = len(cores)
    y_p = np.stack([R[i]["y_p"] for i in range(n)])
    y_s = np.concatenate([R[i]["y_s"].reshape(4, 8, D) for i in range(n)])
    cmp_p = np.stack([R[i]["cmp_p"].reshape(SEQ, 2, 2, 64) for i in range(n)])[None]
    slc_p = np.stack([R[i]["slc_p"].reshape(SEQ, 2, 2, 64) for i in range(n)])[None]
    win_p = np.stack([R[i]["win_p"].reshape(512, 2, 2, 64) for i in range(n)])[None]
    conv_p = np.stack([R[i]["conv_p"].transpose(2, 1, 0).reshape(2, 256) for i in range(n)])[None]
    cmp_s = np.concatenate([R[i]["cmp_s"].reshape(4, 8, 2, 2, 64) for i in range(n)])[None]
    slc_s = np.concatenate([R[i]["slc_s"].reshape(4, 8, 2, 2, 64) for i in range(n)])[None]
    win_s = np.concatenate([R[i]["win_s"].reshape(4, 512, 2, 2, 64) for i in range(n)])[None]
    conv_s = np.concatenate([R[i]["conv_s"].transpose(2, 3, 1, 0).reshape(4, 2, 256) for i in range(n)])[None]
    return (y_p, y_s, cmp_p, slc_p, win_p, conv_p, cmp_s, slc_s, win_s, conv_s)
```
